# Optimizing a Trainium2 kernel written in Bass

```python
import jax, jax.numpy as jnp
from jax import lax
import numpy as np

D_MODEL = 4096
BATCH = 4
SEQ = 2048
DEPTH = 1
DEC_BATCH = 128
DEC_SEQ = 8
PAST_LEN = 8192
PAGE_SIZE = 128

MIX_WIDTH = D_MODEL
ATTN_WIDTH = MIX_WIDTH // 2
POOL_WIDTH = MIX_WIDTH - ATTN_WIDTH
HEAD_DIM = 64
N_HEADS = ATTN_WIDTH // HEAD_DIM
N_KV_HEADS = N_HEADS // 8
GROUP = N_HEADS // N_KV_HEADS
KV_WIDTH = N_KV_HEADS * HEAD_DIM
WINDOW = 128
ROT_DIM = HEAD_DIM // 4
ROPE_THETA = 500000.0
POOL_WINDOWS = (2, 4, 8, 16)
N_POOL_GROUPS = len(POOL_WINDOWS)
POOL_GROUP_WIDTH = POOL_WIDTH // N_POOL_GROUPS
POOL_STATE = max(POOL_WINDOWS) - 1
IN_COLS = ATTN_WIDTH + 2 * KV_WIDTH + POOL_WIDTH
PEER_HEADS = 8
PEER_KEY_DIM = 256
PEER_HALF = PEER_KEY_DIM // 2
N_KEYS = 128
N_EXPERTS = N_KEYS * N_KEYS
PEER_TOPK = 16
PEER_BLOCK = 64
ALPHA = (2 * DEPTH) ** 0.25
BETA = (8 * DEPTH) ** -0.25
LN_EPS = 1e-5
N_MOD = 6
NEG_INF = -1e30
F32 = jnp.float32

kernel_name = 'hymba_swa_sink_pool_peer_step'


def layer_norm(x, g, b):
    xf = x.astype(F32)
    mu = xf.mean(-1, keepdims=True)
    var = jnp.square(xf - mu).mean(-1, keepdims=True)
    return ((xf - mu) * lax.rsqrt(var + LN_EPS) * g.astype(F32) + b.astype(F32)).astype(x.dtype)


def adaln_modulation(c, w_ada, b_ada):
    mod = jax.nn.silu(c) @ w_ada + b_ada
    mod = mod.reshape(c.shape[0], N_MOD, D_MODEL)[:, :, None, :]
    return tuple(mod[:, i] for i in range(N_MOD))


def rope(x, pos):
    half = ROT_DIM // 2
    inv_freq = ROPE_THETA ** (-jnp.arange(half, dtype=F32) / half)
    ang = pos.astype(F32)[:, None] * inv_freq[None, :]
    shape = (pos.shape[0],) + (1,) * (x.ndim - 3) + (half,)
    cos = jnp.cos(ang).reshape(shape).astype(x.dtype)
    sin = jnp.sin(ang).reshape(shape).astype(x.dtype)
    x1 = x[..., :half]
    x2 = x[..., half:ROT_DIM]
    return jnp.concatenate([x1 * cos - x2 * sin, x2 * cos + x1 * sin, x[..., ROT_DIM:]], axis=-1)


def sink_attend(q, k, v, mask, sinks):
    s = jnp.einsum('...qhgd,...khd->...hgqk', q, k).astype(F32) * (HEAD_DIM ** -0.5)
    s = jnp.where(mask, s, NEG_INF)
    sink = sinks.astype(F32).reshape(N_KV_HEADS, GROUP, 1, 1)
    m = jnp.maximum(s.max(-1, keepdims=True), sink)
    p = jnp.exp(s - m)
    denom = p.sum(-1, keepdims=True) + jnp.exp(sink - m)
    return jnp.einsum('...hgqk,...khd->...qhgd', (p / denom).astype(v.dtype), v)


def banded_window_attention(q, k, v, sinks):
    b, s = q.shape[0], q.shape[1]
    nb = s // WINDOW
    qb = q.reshape(b, nb, WINDOW, N_KV_HEADS, GROUP, HEAD_DIM)

    def band(t):
        tp = jnp.pad(t, ((0, 0), (WINDOW, 0), (0, 0), (0, 0))).reshape(b, nb + 1, WINDOW, N_KV_HEADS, HEAD_DIM)
        return jnp.concatenate([tp[:, :-1], tp[:, 1:]], axis=2)

    i = jnp.arange(WINDOW)[:, None]
    j = jnp.arange(2 * WINDOW)[None, :]
    blk = jnp.arange(nb)[:, None, None]
    diff = WINDOW + i - j
    mask = (diff >= 0) & (diff <= WINDOW) & (blk * WINDOW + j - WINDOW >= 0)
    o = sink_attend(qb, band(k), band(v), mask[:, None, None], sinks)
    return o.reshape(b, s, N_KV_HEADS, GROUP, HEAD_DIM)


def cached_window_attention(q, k, v, cache_k, cache_v, pos, sinks):
    wc = cache_k.shape[1]
    kc = jnp.concatenate([cache_k, k], axis=1)
    vc = jnp.concatenate([cache_v, v], axis=1)
    kpos = jnp.arange(wc + k.shape[1], dtype=jnp.int32) + (PAST_LEN - wc)
    diff = pos[:, None] - kpos[None, :]
    mask = (diff >= 0) & (diff <= WINDOW)
    o = sink_attend(q, kc, vc, mask, sinks)
    return o, kc[:, -wc:], vc[:, -wc:]


def multiscale_pool(u, prev, pos, pool_w, pool_scale):
    n, t, _ = u.shape
    ext = jnp.concatenate([prev, u], axis=1)
    cs = jnp.pad(jnp.cumsum(ext.astype(F32), axis=1), ((0, 0), (1, 0), (0, 0)))
    outs = []
    for gi, w in enumerate(POOL_WINDOWS):
        sl = slice(gi * POOL_GROUP_WIDTH, (gi + 1) * POOL_GROUP_WIDTH)
        hi = cs[:, POOL_STATE + 1:POOL_STATE + 1 + t, sl]
        lo = cs[:, POOL_STATE + 1 - w:POOL_STATE + 1 - w + t, sl]
        cnt = jnp.minimum(w, pos + 1).astype(F32)[None, :, None]
        outs.append((hi - lo) / cnt)
    pooled = jnp.stack(outs, axis=2)
    d = (pooled - u.astype(F32).reshape(n, t, N_POOL_GROUPS, POOL_GROUP_WIDTH)).astype(u.dtype)
    y = jnp.einsum('ntgc,gcd->ntgd', d, pool_w).reshape(n, t, POOL_WIDTH) * pool_scale
    return y, ext[:, -POOL_STATE:]


def token_mixing(h, pos, prev_k, prev_v, prev_pool, w_in, sinks, pool_w, pool_scale, w_out):
    n, t, _ = h.shape
    proj = h @ w_in
    q = rope(proj[..., :ATTN_WIDTH].reshape(n, t, N_KV_HEADS, GROUP, HEAD_DIM), pos)
    k = rope(proj[..., ATTN_WIDTH:ATTN_WIDTH + KV_WIDTH].reshape(n, t, N_KV_HEADS, HEAD_DIM), pos)
    v = proj[..., ATTN_WIDTH + KV_WIDTH:ATTN_WIDTH + 2 * KV_WIDTH].reshape(n, t, N_KV_HEADS, HEAD_DIM)
    u = proj[..., ATTN_WIDTH + 2 * KV_WIDTH:]
    if prev_k is None:
        attn = banded_window_attention(q, k, v, sinks)
        new_k, new_v = k[:, -WINDOW:], v[:, -WINDOW:]
        prev_pool = jnp.zeros((n, POOL_STATE, POOL_WIDTH), u.dtype)
    else:
        attn, new_k, new_v = cached_window_attention(q, k, v, prev_k, prev_v, pos, sinks)
    pooled, new_pool = multiscale_pool(u, prev_pool, pos, pool_w, pool_scale)
    y = jnp.concatenate([attn.reshape(n, t, ATTN_WIDTH), pooled], axis=-1) @ w_out
    return y, (new_k, new_v, new_pool)


def peer_ffn(h, peer_wq, peer_subkeys, peer_u, peer_v):
    n, t, d = h.shape
    tot = n * t
    x = h.reshape(tot, d)
    q = (x @ peer_wq).reshape(tot, PEER_HEADS, 2, PEER_HALF)
    s = jnp.einsum('thpc,hpkc->thpk', q, peer_subkeys).astype(F32)
    top, idx = lax.top_k(s, PEER_TOPK)
    cand = (top[..., 0, :, None] + top[..., 1, None, :]).reshape(tot, PEER_HEADS, PEER_TOPK * PEER_TOPK)
    cidx = (idx[..., 0, :, None] * N_KEYS + idx[..., 1, None, :]).reshape(tot, PEER_HEADS, PEER_TOPK * PEER_TOPK)
    best, sel = lax.top_k(cand, PEER_TOPK)
    eidx = jnp.take_along_axis(cidx, sel, axis=-1).reshape(tot, PEER_HEADS * PEER_TOPK)
    gate = jax.nn.softmax(best, axis=-1).reshape(tot, PEER_HEADS * PEER_TOPK).astype(h.dtype)
    nblk = -(-tot // PEER_BLOCK)
    pad = nblk * PEER_BLOCK - tot
    xb = jnp.pad(x, ((0, pad), (0, 0))).reshape(nblk, PEER_BLOCK, d)
    ib = jnp.pad(eidx, ((0, pad), (0, 0))).reshape(nblk, PEER_BLOCK, PEER_HEADS * PEER_TOPK)
    gb = jnp.pad(gate, ((0, pad), (0, 0))).reshape(nblk, PEER_BLOCK, PEER_HEADS * PEER_TOPK)

    def expert_block(args):
        xk, ik, gk = args
        a = jnp.einsum('tkd,td->tk', peer_u[ik], xk)
        act = jax.nn.gelu(a, approximate=False) * gk
        return jnp.einsum('tk,tkd->td', act, peer_v[ik])

    y = lax.map(expert_block, (xb, ib, gb)).reshape(nblk * PEER_BLOCK, d)[:tot]
    return y.reshape(n, t, d)


def decoder_layer(x, c, pos, prev_k, prev_v, prev_pool, w_ada, b_ada, w_in, sinks, pool_w, pool_scale,
                  w_out, ln1_g, ln1_b, peer_wq, peer_subkeys, peer_u, peer_v, ln2_g, ln2_b):
    sh1, sc1, g1, sh2, sc2, g2 = adaln_modulation(c, w_ada, b_ada)
    y1, state = token_mixing(x * (1 + sc1) + sh1, pos, prev_k, prev_v, prev_pool,
                             w_in, sinks, pool_w, pool_scale, w_out)
    x = layer_norm(ALPHA * x + g1 * y1, ln1_g, ln1_b)
    y2 = peer_ffn(x * (1 + sc2) + sh2, peer_wq, peer_subkeys, peer_u, peer_v)
    x = layer_norm(ALPHA * x + g2 * y2, ln2_g, ln2_b)
    return x, state


def setup_inputs(seed: int = 0) -> dict:
    key = jax.random.key(seed)
    ks = jax.random.split(key, 24)
    win = min(WINDOW, PAST_LEN)

    def nrm(k, shape, scale):
        return jax.random.normal(k, shape, F32) * scale

    return {
        'x_prompt': nrm(ks[0], (BATCH, SEQ, D_MODEL), 1.0),
        'x_sample': nrm(ks[1], (DEC_BATCH, DEC_SEQ, D_MODEL), 1.0),
        'cache_k': nrm(ks[2], (DEPTH, DEC_BATCH, win, N_KV_HEADS, HEAD_DIM), 1.0),
        'cache_v': nrm(ks[3], (DEPTH, DEC_BATCH, win, N_KV_HEADS, HEAD_DIM), 1.0),
        'state_pool': nrm(ks[4], (DEPTH, DEC_BATCH, POOL_STATE, POOL_WIDTH), 1.0),
        'c_prompt': nrm(ks[5], (BATCH, D_MODEL), 1.0),
        'c_sample': nrm(ks[6], (DEC_BATCH, D_MODEL), 1.0),
        'w_ada': nrm(ks[7], (DEPTH, D_MODEL, N_MOD * D_MODEL), 0.5 * D_MODEL ** -0.5),
        'b_ada': nrm(ks[8], (DEPTH, N_MOD * D_MODEL), 0.01),
        'w_in': nrm(ks[9], (DEPTH, D_MODEL, IN_COLS), D_MODEL ** -0.5),
        'sinks': nrm(ks[10], (DEPTH, N_HEADS), 1.0),
        'pool_w': nrm(ks[11], (DEPTH, N_POOL_GROUPS, POOL_GROUP_WIDTH, POOL_GROUP_WIDTH), POOL_GROUP_WIDTH ** -0.5),
        'pool_scale': 1.0 + nrm(ks[12], (DEPTH, POOL_WIDTH), 0.1),
        'w_out': nrm(ks[13], (DEPTH, MIX_WIDTH, D_MODEL), BETA * MIX_WIDTH ** -0.5),
        'ln1_g': 1.0 + nrm(ks[14], (DEPTH, D_MODEL), 0.05),
        'ln1_b': nrm(ks[15], (DEPTH, D_MODEL), 0.02),
        'peer_wq': nrm(ks[16], (DEPTH, D_MODEL, PEER_HEADS * PEER_KEY_DIM), D_MODEL ** -0.5),
        'peer_subkeys': nrm(ks[17], (DEPTH, PEER_HEADS, 2, N_KEYS, PEER_HALF), PEER_HALF ** -0.5),
        'peer_u': nrm(ks[18], (DEPTH, N_EXPERTS, D_MODEL), D_MODEL ** -0.5),
        'peer_v': nrm(ks[19], (DEPTH, N_EXPERTS, D_MODEL), BETA),
        'ln2_g': 1.0 + nrm(ks[20], (DEPTH, D_MODEL), 0.05),
        'ln2_b': nrm(ks[21], (DEPTH, D_MODEL), 0.02),
    }


def reference(x_prompt, x_sample, cache_k, cache_v, state_pool, c_prompt, c_sample, w_ada, b_ada, w_in,
              sinks, pool_w, pool_scale, w_out, ln1_g, ln1_b, peer_wq, peer_subkeys, peer_u, peer_v,
              ln2_g, ln2_b):
    pos_p = jnp.arange(x_prompt.shape[1], dtype=jnp.int32)
    pos_s = PAST_LEN + jnp.arange(x_sample.shape[1], dtype=jnp.int32)
    xp, xs = x_prompt, x_sample
    kp, vp, pp, ksl, vsl, psl = [], [], [], [], [], []
    for l in range(DEPTH):
        lw = (w_ada[l], b_ada[l], w_in[l], sinks[l], pool_w[l], pool_scale[l], w_out[l], ln1_g[l], ln1_b[l],
              peer_wq[l], peer_subkeys[l], peer_u[l], peer_v[l], ln2_g[l], ln2_b[l])
        xp, (k1, v1, p1) = decoder_layer(xp, c_prompt, pos_p, None, None, None, *lw)
        xs, (k2, v2, p2) = decoder_layer(xs, c_sample, pos_s, cache_k[l], cache_v[l], state_pool[l], *lw)
        kp.append(k1)
        vp.append(v1)
        pp.append(p1)
        ksl.append(k2)
        vsl.append(v2)
        psl.append(p2)
    return (xp, xs, jnp.stack(kp), jnp.stack(vp), jnp.stack(pp), jnp.stack(ksl), jnp.stack(vsl), jnp.stack(psl))
```

```python
import numpy as np
from contextlib import ExitStack
import concourse.bass as bass
import concourse.mybir as mybir
from concourse.bass_utils import run_bass_kernel_spmd

F32 = mybir.dt.float32
BF16 = mybir.dt.bfloat16
ALU = mybir.AluOpType
AF = mybir.ActivationFunctionType
AX = mybir.AxisListType
ENG = ('pe', 'act', 'dve', 'pool', 'sp')


class Prog:
    def __init__(self, nc, es):
        self.nc = nc
        self.es = es
        self.ops = {e: [] for e in ENG}
        self.n = {e: 0 for e in ENG}
        self.seen = {e: {} for e in ENG}
        self.res = {}
        self.dcount = {}
        self.targets = {e: set() for e in ENG}
        self.out_slots = set()

    def _deps(self, reads, writes):
        d = []
        for k in reads:
            r = self.res.get(k)
            if r and r[0] is not None:
                d.append(r[0])
        for k in writes:
            r = self.res.get(k)
            if r:
                if r[0] is not None:
                    d.append(r[0])
                for sk, v in r[1].items():
                    d.append((sk[0], sk[1], v))
        return d

    def _waits(self, eng, deps, skip_same=False):
        for kind, key, val in deps:
            if kind == 'e' and key == eng and skip_same:
                continue
            if kind == 'd':
                val = self.dcount[key]
            sk = (kind, key)
            if self.seen[eng].get(sk, -1) >= val:
                continue
            self.seen[eng][sk] = val
            self.ops[eng].append(('w', kind, key, val))
            if kind == 'e':
                self.targets[key].add(val)

    def _update(self, me, reads, writes):
        for k in writes:
            self.res[k] = [me, {}]
        for k in reads:
            r = self.res.setdefault(k, [None, {}])
            sk = (me[0], me[1])
            if r[1].get(sk, -1) < me[2]:
                r[1][sk] = me[2]

    def op(self, eng, fn, reads=(), writes=(), skip_same=False):
        self._waits(eng, self._deps(reads, writes), skip_same)
        idx = self.n[eng]
        self.n[eng] += 1
        self.ops[eng].append(('i', fn, idx))
        self._update(('e', eng, idx), reads, writes)

    def dma(self, eng, out, in_, reads=(), writes=(), slot=None, is_out=False):
        self._waits(eng, self._deps(reads, writes))
        c = self.dcount.get(slot, 0) + 16
        self.dcount[slot] = c
        self.ops[eng].append(('d', out, in_, slot))
        self._update(('d', slot, c), reads, writes)
        if is_out:
            self.out_slots.add(slot)

    def barrier(self):
        for e in ENG:
            deps = []
            for f in ENG:
                if f != e and self.n[f] > 0:
                    deps.append(('e', f, self.n[f] - 1))
            for s, c in self.dcount.items():
                deps.append(('d', s, c))
            self._waits(e, deps)

    def mm(self, out, lhsT, rhs, start, stop, reads=(), writes=()):
        self.op('pe', lambda h: h.matmul(out, lhsT, rhs, start=start, stop=stop), reads, writes, skip_same=True)

    def tr(self, out, in_, ident, reads=(), writes=()):
        self.op('pe', lambda h: h.transpose(out, in_, ident), reads, writes, skip_same=True)

    def actf(self, out, in_, func, bias=None, scale=None, reads=(), writes=(), eng='act'):
        kw = {}
        if bias is not None:
            kw['bias'] = bias
        if scale is not None:
            kw['scale'] = scale
        self.op(eng, lambda h: h.activation(out, in_, func, **kw), reads, writes)

    def tt(self, eng, out, in0, in1, op, reads=(), writes=()):
        self.op(eng, lambda h: h.tensor_tensor(out, in0, in1, op), reads, writes)

    def ts(self, eng, out, in0, s1, s2, op0, op1=None, reads=(), writes=()):
        if op1 is None:
            self.op(eng, lambda h: h.tensor_scalar(out, in0, s1, None, op0), reads, writes)
        else:
            self.op(eng, lambda h: h.tensor_scalar(out, in0, s1, s2, op0, op1), reads, writes)

    def stt(self, eng, out, in0, scalar, in1, op0, op1, reads=(), writes=()):
        self.op(eng, lambda h: h.scalar_tensor_tensor(out, in0, scalar, in1, op0, op1), reads, writes)

    def copy(self, eng, out, in_, reads=(), writes=()):
        if eng == 'act':
            self.op(eng, lambda h: h.copy(out, in_), reads, writes)
        else:
            self.op(eng, lambda h: h.tensor_copy(out, in_), reads, writes)

    def emit(self):
        nc = self.nc
        es = self.es
        sem = {e: es.enter_context(nc.semaphore("sem_" + e)) for e in ENG}
        dsem = {}
        for i, s in enumerate(self.dcount):
            dsem[s] = es.enter_context(nc.semaphore("dsem%d" % i))
        rank = {e: {idx: i + 1 for i, idx in enumerate(sorted(self.targets[e]))} for e in ENG}
        for s in sorted(self.out_slots, key=str):
            self.ops['sp'].append(('w', 'd', s, self.dcount[s]))
        block = es.enter_context(nc.Block())

        def run(e, h):
            for rec in self.ops[e]:
                if rec[0] == 'w':
                    _, kind, key, val = rec
                    if kind == 'e':
                        h.wait_ge(sem[key], rank[key][val])
                    else:
                        h.wait_ge(dsem[key], val)
                elif rec[0] == 'i':
                    ins = rec[1](h)
                    if rec[2] in rank[e]:
                        ins.then_inc(sem[e], 1)
                else:
                    _, out, in_, slot = rec
                    h.dma_start(out=out, in_=in_).then_inc(dsem[slot], 16)

        block.tensor(lambda h: run('pe', h))
        block.scalar(lambda h: run('act', h))
        block.vector(lambda h: run('dve', h))
        block.gpsimd(lambda h: run('pool', h))
        block.sync(lambda h: run('sp', h))
        print("PROG ops:", {e: len(self.ops[e]) for e in ENG}, "dsems", len(dsem))


CUT = 99
D = 4096
ALPHA = 2.0 ** 0.25
LN_EPS = 1e-5
MP, MC, MFP, MSN, IDM, BP, BC, BPF, BCF, BSA, BSB, BSM, MSC = 0, 1, 2, 3, 4, 5, 9, 13, 17, 21, 25, 29, 33
NMAT = 34
STAGES = 99
VAL_ENG = 'pool'


def build_nc(stages=STAGES, groups=(0, 1, 2), skipA=False):
    nc = bass.Bass("TRN2", target_bir_lowering=False)
    dt_in = lambda n, s: nc.dram_tensor(n, s, F32, kind="ExternalInput").ap()
    dt_out = lambda n, s: nc.dram_tensor(n, s, F32, kind="ExternalOutput").ap()
    xT_d = dt_in("xT", [D, 1280])
    cT_d = dt_in("cT", [128, 32, 17])
    cmat_d = dt_in("cmat", [128, NMAT, 128])
    rope_d = dt_in("rope", [128, 10, 2, 8])
    vecs_d = dt_in("vecs", [128, 384])
    cKT_d = dt_in("cKT", [128, 16, 4, 128])
    cV_d = dt_in("cV", [128, 16, 4, 128])
    ckn_d = dt_in("ckn", [16, 128, 256])
    cvn_d = dt_in("cvn", [16, 128, 256])
    sptm_d = dt_in("sptm", [120, 2, 2048])
    spn_d = dt_in("spn", [16, 15, 2048])
    wada_d = dt_in("w_ada", [D, 24576] if not skipA else [128, 128])
    win_d = dt_in("w_in", [D, 5120])
    wout_d = dt_in("w_out", [D, D] if stages >= 4 else [128, 128])
    wq_d = dt_in("wq", [D, 2048] if stages >= 5 else [128, 128])
    subk_d = dt_in("subk", [128, 16, 128])
    UT_d = dt_in("UT", [D, 16384] if stages >= 5 else [128, 128])
    V_d = dt_in("V", [16384, D] if stages >= 5 else [128, 128])
    poolw_d = dt_in("poolw", [128, 4, 4, 512])
    yT_d = dt_out("yT", [D, 1152])
    nkp_d = dt_out("nkp", [128, 256])
    nvp_d = dt_out("nvp", [128, 256])
    npp_d = dt_out("npp", [15, 2048])
    nks_d = dt_out("nks", [16, 128, 256])
    nvs_d = dt_out("nvs", [16, 128, 256])
    nps_d = dt_out("nps", [16, 15, 2048])

    es = ExitStack()
    with es:
        P = Prog(nc, es)
        sb = lambda name, shape, dt: es.enter_context(nc.sbuf_tensor("s_" + name, shape, dt))
        ps = [es.enter_context(nc.psum_tensor("ps%d" % i, [128, 512], F32)) for i in range(8)]
        psk = lambda i: ('ps', i)

        cmat = sb("cmat", [128, NMAT, 128], BF16)
        rope = sb("rope", [128, 10, 2, 8], F32)
        vecs = sb("vecs", [128, 384], F32)
        modP = sb("modP", [128, 192], F32)
        modS = sb("modS", [128, 192, 16], F32)
        cT = sb("cTs", [128, 32, 17], F32)
        siluT = sb("siluT", [128, 32, 17], BF16)
        sinkE = sb("sinkE", [128, 32], F32)
        ones_b = sb("ones_b", [128, 128], BF16)
        ones_f = sb("ones_f", [128, 128], F32)
        W = [sb("W%d" % i, [128, 8192], BF16) for i in range(3)]
        R2 = sb("R2", [128, 12288], BF16)
        big = sb("big", [128, 12288], F32)
        S2 = sb("S2", [128, 14848], F32)

        b_adaT = vecs[:, 0:192]
        pscT = vecs[:, 192:208]
        ln1g, ln1b, ln2g, ln2b = (vecs[:, 208 + 32 * i:240 + 32 * i] for i in range(4))
        ident = cmat[:, IDM, :]

        class Carver:
            def __init__(self, t):
                self.t = t
                self.off = 0

            def get(self, shape, dt):
                n = int(np.prod(shape[1:]))
                nb = n * (2 if dt == BF16 else 4)
                nw = (nb + 3) // 4
                a = self.t[:, self.off:self.off + nw]
                self.off += nw
                assert self.off <= 14848, self.off
                if dt == BF16:
                    a = a.bitcast(BF16)[:, 0:n]
                if len(shape) == 3:
                    a = a.rearrange("p (a b) -> p a b", a=shape[1])
                elif len(shape) == 4:
                    a = a.rearrange("p (a b c) -> p a b c", a=shape[1], b=shape[2])
                return a

        wslot = [0]

        def wload(src_ap, view_shape, key_extra=None):
            s = wslot[0] % 3
            wslot[0] += 1
            n = int(np.prod(view_shape[1:]))
            a = W[s][0:view_shape[0], 0:n]
            if len(view_shape) == 3:
                a = a.rearrange("p (a b) -> p a b", a=view_shape[1])
            P.dma('pool', a, src_ap, writes=[('W', s)], slot=('W', s))
            return a, ('W', s)

        P.dma('pool', cmat[:], cmat_d, writes=['cmat'], slot='c_cmat')
        P.dma('sp', rope[:], rope_d, writes=['rope'], slot='c_rope')
        P.dma('sp', vecs[:], vecs_d, writes=['vecs'], slot='c_vecs')
        P.dma('sp', cT[:], cT_d, writes=['cT'], slot='c_cT')
        P.op('dve', lambda h: h.memset(ones_b[:], 1.0), writes=['ones_b'])
        P.op('dve', lambda h: h.memset(ones_f[:], 1.0), writes=['ones_f'])
        P.actf(siluT[:], cT[:], AF.Silu, reads=['cT'], writes=['siluT'])
        P.actf(sinkE[:], vecs[:, 336:368], AF.Exp, reads=['vecs'], writes=['sinkE'])
        P.dma('sp', nks_d[:, 0:120, :], ckn_d[:, 8:128, :], slot='o_misc', is_out=True)
        P.dma('sp', nvs_d[:, 0:120, :], cvn_d[:, 8:128, :], slot='o_misc', is_out=True)
        P.dma('sp', nps_d[:, 0:7, :], spn_d[:, 8:15, :], slot='o_misc', is_out=True)

        wada_v = wada_d.rearrange("(kc p) c -> p kc c", p=128)
        if skipA:
            P.op('dve', lambda h: h.memset(modP[:], 0.25), writes=['mod'])
            P.op('dve', lambda h: h.memset(modS[:], 0.25), writes=['mod'])
        WA = [big[:, 0:8192].bitcast(BF16).rearrange("p (a b) -> p a b", a=32),
              S2[:, 0:8192].bitcast(BF16).rearrange("p (a b) -> p a b", a=32)]
        for cg in range(0 if skipA else 48):
            s = cg % 2
            P.dma('pool', WA[s], wada_v[:, :, cg * 512:(cg + 1) * 512], writes=[('WA', s)], slot=('WA', s))
            bank = cg % 2
            pv = ps[bank][:, 0:68].rearrange("p (j n) -> p j n", j=4)
            for j in range(4):
                for kc in range(32):
                    P.mm(pv[:, j, :], WA[s][:, kc, j * 128:(j + 1) * 128], siluT[:, kc, :], kc == 0, kc == 31,
                         reads=[('WA', s), 'siluT'], writes=[psk(bank)])
            m0 = cg * 4
            P.tt('dve', modP[:, m0:m0 + 4].unsqueeze(2), pv[:, :, 0:1], b_adaT[:, m0:m0 + 4].unsqueeze(2), ALU.add,
                 reads=[psk(bank), 'vecs'], writes=['mod'])
            P.tt('dve', modS[:, m0:m0 + 4, :], pv[:, :, 1:17],
                 b_adaT[:, m0:m0 + 4].unsqueeze(2).to_broadcast([128, 4, 16]), ALU.add,
                 reads=[psk(bank), 'vecs'], writes=['mod'])
        for m in (1, 4):
            P.ts('dve', modP[:, m * 32:(m + 1) * 32], modP[:, m * 32:(m + 1) * 32], 1.0, None, ALU.add,
                 reads=['mod'], writes=['mod'])
            P.ts('dve', modS[:, m * 32:(m + 1) * 32, :], modS[:, m * 32:(m + 1) * 32, :], 1.0, None, ALU.add,
                 reads=['mod'], writes=['mod'])
        P.barrier()

        xv = xT_d.rearrange("(kc p) t -> p kc t", p=128)
        yv = yT_d.rearrange("(kc p) t -> p kc t", p=128)
        win_v = win_d.rearrange("(kc p) c -> p kc c", p=128)
        wout_v = wout_d.rearrange("(kc p) c -> p kc c", p=128)
        wq_v = wq_d.rearrange("(kc p) c -> p kc c", p=128)
        UT_v = UT_d.rearrange("(kc p) c -> p kc c", p=128)
        V_v = V_d.rearrange("(c p) f -> p c f", p=128)

        def bc_s(ap_b16, n=8):
            return ap_b16.unsqueeze(2).to_broadcast([ap_b16.shape[0], 16, n])

        pbank = [0]

        def nextbank(lo, hi):
            b = lo + pbank[0] % (hi - lo)
            pbank[0] += 1
            return b

        def ln_block(z, gcol, bcol, g, mod_post, stash):
            cv = Carver(S2)
            cv.off = 11000
            mean = cv.get([128, 384], F32)
            rstd = cv.get([128, 384], F32)
            tmpn = cv.get([128, 384], F32)
            P.op('act', lambda h: h.mul(mean, ps[6][:, 0:384], 1.0 / D), reads=[psk(6)], writes=['mean'])
            P.op('act', lambda h: h.mul(rstd, ps[7][:, 0:384], 1.0 / D), reads=[psk(7)], writes=['rstd'])
            P.tt('dve', tmpn, mean, mean, ALU.mult, reads=['mean'], writes=['tmpn'])
            P.tt('dve', rstd, rstd, tmpn, ALU.subtract, reads=['rstd', 'tmpn'], writes=['rstd'])
            P.ts('dve', rstd, rstd, LN_EPS, None, ALU.add, reads=['rstd'], writes=['rstd'])
            P.actf(rstd, rstd, AF.Sqrt, reads=['rstd'], writes=['rstd'])
            P.op('dve', lambda h: h.reciprocal(rstd, rstd), reads=['rstd'], writes=['rstd'])
            for fc in range(32):
                zk = ('z', fc)
                P.tt('dve', z[:, fc, :], z[:, fc, :], mean, ALU.subtract, reads=[zk, 'mean'], writes=[zk])
                P.tt('dve', z[:, fc, :], z[:, fc, :], rstd, ALU.mult, reads=[zk, 'rstd'], writes=[zk])
                P.ts('dve', z[:, fc, :], z[:, fc, :], gcol[:, fc:fc + 1], bcol[:, fc:fc + 1], ALU.mult, ALU.add,
                     reads=[zk, 'vecs'], writes=[zk])
                if mod_post is not None:
                    mod_post(fc)
                if stash is not None and fc % 4 == 3:
                    stash(fc - 3)

        for g in (groups if stages >= 2 else ()):
            c0 = 3 * g * 128
            smp = (g == 2)
            ncp = 384 if smp else 512
            own = [1, 2] if smp else [1, 2, 3]
            tiles_all = [0, 1, 2, 3]
            hT = big[:, 0:8192].bitcast(BF16).rearrange("p (a b) -> p a b", a=32)
            xs = big[:, 8192:12288].rearrange("p (s j t) -> p s j t", s=2, j=4)
            mixT = R2[:, :].rearrange("p (a b) -> p a b", a=32)
            cv = Carver(S2)
            KT = cv.get([128, 4, 512], BF16)
            Vtok = cv.get([128, 4, 512], BF16)
            stf = [cv.get([128, 512], F32) for _ in range(2)]
            stb = [cv.get([128, 512], BF16) for _ in range(2)]
            rt = cv.get([128, 4, 64], F32)
            QTz = cv.get([128, 2, 4, 384], BF16)
            Eb = cv.get([128, 2048], BF16)
            Ef = cv.get([128, 2048], F32)
            Rr = cv.get([128, 2, 512], F32)
            cK = cv.get([128, 16, 128], BF16)
            cVt = cv.get([128, 16, 128], BF16)
            Ec = cv.get([128, 1024], BF16)
            tmpS = cv.get([128, 128], F32)
            off_pool = cv.off
            P.op('dve', lambda h: h.memset(QTz, 0.0), writes=['QT'])
            for kq in range(8):
                sl = kq % 2
                P.dma('sp', xs[:, sl], xv[:, kq * 4:(kq + 1) * 4, c0:c0 + 512], writes=[('xs', sl)], slot=('xs', sl))
                for j in range(4):
                    kc = kq * 4 + j
                    if kc % 2 == 0:
                        P.ts('dve', hT[:, kc, 0:ncp], xs[:, sl, j, 0:ncp], modP[:, 32 + kc:33 + kc], modP[:, kc:kc + 1],
                             ALU.mult, ALU.add, reads=[('xs', sl), 'mod'], writes=[('hT', kc)])
                    else:
                        P.actf(hT[:, kc, 0:ncp], xs[:, sl, j, 0:ncp], AF.Identity, bias=modP[:, kc:kc + 1],
                               scale=modP[:, 32 + kc:33 + kc], reads=[('xs', sl), 'mod'], writes=[('hT', kc)])
                    if smp:
                        tv = tmpS.rearrange("p (b t) -> p b t", b=16)
                        P.tt('dve', tv, xs[:, sl, j, 384:512].rearrange("p (b t) -> p b t", b=16),
                             bc_s(modS[:, 32 + kc, :]), ALU.mult, reads=[('xs', sl), 'mod'], writes=['tmpS'])
                        P.tt('dve', hT[:, kc, 384:512].rearrange("p (b t) -> p b t", b=16), tv,
                             bc_s(modS[:, kc, :]), ALU.add, reads=['tmpS', 'mod'], writes=[('hT', kc)])
            if CUT == 1:
                P.barrier()
                continue

            def rope_inplace(f, gt):
                f3 = f.rearrange("p (h d) -> p h d", h=8)
                x1 = f3[:, :, 0:8]
                x2 = f3[:, :, 8:16]
                cs = rope[:, gt, 0, :].unsqueeze(1).to_broadcast([128, 8, 8])
                sn = rope[:, gt, 1, :].unsqueeze(1).to_broadcast([128, 8, 8])
                r = [rt[:, i, :].rearrange("p (h d) -> p h d", h=8) for i in range(4)]
                P.tt('dve', r[0], x1, cs, ALU.mult, reads=['stf', 'rope'], writes=['rt'])
                P.tt('dve', r[1], x2, sn, ALU.mult, reads=['stf', 'rope'], writes=['rt'])
                P.tt('dve', r[2], x2, cs, ALU.mult, reads=['stf', 'rope'], writes=['rt'])
                P.tt('dve', r[3], x1, sn, ALU.mult, reads=['stf', 'rope'], writes=['rt'])
                P.tt('dve', x1, r[0], r[1], ALU.subtract, reads=['rt'], writes=['stf'])
                P.tt('dve', x2, r[2], r[3], ALU.add, reads=['rt'], writes=['stf'])

            def inproj(cg, lts, evac):
                tl = []
                for half in range(2):
                    a, k = wload(win_v[:, :, cg * 512 + half * 256: cg * 512 + half * 256 + 256], [128, 32, 256])
                    tl.append((a, k))
                for lt in lts:
                    bank = nextbank(0, 4)
                    for half in range(2):
                        a, k = tl[half]
                        for kc in range(32):
                            P.mm(ps[bank][:, half * 256:(half + 1) * 256], hT[:, kc, lt * 128:(lt + 1) * 128], a[:, kc, :],
                                 kc == 0, kc == 31, reads=[('hT', kc), k], writes=[psk(bank)])
                    if CUT >= 3:
                        evac(lt, bank)

            def out_rows(dst_p, dst_s, src, lt, view=None):
                if not smp:
                    return
                if lt == 2 and dst_p is not None:
                    P.dma('sp', dst_p, src if view is None else view(src), reads=['stf'], slot='o_misc', is_out=True)
                if lt == 3:
                    for b in range(16):
                        s_ = src[b * 8:(b + 1) * 8]
                        P.dma('sp', dst_s(b), s_ if view is None else view(s_), reads=['stf'], slot='o_misc', is_out=True)

            kview = lambda a: a.rearrange("p (k c d) -> p k c d", k=4, c=2)[:, :, 0, :]

            def evac_k(lt, bank):
                gt = 3 * g + lt
                f = stf[0]
                P.copy('act', f, ps[bank][:, :], reads=[psk(bank)], writes=['stf'])
                if CUT == 3:
                    return
                rope_inplace(f, gt)
                P.copy('act', stb[0], f, reads=['stf'], writes=['stb'])
                if CUT == 4:
                    return
                pb = ps[4 + lt % 2][:].bitcast(BF16)
                for kv in range(4):
                    P.tr(pb[:, kv * 128:(kv + 1) * 128], stb[0][:, kv * 128:(kv + 1) * 128], ident,
                         reads=['stb', 'cmat'], writes=[psk(4 + lt % 2)])
                P.copy('dve', KT[:, :, lt * 128:(lt + 1) * 128], pb[:, 0:512].rearrange("p (k t) -> p k t", k=4),
                       reads=[psk(4 + lt % 2)], writes=['KT'])
                out_rows(nkp_d.rearrange("p (k d) -> p k d", k=4),
                         lambda b: nks_d[b, 120:128, :].rearrange("p (k d) -> p k d", k=4), f, lt, kview)
            inproj(0, tiles_all, evac_k)
            if CUT <= 5:
                P.barrier()
                continue

            def evac_v(lt, bank):
                P.copy('dve', Vtok[:, lt, :], ps[bank][:, :], reads=[psk(bank)], writes=[('Vtok', lt)])
                if smp and lt >= 2:
                    f = stf[0]
                    P.copy('dve', f, ps[bank][:, :], reads=[psk(bank)], writes=['stf'])
                    out_rows(nvp_d.rearrange("p (k d) -> p k d", k=4),
                             lambda b: nvs_d[b, 120:128, :].rearrange("p (k d) -> p k d", k=4), f, lt, kview)
            inproj(1, tiles_all, evac_v)
            if CUT == 6:
                P.barrier()
                continue

            for kvh in range(4 if stages >= 3 else 0):
                def evac_q(lt, bank):
                    gt = 3 * g + lt
                    f = stf[1]
                    P.copy('act', f, ps[bank][:, :], reads=[psk(bank)], writes=['stf'])
                    rope_inplace(f, gt)
                    P.copy('act', stb[1], f, reads=['stf'], writes=['stb'])
                    pb = ps[4 + lt % 2][:].bitcast(BF16)
                    for pc in range(4):
                        P.tr(pb[:, pc * 128:(pc + 1) * 128], stb[1][:, pc * 128:(pc + 1) * 128], ident,
                             reads=['stb', 'cmat'], writes=[psk(4 + lt % 2)])
                    for c in range(2):
                        hs = slice(c * 64, (c + 1) * 64)
                        P.copy('dve', QTz[hs, c, :, (lt - 1) * 128:lt * 128], pb[hs, 0:512].rearrange("p (k t) -> p k t", k=4),
                               reads=[psk(4 + lt % 2)], writes=['QT'])
                inproj(2 + kvh, [1, 2, 3], evac_q)
                if smp:
                    P.dma('pool', cK, cKT_d[:, :, kvh, :], writes=['cK'], slot='cK')
                    P.dma('pool', cVt, cV_d[:, :, kvh, :], writes=['cV'], slot='cV')
                for lt in [1, 2, 3]:
                    if CUT == 7:
                        break
                    qc = (lt - 1) * 128
                    is_s = smp and lt == 3
                    vsl = slice(kvh * 128, (kvh + 1) * 128)
                    if not is_s:
                        for kt, ktile in enumerate([lt - 1, lt]):
                            for c in range(2):
                                bank = kt * 2 + c
                                hs = slice(c * 64, (c + 1) * 64)
                                P.mm(ps[bank][:, :].rearrange("p (a b) -> p a b", a=4),
                                     KT[:, kvh, ktile * 128:(ktile + 1) * 128], QTz[:, c, :, qc:qc + 128], True, True,
                                     reads=['KT', 'QT'], writes=[psk(bank)])
                                if CUT == 8:
                                    continue
                                P.actf(Ef[:, bank * 512:(bank + 1) * 512], ps[bank][:, :], AF.Exp, scale=0.125,
                                       reads=[psk(bank)], writes=[('Ef', bank)])
                            if CUT in (8, 9):
                                continue
                            midx = MC if kt == 1 else (MFP if (g == 0 and lt == 1) else MP)
                            ev = Eb[:, kt * 1024:(kt + 1) * 1024].rearrange("p (a b) -> p a b", a=8)
                            efv = Ef[:, kt * 1024:(kt + 1) * 1024].rearrange("p (a b) -> p a b", a=8)
                            P.tt('dve', ev, efv, cmat[:, midx, :].unsqueeze(1).to_broadcast([128, 8, 128]), ALU.mult,
                                 reads=[('Ef', kt * 2), ('Ef', kt * 2 + 1), 'cmat'], writes=[('Eb', kt * 2), ('Eb', kt * 2 + 1)])
                        for c in range(2):
                            if CUT in (8, 9, 10):
                                continue
                            P.mm(ps[6 + c][:, :], ones_b[:], Eb[:, c * 512:(c + 1) * 512], True, False,
                                 reads=[('Eb', c), 'ones_b'], writes=[psk(6 + c)])
                            P.mm(ps[6 + c][:, :], ones_b[:], Eb[:, (2 + c) * 512:(3 + c) * 512], False, True,
                                 reads=[('Eb', 2 + c)], writes=[psk(6 + c)])
                            P.mm(ps[4 + c][:, :], Vtok[:, lt - 1, vsl], Eb[:, c * 512:(c + 1) * 512], True, False,
                                 reads=[('Eb', c), ('Vtok', lt - 1 if not is_s else 3)], writes=[psk(4 + c)])
                            P.mm(ps[4 + c][:, :], Vtok[:, lt, vsl], Eb[:, (2 + c) * 512:(3 + c) * 512], False, True,
                                 reads=[('Eb', 2 + c), ('Vtok', lt)], writes=[psk(4 + c)])
                    else:
                        for c in range(2):
                            hs = slice(c * 64, (c + 1) * 64)
                            P.mm(ps[c][:, :].rearrange("p (a b) -> p a b", a=4), KT[:, kvh, 384:512], QTz[:, c, :, qc:qc + 128],
                                 True, True, reads=['KT', 'QT'], writes=[psk(c)])
                            P.actf(Ef[:, c * 512:(c + 1) * 512], ps[c][:, :], AF.Exp, scale=0.125,
                                   reads=[psk(c)], writes=[('Ef', c)])
                        ev = Eb[:, 0:1024].rearrange("p (a b) -> p a b", a=8)
                        efv = Ef[:, 0:1024].rearrange("p (a b) -> p a b", a=8)
                        P.tt('dve', ev, efv, cmat[:, MSN, :].unsqueeze(1).to_broadcast([128, 8, 128]), ALU.mult,
                             reads=[('Ef', 0), ('Ef', 1), 'cmat'], writes=[('Eb', 0), ('Eb', 1)])
                        for b in range(16):
                            for c in range(2):
                                hs = slice(c * 64, (c + 1) * 64)
                                off = ((b % 8) * 2 + c) * 32
                                P.mm(ps[2 + b // 8][:, off:off + 32].rearrange("p (a t) -> p a t", a=4), cK[:, b, :],
                                     QTz[:, c, :, qc + b * 8:qc + b * 8 + 8], True, True,
                                     reads=['cK', 'QT'], writes=[psk(2 + b // 8)])
                        for hb in range(2):
                            P.actf(Ef[:, 1024 + hb * 512:1024 + (hb + 1) * 512], ps[2 + hb][:, :], AF.Exp, scale=0.125,
                                   reads=[psk(2 + hb)], writes=[('Ef', 2 + hb)])
                        ecv = Ec.rearrange("p (a t) -> p a t", t=8)
                        efv = Ef[:, 1024:2048].rearrange("p (a t) -> p a t", t=8)
                        P.tt('dve', ecv, efv, cmat[:, MSC, 0:8].unsqueeze(1).to_broadcast([128, 128, 8]), ALU.mult,
                             reads=[('Ef', 2), ('Ef', 3), 'cmat'], writes=['Ec'])
                        Ec4 = Ec.rearrange("p (b c a t) -> p b c a t", b=16, c=2, a=4)
                        for c in range(2):
                            pperm = lambda bk: ps[bk][:, :].rearrange("p (a b t) -> p b a t", a=4, b=16)
                            P.mm(ps[6 + c][:, :], ones_b[:], Eb[:, c * 512:(c + 1) * 512], True, False,
                                 reads=[('Eb', c), 'ones_b'], writes=[psk(6 + c)])
                            P.mm(pperm(6 + c), ones_b[:], Ec4[:, :, c, :, :], False, True,
                                 reads=['Ec'], writes=[psk(6 + c)])
                            P.mm(ps[4 + c][:, :], Vtok[:, 3, vsl], Eb[:, c * 512:(c + 1) * 512], True, False,
                                 reads=[('Eb', c), ('Vtok', lt - 1 if not is_s else 3)], writes=[psk(4 + c)])
                            for b in range(16):
                                P.mm(pperm(4 + c)[:, b, :, :], cVt[:, b, :], Ec4[:, b, c, :, :], False, b == 15,
                                     reads=['Ec', 'cV'], writes=[psk(4 + c)])
                    for c in range(2):
                        if CUT in (8, 9, 10, 11):
                            continue
                        hs = slice(c * 64, (c + 1) * 64)
                        rv = Rr[hs, c, :].rearrange("p (a b) -> p a b", a=4)
                        sk = sinkE[hs, c * 16 + kvh * 4: c * 16 + kvh * 4 + 4].unsqueeze(2).to_broadcast([64, 4, 128])
                        P.tt('dve', rv, ps[6 + c][hs, :].rearrange("p (a b) -> p a b", a=4), sk, ALU.add,
                             reads=[psk(6 + c), 'sinkE'], writes=['Rr'])
                        P.op('dve', lambda h, rv=rv: h.reciprocal(rv, rv), reads=['Rr'], writes=['Rr'])
                        P.tt('dve', mixT[hs, kvh * 4:(kvh + 1) * 4, qc:qc + 128],
                             ps[4 + c][hs, :].rearrange("p (a b) -> p a b", a=4), rv, ALU.mult,
                             reads=[psk(4 + c), 'Rr'], writes=['mixT'])

            if CUT in (7, 8, 9, 10, 11, 12):
                P.barrier()
                continue
            cv.off = off_pool
            Utok = cv.get([128, 4, 512], BF16)
            dT = cv.get([128, 4, 384], BF16)
            SPt = cv.get([128, 2, 512], BF16)
            for g4 in range(4 if stages >= 3 else (4 if stages >= 2 else 0)):
                def evac_u(lt, bank):
                    P.copy('dve', Utok[:, lt, :], ps[bank][:, :], reads=[psk(bank)], writes=[('Utok', lt)])
                    if smp and lt >= 2:
                        f = stf[0]
                        P.copy('dve', f, ps[bank][:, :], reads=[psk(bank)], writes=['stf'])
                        if lt == 2:
                            P.dma('sp', npp_d[:, g4 * 512:(g4 + 1) * 512], f[113:128, :], reads=['stf'], slot='o_misc', is_out=True)
                        else:
                            for b in range(16):
                                P.dma('sp', nps_d[b, 7:15, g4 * 512:(g4 + 1) * 512], f[b * 8:(b + 1) * 8, :], reads=['stf'],
                                      slot='o_misc', is_out=True)
                inproj(6 + g4, tiles_all, evac_u)
                if stages < 3:
                    continue
                Wp, wpk = wload(poolw_d[:, g4], [128, 4, 512])
                if smp:
                    P.dma('pool', SPt[0:120], sptm_d[:, :, g4 * 512:(g4 + 1) * 512], writes=['SPt'], slot='SPt')
                for lt in [1, 2, 3]:
                    bank = nextbank(0, 2)
                    first = (g == 0 and lt == 1)
                    for cc in range(4):
                        o = ps[bank][:, cc * 128:(cc + 1) * 128]
                        csl = slice(cc * 128, (cc + 1) * 128)
                        if smp and lt == 3:
                            P.mm(o, SPt[0:120, 0, csl], cmat[0:120, BSA + g4, :], True, False, reads=['SPt', 'cmat'], writes=[psk(bank)])
                            P.mm(o, SPt[0:120, 1, csl], cmat[0:120, BSB + g4, :], False, False, reads=['SPt'], writes=[psk(bank)])
                            P.mm(o, Utok[:, 3, csl], cmat[:, BSM + g4, :], False, True, reads=[('Utok', 3)], writes=[psk(bank)])
                        else:
                            P.mm(o, Utok[:, lt - 1, csl], cmat[:, (BPF if first else BP) + g4, :], True, False,
                                 reads=[('Utok', lt - 1), 'cmat'], writes=[psk(bank)])
                            P.mm(o, Utok[:, lt, csl], cmat[:, (BCF if first else BC) + g4, :], False, True,
                                 reads=[('Utok', lt)], writes=[psk(bank)])
                    P.copy('dve', dT[:, :, (lt - 1) * 128:lt * 128], ps[bank][:, :].rearrange("p (a b) -> p a b", a=4),
                           reads=[psk(bank)], writes=['dT'])
                for dc in range(4):
                    bank = nextbank(2, 4)
                    for cc in range(4):
                        P.mm(ps[bank][:, 0:384], Wp[:, cc, dc * 128:(dc + 1) * 128], dT[:, cc, :], cc == 0, cc == 3,
                             reads=['dT', wpk], writes=[psk(bank)])
                    fcm = 16 + g4 * 4 + dc
                    P.ts('dve', mixT[:, fcm, :], ps[bank][:, 0:384], pscT[:, g4 * 4 + dc:g4 * 4 + dc + 1], None, ALU.mult,
                         reads=[psk(bank), 'vecs'], writes=['mixT'])
            if stages < 4:
                P.barrier()
                continue

            P.barrier()
            z = big[:, :].rearrange("p (a b) -> p a b", a=32)
            h2T = R2[:, :].rearrange("p (a b) -> p a b", a=32)
            cv = Carver(S2)
            sqt = [cv.get([128, 384], F32) for _ in range(2)]
            tmpz = cv.get([128, 128], F32)
            oc0 = 3 * g * 128
            for kq in range(8):
                P.dma('sp', z[:, kq * 4:(kq + 1) * 4, :], xv[:, kq * 4:(kq + 1) * 4, c0 + 128:c0 + 512],
                      writes=[('z', kq * 4 + j) for j in range(4)], slot=('zl', kq))
                for j in range(4):
                    fc = kq * 4 + j
                    P.op('act', lambda h, fc=fc: h.mul(z[:, fc, :], z[:, fc, :], ALPHA), reads=[('z', fc)], writes=[('z', fc)])

            def z_accum(fc, src, gbase, srckey):
                zk = ('z', fc)
                P.stt('dve', z[:, fc, 0:ncp - 128], src[:, 0:ncp - 128], modP[:, gbase + fc:gbase + fc + 1], z[:, fc, 0:ncp - 128],
                      ALU.mult, ALU.add, reads=[srckey, zk, 'mod'], writes=[zk])
                if smp:
                    tv = tmpz.rearrange("p (b t) -> p b t", b=16)
                    P.tt('dve', tv, src[:, 256:384].rearrange("p (b t) -> p b t", b=16), bc_s(modS[:, gbase + fc, :]), ALU.mult,
                         reads=[srckey, 'mod'], writes=['tmpz'])
                    P.tt('dve', z[:, fc, 256:384], z[:, fc, 256:384], tmpz, ALU.add, reads=['tmpz', zk], writes=[zk])
                sq = sqt[fc % 2]
                P.actf(sq, z[:, fc, :], AF.Square, reads=[zk], writes=[('sqt', fc % 2)])
                P.mm(ps[6][:, 0:384], ones_f[:], z[:, fc, :], fc == 0, fc == 31, reads=[zk, 'ones_f'], writes=[psk(6)])
                P.mm(ps[7][:, 0:384], ones_f[:], sq, fc == 0, fc == 31, reads=[('sqt', fc % 2)], writes=[psk(7)])

            for wt in range(16):
                a, k = wload(wout_v[:, :, wt * 256:(wt + 1) * 256], [128, 32, 256])
                for j in range(2):
                    fc = wt * 2 + j
                    bank = nextbank(0, 4)
                    for kc in range(32):
                        P.mm(ps[bank][:, 0:384], a[:, kc, j * 128:(j + 1) * 128], mixT[:, kc, :], kc == 0, kc == 31,
                             reads=['mixT', k], writes=[psk(bank)])
                    z_accum(fc, ps[bank], 64, psk(bank))

            def post1(fc):
                zk = ('z', fc)
                P.actf(h2T[:, fc, 0:ncp - 128], z[:, fc, 0:ncp - 128], AF.Identity, bias=modP[:, 96 + fc:97 + fc],
                       scale=modP[:, 128 + fc:129 + fc], reads=[zk, 'mod'], writes=[('h2T', fc)])
                if smp:
                    tv = tmpz.rearrange("p (b t) -> p b t", b=16)
                    P.tt('dve', tv, z[:, fc, 256:384].rearrange("p (b t) -> p b t", b=16), bc_s(modS[:, 128 + fc, :]), ALU.mult,
                         reads=[zk, 'mod'], writes=['tmpz'])
                    P.tt('dve', h2T[:, fc, 256:384].rearrange("p (b t) -> p b t", b=16), tv, bc_s(modS[:, 96 + fc, :]), ALU.add,
                         reads=['tmpz', 'mod'], writes=[('h2T', fc)])
                P.op('act', lambda h: h.mul(z[:, fc, :], z[:, fc, :], ALPHA), reads=[zk], writes=[zk])

            def stash1(fc0):
                P.dma('sp', yv[:, fc0:fc0 + 4, oc0:oc0 + 384], z[:, fc0:fc0 + 4, :], reads=[('z', fc0 + j) for j in range(4)],
                      writes=[('yst', fc0)], slot=('yst', fc0))
            ln_block(z, ln1g, ln1b, g, post1, stash1)
            P.barrier()
            if stages < 5:
                continue

            y2T = big[:, :].rearrange("p (a b) -> p a b", a=32)
            cv = Carver(S2)
            s_sb = cv.get([128, 3, 16, 128], F32)
            tau = cv.get([128, 3, 8], F32)
            off_tmp = cv.off
            qT = cv.get([128, 16, 384], BF16)
            t16 = cv.get([128, 16, 16], F32)
            wk = cv.get([128, 256], F32)
            cand = cv.get([128, 8, 256], F32)
            b16 = cv.get([128, 8, 16], F32)
            smal = cv.get([128, 8, 8], F32)
            for wt in range(8):
                a, k = wload(wq_v[:, :, wt * 256:(wt + 1) * 256], [128, 32, 256])
                for j in range(2):
                    pc = wt * 2 + j
                    bank = nextbank(0, 4)
                    for kc in range(32):
                        P.mm(ps[bank][:, 0:384], a[:, kc, j * 128:(j + 1) * 128], h2T[:, kc, :], kc == 0, kc == 31,
                             reads=[('h2T', kc), k], writes=[psk(bank)])
                    P.copy('dve', qT[:, pc, :], ps[bank][:, 0:384], reads=[psk(bank)], writes=['qT'])
            subk, subk_key = wload(subk_d, [128, 16, 128])
            for t in range(3):
                for pc in range(16):
                    bk = 4 + pc // 4
                    P.mm(ps[bk][:, (pc % 4) * 128:(pc % 4 + 1) * 128], qT[:, pc, t * 128:(t + 1) * 128], subk[:, pc, :], True, True,
                         reads=['qT', subk_key], writes=[psk(bk)])
                for q4 in range(4):
                    P.copy('act' if q4 % 2 else 'dve', s_sb[:, t, q4 * 4:(q4 + 1) * 4, :],
                           ps[4 + q4][:, :].rearrange("p (a b) -> p a b", a=4), reads=[psk(4 + q4)], writes=['s_sb'])
                for pc in range(16):
                    P.op('dve', lambda h, pc=pc, t=t: h.max(t16[:, pc, 0:8], s_sb[:, t, pc, :]), reads=['s_sb'], writes=['t16'])
                    P.op('dve', lambda h, pc=pc, t=t: h.match_replace(wk[:, 0:128], t16[:, pc, 0:8], s_sb[:, t, pc, :], -1e30),
                         reads=['s_sb', 't16'], writes=['wk'])
                    P.op('dve', lambda h, pc=pc: h.max(t16[:, pc, 8:16], wk[:, 0:128]), reads=['wk'], writes=['t16'])
                t16v = t16.rearrange("p (h q) k -> p h q k", q=2)
                s_v = s_sb[:, t].rearrange("p (h q) k -> p h q k", q=2)

                def cand_top(tag):
                    P.tt(VAL_ENG, cand.rearrange("p h (i j) -> p h i j", i=16),
                         t16v[:, :, 0, :].unsqueeze(3).to_broadcast([128, 8, 16, 16]),
                         t16v[:, :, 1, :].unsqueeze(2).to_broadcast([128, 8, 16, 16]), ALU.add, reads=['t16'], writes=['cand'])
                    for hh in range(8):
                        P.op('dve', lambda h, hh=hh: h.max(b16[:, hh, 0:8], cand[:, hh, :]), reads=['cand'], writes=['b16'])
                        P.op('dve', lambda h, hh=hh: h.match_replace(wk[:, :], b16[:, hh, 0:8], cand[:, hh, :], -1e30),
                             reads=['cand', 'b16'], writes=['wk'])
                        P.op('dve', lambda h, hh=hh: h.max(b16[:, hh, 8:16], wk[:, :]), reads=['wk'], writes=['b16'])
                cand_top(0)
                mcol = smal[:, 0, :]
                zcol = smal[:, 1, :]
                P.copy('dve', mcol, b16[:, :, 0], reads=['b16'], writes=['smal'])
                P.tt('dve', b16[:, :, :], b16[:, :, :], mcol.unsqueeze(2).to_broadcast([128, 8, 16]), ALU.subtract,
                     reads=['b16', 'smal'], writes=['b16'])
                P.actf(b16[:, :, :], b16[:, :, :], AF.Exp, reads=['b16'], writes=['b16'])
                P.op('dve', lambda h: h.reduce_sum(zcol, b16[:, :, :], AX.X), reads=['b16'], writes=['smal'])
                P.actf(zcol, zcol, AF.Ln, reads=['smal'], writes=['smal'])
                P.tt('dve', zcol, zcol, mcol, ALU.add, reads=['smal'], writes=['smal'])
                P.tt('dve', s_v[:, :, 1, :], s_v[:, :, 1, :], zcol.unsqueeze(2).to_broadcast([128, 8, 128]), ALU.subtract,
                     reads=['s_sb', 'smal'], writes=['s_sb'])
                P.tt('dve', t16v[:, :, 1, :], t16v[:, :, 1, :], zcol.unsqueeze(2).to_broadcast([128, 8, 16]), ALU.subtract,
                     reads=['t16', 'smal'], writes=['t16'])
                cand_top(1)
                P.copy('dve', tau[:, t, :], b16[:, :, 15], reads=['b16'], writes=['tau'])
            cv.off = off_tmp
            P.barrier()
            actT = [cv.get([128, 8, 384], BF16) for _ in range(2)]
            gel = [cv.get([128, 384], F32) for _ in range(2)]
            valb = [cv.get([128, 8, 128], F32) for _ in range(2)]
            Ebf = [cv.get([128, 8, 128], BF16) for _ in range(2)]
            Gb = [cv.get([128, 8, 128], BF16) for _ in range(2)]
            Uslot = [W[0][:, :].rearrange("p (a b) -> p a b", a=32), W[1][:, :].rearrange("p (a b) -> p a b", a=32)]
            Vslot = [W[2][:, 0:4096].rearrange("p (a b) -> p a b", a=8), W[2][:, 4096:8192].rearrange("p (a b) -> p a b", a=8)]
            NCH = 128
            ucount = [0]
            pend = [None]
            finp = [None]

            def stage1(i, t, sl):
                s_v = s_sb[:, t].rearrange("p (h q) k -> p h q k", q=2)
                P.tt(VAL_ENG, valb[sl], s_v[:, :, 1, :], s_v[:, :, 0, i:i + 1].to_broadcast([128, 8, 128]), ALU.add,
                     reads=['s_sb'], writes=[('valb', sl)])
                P.actf(Ebf[sl], valb[sl], AF.Exp, reads=[('valb', sl)], writes=[('Ebf', sl)])

            def stage2(i, t, sl):
                gb = 2 + i % 2
                P.tt('dve', valb[sl], valb[sl], tau[:, t, :].unsqueeze(2).to_broadcast([128, 8, 128]), ALU.is_ge,
                     reads=[('valb', sl), 'tau'], writes=[('valb', sl)])
                P.tt('dve', Gb[sl], valb[sl], Ebf[sl], ALU.mult, reads=[('valb', sl), ('Ebf', sl)], writes=[('Gb', sl)])
                for hh in range(8):
                    P.mm(ps[gb][:, t * 128:(t + 1) * 128], Gb[sl][:, hh, :], ident, hh == 0, hh == 7,
                         reads=[('Gb', sl), 'cmat'], writes=[psk(gb)])

            def finalize(i):
                EB, ci = i // 8, i % 8
                P.tt('dve', actT[EB % 2][:, ci, :], ps[2 + i % 2][:, 0:384], gel[i % 2], ALU.mult,
                     reads=[psk(2 + i % 2), ('gel', i % 2)], writes=[('actT', EB % 2, ci)])

            def step_unit(i, t):
                sl = ucount[0] % 2
                ucount[0] += 1
                stage1(i, t, sl)
                if finp[0] is not None and t == 2:
                    finalize(finp[0])
                    finp[0] = None
                if pend[0] is not None:
                    pi, pt, psl = pend[0]
                    stage2(pi, pt, psl)
                    if pt == 2:
                        finp[0] = pi
                pend[0] = (i, t, sl)
                emit_one_add()

            def flush_units():
                if finp[0] is not None:
                    finalize(finp[0])
                    finp[0] = None
                if pend[0] is not None:
                    pi, pt, psl = pend[0]
                    stage2(pi, pt, psl)
                    if pt == 2:
                        finalize(pi)
                    pend[0] = None

            vcount = [0]
            ypend = []

            def emit_one_add():
                if not ypend:
                    return
                fc, yb, first = ypend.pop(0)
                if first:
                    P.copy('dve', y2T[:, fc, :], ps[yb][:, 0:384], reads=[psk(yb)], writes=[('z', fc)])
                else:
                    P.tt('dve', y2T[:, fc, :], y2T[:, fc, :], ps[yb][:, 0:384], ALU.add, reads=[psk(yb), ('z', fc)],
                         writes=[('z', fc)])

            def vpart(v):
                EB, fq = v // 8, v % 8
                while ypend:
                    emit_one_add()
                vs = vcount[0] % 2
                vcount[0] += 1
                va = Vslot[vs]
                P.dma('pool', va, V_v[:, EB * 8:(EB + 1) * 8, fq * 512:(fq + 1) * 512], writes=[('Wv', vs)], slot=('Wv', vs))
                for fj in range(4):
                    fc = fq * 4 + fj
                    yb = 4 + fj
                    for ci in range(8):
                        P.mm(ps[yb][:, 0:384], va[:, ci, fj * 128:(fj + 1) * 128], actT[EB % 2][:, ci, :], ci == 0, ci == 7,
                             reads=[('actT', EB % 2, ci), ('Wv', vs)], writes=[psk(yb)])
                    ypend.append((fc, yb, EB == 0))

            VLAG = 9
            abank = [0, 1]
            for i in range(NCH):
                if i % 2 == 0:
                    us = (i // 2) % 2
                    P.dma('pool', Uslot[us], UT_v[:, :, i * 128:(i + 2) * 128], writes=[('W', us)], slot=('W', us))
                us = (i // 2) % 2
                j = i % 2
                ab = nextbank(0, 2)
                for kc in range(32):
                    P.mm(ps[ab][:, 0:384], Uslot[us][:, kc, j * 128:(j + 1) * 128], h2T[:, kc, :], kc == 0, kc == 31,
                         reads=[('h2T', kc), ('W', us)], writes=[psk(ab)])
                abank[i % 2] = ab
                if i % 2 == 1:
                    for ii in (i - 1, i):
                        P.actf(gel[ii % 2], ps[abank[ii % 2]][:, 0:384], AF.Gelu, reads=[psk(abank[ii % 2])], writes=[('gel', ii % 2)])
                if i >= VLAG:
                    vpart(i - VLAG)
                for t in range(3):
                    step_unit(i, t)
            flush_units()
            for v in range(NCH - VLAG, NCH):
                vpart(v)
            while ypend:
                emit_one_add()
            P.barrier()
            cv = Carver(S2)
            sqt = [cv.get([128, 384], F32) for _ in range(2)]
            tmpz = cv.get([128, 128], F32)
            xst = [cv.get([128, 4, 384], F32) for _ in range(2)]
            for kq in range(8):
                sl = kq % 2
                P.dma('sp', xst[sl], yv[:, kq * 4:(kq + 1) * 4, oc0:oc0 + 384], reads=[('yst', kq * 4)], writes=[('xst', sl)],
                      slot=('xst', sl))
                for j in range(4):
                    fc = kq * 4 + j
                    zk = ('z', fc)
                    P.stt('dve', y2T[:, fc, 0:ncp - 128], y2T[:, fc, 0:ncp - 128], modP[:, 160 + fc:161 + fc], xst[sl][:, j, 0:ncp - 128],
                          ALU.mult, ALU.add, reads=[zk, ('xst', sl), 'mod'], writes=[zk])
                    if smp:
                        tv = tmpz.rearrange("p (b t) -> p b t", b=16)
                        P.tt('dve', tv, y2T[:, fc, 256:384].rearrange("p (b t) -> p b t", b=16), bc_s(modS[:, 160 + fc, :]), ALU.mult,
                             reads=[zk, 'mod'], writes=['tmpz'])
                        P.tt('dve', y2T[:, fc, 256:384], tmpz, xst[sl][:, j, 256:384], ALU.add, reads=['tmpz', ('xst', sl)], writes=[zk])
                    sq = sqt[fc % 2]
                    P.actf(sq, y2T[:, fc, :], AF.Square, reads=[zk], writes=[('sqt', fc % 2)])
                    P.mm(ps[6][:, 0:384], ones_f[:], y2T[:, fc, :], fc == 0, fc == 31, reads=[zk, 'ones_f'], writes=[psk(6)])
                    P.mm(ps[7][:, 0:384], ones_f[:], sq, fc == 0, fc == 31, reads=[('sqt', fc % 2)], writes=[psk(7)])

            def stash2(fc0):
                P.dma('sp', yv[:, fc0:fc0 + 4, oc0:oc0 + 384], y2T[:, fc0:fc0 + 4, :], reads=[('z', fc0 + j) for j in range(4)],
                      writes=[('yst', fc0)], slot='o_y', is_out=True)
            ln_block(y2T, ln2g, ln2b, g, None, stash2)
            P.barrier()
        P.emit()
    return nc


def _bf(x):
    return np.ascontiguousarray(x, dtype=np.float32)


def _consts(hf):
    cm = np.zeros((NMAT, 128, 128), np.float32)
    k = np.arange(128)[:, None]
    q = np.arange(128)[None, :]
    cm[MP] = (k >= q)
    cm[MC] = (k <= q)
    cm[MFP] = cm[MP] if hf == 1 else 0.0
    cm[MSN] = ((k // 8) == (q // 8)) & ((k % 8) <= (q % 8))
    cm[IDM] = np.eye(128)
    cm[MSC, :, 0:8] = (np.arange(128)[:, None] >= np.arange(8)[None, :])
    for g4, w in enumerate((2, 4, 8, 16)):
        tp = np.arange(128)[:, None]
        t = np.arange(128)[None, :]
        cur = ((tp <= t) & (tp > t - w)).astype(np.float32)
        prv = (tp - 128 > t - w).astype(np.float32)
        cm[BP + g4] = prv / w
        cm[BC + g4] = cur / w - np.eye(128)
        if hf == 1:
            cm[BPF + g4] = cm[BP + g4]
            cm[BCF + g4] = cm[BC + g4]
        else:
            cnt = np.minimum(w, t + 1).astype(np.float32)
            cm[BPF + g4] = 0.0
            cm[BCF + g4] = cur / cnt - np.eye(128)
        bq, tq = np.arange(128)[None, :] // 8, np.arange(128)[None, :] % 8
        r_b, r_r = np.arange(120)[:, None] // 15, np.arange(120)[:, None] % 15
        for half, idx in ((0, BSA), (1, BSB)):
            m = ((r_b + 8 * half) == bq) & (r_r > 15 + tq - w)
            cm[idx + g4, 0:120] = m / w
        bk, tk = np.arange(128)[:, None] // 8, np.arange(128)[:, None] % 8
        m = (bk == bq) & (tk <= tq) & (tk > tq - w)
        cm[BSM + g4] = m / w - np.eye(128)
    return np.ascontiguousarray(cm.transpose(1, 0, 2))


def _rope_tab(hf):
    half = 8
    inv = (np.float32(500000.0) ** (-np.arange(half, dtype=np.float32) / np.float32(half))).astype(np.float32)
    tab = np.zeros((128, 10, 2, 8), np.float32)
    for gt in range(10):
        if gt < 9:
            pos = hf * 1024 + (gt - 1) * 128 + np.arange(128)
        else:
            pos = 8192 + (np.arange(128) % 8)
        ang = pos.astype(np.float32)[:, None] * inv[None, :]
        tab[:, gt, 0, :] = np.cos(ang)
        tab[:, gt, 1, :] = np.sin(ang)
    return tab


_NC_CACHE = {}


def _prep(x_prompt, x_sample, cache_k, cache_v, state_pool, c_prompt, c_sample, w_ada, b_ada, w_in,
          sinks, pool_w, pool_scale, w_out, ln1_g, ln1_b, peer_wq, peer_subkeys, peer_u, peer_v,
          ln2_g, ln2_b):
    f = lambda a: np.asarray(a, dtype=np.float32)
    x_prompt, x_sample, cache_k, cache_v, state_pool = map(f, (x_prompt, x_sample, cache_k, cache_v, state_pool))
    c_prompt, c_sample = f(c_prompt), f(c_sample)
    wi = f(w_in)[0]
    kcol = wi[:, 2048:2304].reshape(D, 4, 1, 64)
    vcol = wi[:, 2304:2560].reshape(D, 4, 1, 64)
    win_r = np.concatenate([np.broadcast_to(kcol, (D, 4, 2, 64)).reshape(D, 512),
                            np.broadcast_to(vcol, (D, 4, 2, 64)).reshape(D, 512),
                            wi[:, 0:2048], wi[:, 2560:4608]], axis=1)
    vecs = np.zeros((128, 384), np.float32)
    vecs[:, 0:192] = f(b_ada)[0].reshape(192, 128).T
    vecs[:, 192:208] = f(pool_scale)[0].reshape(16, 128).T
    for i, a in enumerate((ln1_g, ln1_b, ln2_g, ln2_b)):
        vecs[:, 208 + 32 * i:240 + 32 * i] = f(a)[0].reshape(32, 128).T
    vecs[:, 336:368] = np.broadcast_to(f(sinks)[0].reshape(16, 2).T.reshape(1, 32), (128, 32))
    common = {
        "w_ada": _bf(f(w_ada)[0]), "w_in": _bf(win_r), "w_out": _bf(f(w_out)[0]), "wq": _bf(f(peer_wq)[0]),
        "subk": _bf(f(peer_subkeys)[0].transpose(3, 0, 1, 2).reshape(128, 16, 128)),
        "UT": _bf(f(peer_u)[0].T), "V": _bf(f(peer_v)[0]),
        "poolw": _bf(f(pool_w)[0].reshape(4, 4, 128, 512).transpose(2, 0, 1, 3)),
        "vecs": vecs,
    }
    in_maps = []
    for r in range(8):
        b, hf = r // 2, r % 2
        xs = x_sample[16 * r:16 * r + 16].reshape(128, D)
        t0 = hf * 1024
        halo = x_prompt[b, t0 - 128:t0] if hf == 1 else np.zeros((128, D), np.float32)
        xT = np.concatenate([halo, x_prompt[b, t0:t0 + 1024], xs], axis=0).T
        cc = np.concatenate([c_prompt[b:b + 1], c_sample[16 * r:16 * r + 16]], axis=0)
        ck = cache_k[0, 16 * r:16 * r + 16]
        cvv = cache_v[0, 16 * r:16 * r + 16]
        ckt = ck.transpose(3, 0, 2, 1)
        cvt = cvv.transpose(1, 0, 2, 3)
        sp = state_pool[0, 16 * r:16 * r + 16]
        m = dict(common)
        m.update({
            "xT": _bf(xT),
            "cT": _bf(cc.T.reshape(32, 128, 17).transpose(1, 0, 2)),
            "cmat": _consts(hf), "rope": _rope_tab(hf),
            "cKT": _bf(np.concatenate([ckt, ckt], axis=0)),
            "cV": _bf(np.concatenate([cvt, cvt], axis=3)),
            "ckn": _bf(ck.reshape(16, 128, 256)), "cvn": _bf(cvv.reshape(16, 128, 256)),
            "sptm": _bf(sp.reshape(2, 120, 2048).transpose(1, 0, 2)), "spn": _bf(sp),
        })
        in_maps.append(m)
    return in_maps


def _assemble(res, cores=tuple(range(8))):
    y_p = np.zeros((4, 2048, D), np.float32)
    y_s = np.zeros((128, 8, D), np.float32)
    nkp = np.zeros((1, 4, 128, 4, 64), np.float32)
    nvp = np.zeros_like(nkp)
    npp = np.zeros((1, 4, 15, 2048), np.float32)
    nks = np.zeros((1, 128, 128, 4, 64), np.float32)
    nvs = np.zeros_like(nks)
    nps = np.zeros((1, 128, 15, 2048), np.float32)
    for r in cores:
        b, hf = r // 2, r % 2
        o = res[cores.index(r)]
        yT = o["yT"]
        y_p[b, hf * 1024:(hf + 1) * 1024] = yT[:, 0:1024].T
        y_s[16 * r:16 * r + 16] = yT[:, 1024:1152].T.reshape(16, 8, D)
        if hf == 1:
            nkp[0, b] = o["nkp"].reshape(128, 4, 64)
            nvp[0, b] = o["nvp"].reshape(128, 4, 64)
            npp[0, b] = o["npp"]
        nks[0, 16 * r:16 * r + 16] = o["nks"].reshape(16, 128, 4, 64)
        nvs[0, 16 * r:16 * r + 16] = o["nvs"].reshape(16, 128, 4, 64)
        nps[0, 16 * r:16 * r + 16] = o["nps"]
    return (y_p, y_s, nkp, nvp, npp, nks, nvs, nps)


def kernel(**inputs):
    in_maps = _prep(**inputs)
    if 'nc' not in _NC_CACHE:
        _NC_CACHE['nc'] = build_nc()
    res = run_bass_kernel_spmd(_NC_CACHE['nc'], in_maps, core_ids=list(range(8))).results
    return _assemble(res)
```

```python
import numpy as np
from contextlib import ExitStack
import concourse.bass as bass
import concourse.mybir as mybir
from concourse.bass_utils import run_bass_kernel_spmd

F32 = mybir.dt.float32
BF16 = mybir.dt.bfloat16
ALU = mybir.AluOpType
AF = mybir.ActivationFunctionType
AX = mybir.AxisListType
ENG = ('pe', 'act', 'dve', 'pool', 'sp')


class Prog:
    def __init__(self, nc, es):
        self.nc = nc
        self.es = es
        self.ops = {e: [] for e in ENG}
        self.n = {e: 0 for e in ENG}
        self.seen = {e: {} for e in ENG}
        self.res = {}
        self.dcount = {}
        self.targets = {e: set() for e in ENG}
        self.out_slots = set()

    def _deps(self, reads, writes):
        d = []
        for k in reads:
            r = self.res.get(k)
            if r and r[0] is not None:
                d.append(r[0])
        for k in writes:
            r = self.res.get(k)
            if r:
                if r[0] is not None:
                    d.append(r[0])
                for sk, v in r[1].items():
                    d.append((sk[0], sk[1], v))
        return d

    def _waits(self, eng, deps, skip_same=False):
        for kind, key, val in deps:
            if kind == 'e' and key == eng and skip_same:
                continue
            if kind == 'd':
                val = self.dcount[key]
            sk = (kind, key)
            if self.seen[eng].get(sk, -1) >= val:
                continue
            self.seen[eng][sk] = val
            self.ops[eng].append(('w', kind, key, val))
            if kind == 'e':
                self.targets[key].add(val)

    def _update(self, me, reads, writes):
        for k in writes:
            self.res[k] = [me, {}]
        for k in reads:
            r = self.res.setdefault(k, [None, {}])
            sk = (me[0], me[1])
            if r[1].get(sk, -1) < me[2]:
                r[1][sk] = me[2]

    def op(self, eng, fn, reads=(), writes=(), skip_same=False):
        self._waits(eng, self._deps(reads, writes), skip_same)
        idx = self.n[eng]
        self.n[eng] += 1
        self.ops[eng].append(('i', fn, idx))
        self._update(('e', eng, idx), reads, writes)

    def dma(self, eng, out, in_, reads=(), writes=(), slot=None, is_out=False):
        self._waits(eng, self._deps(reads, writes))
        c = self.dcount.get(slot, 0) + 16
        self.dcount[slot] = c
        self.ops[eng].append(('d', out, in_, slot))
        self._update(('d', slot, c), reads, writes)
        if is_out:
            self.out_slots.add(slot)

    def barrier(self):
        for e in ENG:
            deps = []
            for f in ENG:
                if f != e and self.n[f] > 0:
                    deps.append(('e', f, self.n[f] - 1))
            for s, c in self.dcount.items():
                deps.append(('d', s, c))
            self._waits(e, deps)

    def mm(self, out, lhsT, rhs, start, stop, reads=(), writes=()):
        self.op('pe', lambda h: h.matmul(out, lhsT, rhs, start=start, stop=stop), reads, writes, skip_same=True)

    def tr(self, out, in_, ident, reads=(), writes=()):
        self.op('pe', lambda h: h.transpose(out, in_, ident), reads, writes, skip_same=True)

    def actf(self, out, in_, func, bias=None, scale=None, reads=(), writes=(), eng='act'):
        kw = {}
        if bias is not None:
            kw['bias'] = bias
        if scale is not None:
            kw['scale'] = scale
        self.op(eng, lambda h: h.activation(out, in_, func, **kw), reads, writes)

    def tt(self, eng, out, in0, in1, op, reads=(), writes=()):
        self.op(eng, lambda h: h.tensor_tensor(out, in0, in1, op), reads, writes)

    def ts(self, eng, out, in0, s1, s2, op0, op1=None, reads=(), writes=()):
        if op1 is None:
            self.op(eng, lambda h: h.tensor_scalar(out, in0, s1, None, op0), reads, writes)
        else:
            self.op(eng, lambda h: h.tensor_scalar(out, in0, s1, s2, op0, op1), reads, writes)

    def stt(self, eng, out, in0, scalar, in1, op0, op1, reads=(), writes=()):
        self.op(eng, lambda h: h.scalar_tensor_tensor(out, in0, scalar, in1, op0, op1), reads, writes)

    def copy(self, eng, out, in_, reads=(), writes=()):
        if eng == 'act':
            self.op(eng, lambda h: h.copy(out, in_), reads, writes)
        else:
            self.op(eng, lambda h: h.tensor_copy(out, in_), reads, writes)

    def emit(self):
        nc = self.nc
        es = self.es
        sem = {e: es.enter_context(nc.semaphore("sem_" + e)) for e in ENG}
        dsem = {}
        for i, s in enumerate(self.dcount):
            dsem[s] = es.enter_context(nc.semaphore("dsem%d" % i))
        rank = {e: {idx: i + 1 for i, idx in enumerate(sorted(self.targets[e]))} for e in ENG}
        for s in sorted(self.out_slots, key=str):
            self.ops['sp'].append(('w', 'd', s, self.dcount[s]))
        block = es.enter_context(nc.Block())

        def run(e, h):
            for rec in self.ops[e]:
                if rec[0] == 'w':
                    _, kind, key, val = rec
                    if kind == 'e':
                        h.wait_ge(sem[key], rank[key][val])
                    else:
                        h.wait_ge(dsem[key], val)
                elif rec[0] == 'i':
                    ins = rec[1](h)
                    if rec[2] in rank[e]:
                        ins.then_inc(sem[e], 1)
                else:
                    _, out, in_, slot = rec
                    h.dma_start(out=out, in_=in_).then_inc(dsem[slot], 16)

        block.tensor(lambda h: run('pe', h))
        block.scalar(lambda h: run('act', h))
        block.vector(lambda h: run('dve', h))
        block.gpsimd(lambda h: run('pool', h))
        block.sync(lambda h: run('sp', h))
        print("PROG ops:", {e: len(self.ops[e]) for e in ENG}, "dsems", len(dsem))


CUT = 99
D = 4096
ALPHA = 2.0 ** 0.25
LN_EPS = 1e-5
MP, MC, MFP, MSN, IDM, BP, BC, BPF, BCF, BSA, BSB, BSM, MSC = 0, 1, 2, 3, 4, 5, 9, 13, 17, 21, 25, 29, 33
NMAT = 34
STAGES = 99
VAL_ENG = 'act'


def build_nc(stages=STAGES, groups=(0, 1, 2), skipA=False):
    nc = bass.Bass("TRN2", target_bir_lowering=False)
    dt_in = lambda n, s: nc.dram_tensor(n, s, F32, kind="ExternalInput").ap()
    dt_out = lambda n, s: nc.dram_tensor(n, s, F32, kind="ExternalOutput").ap()
    xT_d = dt_in("xT", [D, 1280])
    cT_d = dt_in("cT", [128, 32, 17])
    cmat_d = dt_in("cmat", [128, NMAT, 128])
    rope_d = dt_in("rope", [128, 10, 2, 8])
    vecs_d = dt_in("vecs", [128, 384])
    cKT_d = dt_in("cKT", [128, 16, 4, 128])
    cV_d = dt_in("cV", [128, 16, 4, 128])
    ckn_d = dt_in("ckn", [16, 128, 256])
    cvn_d = dt_in("cvn", [16, 128, 256])
    sptm_d = dt_in("sptm", [120, 2, 2048])
    spn_d = dt_in("spn", [16, 15, 2048])
    wada_d = dt_in("w_ada", [D, 24576] if not skipA else [128, 128])
    win_d = dt_in("w_in", [D, 5120])
    wout_d = dt_in("w_out", [D, D] if stages >= 4 else [128, 128])
    wq_d = dt_in("wq", [D, 2048] if stages >= 5 else [128, 128])
    subk_d = dt_in("subk", [128, 16, 128])
    UT_d = dt_in("UT", [D, 16384] if stages >= 5 else [128, 128])
    V_d = dt_in("V", [16384, D] if stages >= 5 else [128, 128])
    poolw_d = dt_in("poolw", [128, 4, 4, 512])
    yT_d = dt_out("yT", [D, 1152])
    nkp_d = dt_out("nkp", [128, 256])
    nvp_d = dt_out("nvp", [128, 256])
    npp_d = dt_out("npp", [15, 2048])
    nks_d = dt_out("nks", [16, 128, 256])
    nvs_d = dt_out("nvs", [16, 128, 256])
    nps_d = dt_out("nps", [16, 15, 2048])

    es = ExitStack()
    with es:
        P = Prog(nc, es)
        sb = lambda name, shape, dt: es.enter_context(nc.sbuf_tensor("s_" + name, shape, dt))
        ps = [es.enter_context(nc.psum_tensor("ps%d" % i, [128, 512], F32)) for i in range(8)]
        psk = lambda i: ('ps', i)

        cmat = sb("cmat", [128, NMAT, 128], BF16)
        rope = sb("rope", [128, 10, 2, 8], F32)
        vecs = sb("vecs", [128, 384], F32)
        modP = sb("modP", [128, 192], F32)
        modS = sb("modS", [128, 192, 16], F32)
        cT = sb("cTs", [128, 32, 17], F32)
        siluT = sb("siluT", [128, 32, 17], BF16)
        sinkE = sb("sinkE", [128, 32], F32)
        ones_b = sb("ones_b", [128, 128], BF16)
        ones_f = sb("ones_f", [128, 128], F32)
        W = [sb("W%d" % i, [128, 8192], BF16) for i in range(3)]
        R2 = sb("R2", [128, 12288], BF16)
        big = sb("big", [128, 12288], F32)
        S2 = sb("S2", [128, 14848], F32)

        b_adaT = vecs[:, 0:192]
        pscT = vecs[:, 192:208]
        ln1g, ln1b, ln2g, ln2b = (vecs[:, 208 + 32 * i:240 + 32 * i] for i in range(4))
        ident = cmat[:, IDM, :]

        class Carver:
            def __init__(self, t):
                self.t = t
                self.off = 0

            def get(self, shape, dt):
                n = int(np.prod(shape[1:]))
                nb = n * (2 if dt == BF16 else 4)
                nw = (nb + 3) // 4
                a = self.t[:, self.off:self.off + nw]
                self.off += nw
                assert self.off <= 14848, self.off
                if dt == BF16:
                    a = a.bitcast(BF16)[:, 0:n]
                if len(shape) == 3:
                    a = a.rearrange("p (a b) -> p a b", a=shape[1])
                elif len(shape) == 4:
                    a = a.rearrange("p (a b c) -> p a b c", a=shape[1], b=shape[2])
                return a

        wslot = [0]

        def wload(src_ap, view_shape, key_extra=None):
            s = wslot[0] % 3
            wslot[0] += 1
            n = int(np.prod(view_shape[1:]))
            a = W[s][0:view_shape[0], 0:n]
            if len(view_shape) == 3:
                a = a.rearrange("p (a b) -> p a b", a=view_shape[1])
            P.dma('pool', a, src_ap, writes=[('W', s)], slot=('W', s))
            return a, ('W', s)

        P.dma('pool', cmat[:], cmat_d, writes=['cmat'], slot='c_cmat')
        P.dma('sp', rope[:], rope_d, writes=['rope'], slot='c_rope')
        P.dma('sp', vecs[:], vecs_d, writes=['vecs'], slot='c_vecs')
        P.dma('sp', cT[:], cT_d, writes=['cT'], slot='c_cT')
        P.op('dve', lambda h: h.memset(ones_b[:], 1.0), writes=['ones_b'])
        P.op('dve', lambda h: h.memset(ones_f[:], 1.0), writes=['ones_f'])
        P.actf(siluT[:], cT[:], AF.Silu, reads=['cT'], writes=['siluT'])
        P.actf(sinkE[:], vecs[:, 336:368], AF.Exp, reads=['vecs'], writes=['sinkE'])
        P.dma('sp', nks_d[:, 0:120, :], ckn_d[:, 8:128, :], slot='o_misc', is_out=True)
        P.dma('sp', nvs_d[:, 0:120, :], cvn_d[:, 8:128, :], slot='o_misc', is_out=True)
        P.dma('sp', nps_d[:, 0:7, :], spn_d[:, 8:15, :], slot='o_misc', is_out=True)

        wada_v = wada_d.rearrange("(kc p) c -> p kc c", p=128)
        if skipA:
            P.op('dve', lambda h: h.memset(modP[:], 0.25), writes=['mod'])
            P.op('dve', lambda h: h.memset(modS[:], 0.25), writes=['mod'])
        WA = [big[:, 0:8192].bitcast(BF16).rearrange("p (a b) -> p a b", a=32),
              S2[:, 0:8192].bitcast(BF16).rearrange("p (a b) -> p a b", a=32)]
        for cg in range(0 if skipA else 48):
            s = cg % 2
            P.dma('pool', WA[s], wada_v[:, :, cg * 512:(cg + 1) * 512], writes=[('WA', s)], slot=('WA', s))
            bank = cg % 2
            pv = ps[bank][:, 0:68].rearrange("p (j n) -> p j n", j=4)
            for j in range(4):
                for kc in range(32):
                    P.mm(pv[:, j, :], WA[s][:, kc, j * 128:(j + 1) * 128], siluT[:, kc, :], kc == 0, kc == 31,
                         reads=[('WA', s), 'siluT'], writes=[psk(bank)])
            m0 = cg * 4
            P.tt('dve', modP[:, m0:m0 + 4].unsqueeze(2), pv[:, :, 0:1], b_adaT[:, m0:m0 + 4].unsqueeze(2), ALU.add,
                 reads=[psk(bank), 'vecs'], writes=['mod'])
            P.tt('dve', modS[:, m0:m0 + 4, :], pv[:, :, 1:17],
                 b_adaT[:, m0:m0 + 4].unsqueeze(2).to_broadcast([128, 4, 16]), ALU.add,
                 reads=[psk(bank), 'vecs'], writes=['mod'])
        for m in (1, 4):
            P.ts('dve', modP[:, m * 32:(m + 1) * 32], modP[:, m * 32:(m + 1) * 32], 1.0, None, ALU.add,
                 reads=['mod'], writes=['mod'])
            P.ts('dve', modS[:, m * 32:(m + 1) * 32, :], modS[:, m * 32:(m + 1) * 32, :], 1.0, None, ALU.add,
                 reads=['mod'], writes=['mod'])
        P.barrier()

        xv = xT_d.rearrange("(kc p) t -> p kc t", p=128)
        yv = yT_d.rearrange("(kc p) t -> p kc t", p=128)
        win_v = win_d.rearrange("(kc p) c -> p kc c", p=128)
        wout_v = wout_d.rearrange("(kc p) c -> p kc c", p=128)
        wq_v = wq_d.rearrange("(kc p) c -> p kc c", p=128)
        UT_v = UT_d.rearrange("(kc p) c -> p kc c", p=128)
        V_v = V_d.rearrange("(c p) f -> p c f", p=128)

        def bc_s(ap_b16, n=8):
            return ap_b16.unsqueeze(2).to_broadcast([ap_b16.shape[0], 16, n])

        pbank = [0]

        def nextbank(lo, hi):
            b = lo + pbank[0] % (hi - lo)
            pbank[0] += 1
            return b

        def ln_block(z, gcol, bcol, g, mod_post, stash):
            cv = Carver(S2)
            cv.off = 11000
            mean = cv.get([128, 384], F32)
            rstd = cv.get([128, 384], F32)
            tmpn = cv.get([128, 384], F32)
            P.op('act', lambda h: h.mul(mean, ps[6][:, 0:384], 1.0 / D), reads=[psk(6)], writes=['mean'])
            P.op('act', lambda h: h.mul(rstd, ps[7][:, 0:384], 1.0 / D), reads=[psk(7)], writes=['rstd'])
            P.tt('dve', tmpn, mean, mean, ALU.mult, reads=['mean'], writes=['tmpn'])
            P.tt('dve', rstd, rstd, tmpn, ALU.subtract, reads=['rstd', 'tmpn'], writes=['rstd'])
            P.ts('dve', rstd, rstd, LN_EPS, None, ALU.add, reads=['rstd'], writes=['rstd'])
            P.actf(rstd, rstd, AF.Sqrt, reads=['rstd'], writes=['rstd'])
            P.op('dve', lambda h: h.reciprocal(rstd, rstd), reads=['rstd'], writes=['rstd'])
            for fc in range(32):
                zk = ('z', fc)
                P.tt('dve', z[:, fc, :], z[:, fc, :], mean, ALU.subtract, reads=[zk, 'mean'], writes=[zk])
                P.tt('dve', z[:, fc, :], z[:, fc, :], rstd, ALU.mult, reads=[zk, 'rstd'], writes=[zk])
                P.ts('dve', z[:, fc, :], z[:, fc, :], gcol[:, fc:fc + 1], bcol[:, fc:fc + 1], ALU.mult, ALU.add,
                     reads=[zk, 'vecs'], writes=[zk])
                if mod_post is not None:
                    mod_post(fc)
                if stash is not None and fc % 4 == 3:
                    stash(fc - 3)

        for g in (groups if stages >= 2 else ()):
            c0 = 3 * g * 128
            smp = (g == 2)
            ncp = 384 if smp else 512
            own = [1, 2] if smp else [1, 2, 3]
            tiles_all = [0, 1, 2, 3]
            hT = big[:, 0:8192].bitcast(BF16).rearrange("p (a b) -> p a b", a=32)
            xs = big[:, 8192:12288].rearrange("p (s j t) -> p s j t", s=2, j=4)
            mixT = R2[:, :].rearrange("p (a b) -> p a b", a=32)
            cv = Carver(S2)
            KT = cv.get([128, 4, 512], BF16)
            Vtok = cv.get([128, 4, 512], BF16)
            stf = [cv.get([128, 512], F32) for _ in range(2)]
            stb = [cv.get([128, 512], BF16) for _ in range(2)]
            rt = cv.get([128, 4, 64], F32)
            QTz = cv.get([128, 2, 4, 384], BF16)
            Eb = cv.get([128, 2048], BF16)
            Ef = cv.get([128, 2048], F32)
            Rr = cv.get([128, 2, 512], F32)
            cK = cv.get([128, 16, 128], BF16)
            cVt = cv.get([128, 16, 128], BF16)
            Ec = cv.get([128, 1024], BF16)
            tmpS = cv.get([128, 128], F32)
            off_pool = cv.off
            P.op('dve', lambda h: h.memset(QTz, 0.0), writes=['QT'])
            for kq in range(8):
                sl = kq % 2
                P.dma('sp', xs[:, sl], xv[:, kq * 4:(kq + 1) * 4, c0:c0 + 512], writes=[('xs', sl)], slot=('xs', sl))
                for j in range(4):
                    kc = kq * 4 + j
                    if kc % 2 == 0:
                        P.ts('dve', hT[:, kc, 0:ncp], xs[:, sl, j, 0:ncp], modP[:, 32 + kc:33 + kc], modP[:, kc:kc + 1],
                             ALU.mult, ALU.add, reads=[('xs', sl), 'mod'], writes=[('hT', kc)])
                    else:
                        P.actf(hT[:, kc, 0:ncp], xs[:, sl, j, 0:ncp], AF.Identity, bias=modP[:, kc:kc + 1],
                               scale=modP[:, 32 + kc:33 + kc], reads=[('xs', sl), 'mod'], writes=[('hT', kc)])
                    if smp:
                        tv = tmpS.rearrange("p (b t) -> p b t", b=16)
                        P.tt('dve', tv, xs[:, sl, j, 384:512].rearrange("p (b t) -> p b t", b=16),
                             bc_s(modS[:, 32 + kc, :]), ALU.mult, reads=[('xs', sl), 'mod'], writes=['tmpS'])
                        P.tt('dve', hT[:, kc, 384:512].rearrange("p (b t) -> p b t", b=16), tv,
                             bc_s(modS[:, kc, :]), ALU.add, reads=['tmpS', 'mod'], writes=[('hT', kc)])
            if CUT == 1:
                P.barrier()
                continue

            def rope_inplace(f, gt):
                f3 = f.rearrange("p (h d) -> p h d", h=8)
                x1 = f3[:, :, 0:8]
                x2 = f3[:, :, 8:16]
                cs = rope[:, gt, 0, :].unsqueeze(1).to_broadcast([128, 8, 8])
                sn = rope[:, gt, 1, :].unsqueeze(1).to_broadcast([128, 8, 8])
                r = [rt[:, i, :].rearrange("p (h d) -> p h d", h=8) for i in range(4)]
                P.tt('dve', r[0], x1, cs, ALU.mult, reads=['stf', 'rope'], writes=['rt'])
                P.tt('dve', r[1], x2, sn, ALU.mult, reads=['stf', 'rope'], writes=['rt'])
                P.tt('dve', r[2], x2, cs, ALU.mult, reads=['stf', 'rope'], writes=['rt'])
                P.tt('dve', r[3], x1, sn, ALU.mult, reads=['stf', 'rope'], writes=['rt'])
                P.tt('dve', x1, r[0], r[1], ALU.subtract, reads=['rt'], writes=['stf'])
                P.tt('dve', x2, r[2], r[3], ALU.add, reads=['rt'], writes=['stf'])

            def inproj(cg, lts, evac):
                tl = []
                for half in range(2):
                    a, k = wload(win_v[:, :, cg * 512 + half * 256: cg * 512 + half * 256 + 256], [128, 32, 256])
                    tl.append((a, k))
                for lt in lts:
                    bank = nextbank(0, 4)
                    for half in range(2):
                        a, k = tl[half]
                        for kc in range(32):
                            P.mm(ps[bank][:, half * 256:(half + 1) * 256], hT[:, kc, lt * 128:(lt + 1) * 128], a[:, kc, :],
                                 kc == 0, kc == 31, reads=[('hT', kc), k], writes=[psk(bank)])
                    if CUT >= 3:
                        evac(lt, bank)

            def out_rows(dst_p, dst_s, src, lt, view=None):
                if not smp:
                    return
                if lt == 2 and dst_p is not None:
                    P.dma('sp', dst_p, src if view is None else view(src), reads=['stf'], slot='o_misc', is_out=True)
                if lt == 3:
                    for b in range(16):
                        s_ = src[b * 8:(b + 1) * 8]
                        P.dma('sp', dst_s(b), s_ if view is None else view(s_), reads=['stf'], slot='o_misc', is_out=True)

            kview = lambda a: a.rearrange("p (k c d) -> p k c d", k=4, c=2)[:, :, 0, :]

            def evac_k(lt, bank):
                gt = 3 * g + lt
                f = stf[0]
                P.copy('act', f, ps[bank][:, :], reads=[psk(bank)], writes=['stf'])
                if CUT == 3:
                    return
                rope_inplace(f, gt)
                P.copy('act', stb[0], f, reads=['stf'], writes=['stb'])
                if CUT == 4:
                    return
                pb = ps[4 + lt % 2][:].bitcast(BF16)
                for kv in range(4):
                    P.tr(pb[:, kv * 128:(kv + 1) * 128], stb[0][:, kv * 128:(kv + 1) * 128], ident,
                         reads=['stb', 'cmat'], writes=[psk(4 + lt % 2)])
                P.copy('dve', KT[:, :, lt * 128:(lt + 1) * 128], pb[:, 0:512].rearrange("p (k t) -> p k t", k=4),
                       reads=[psk(4 + lt % 2)], writes=['KT'])
                out_rows(nkp_d.rearrange("p (k d) -> p k d", k=4),
                         lambda b: nks_d[b, 120:128, :].rearrange("p (k d) -> p k d", k=4), f, lt, kview)
            inproj(0, tiles_all, evac_k)
            if CUT <= 5:
                P.barrier()
                continue

            def evac_v(lt, bank):
                P.copy('dve', Vtok[:, lt, :], ps[bank][:, :], reads=[psk(bank)], writes=[('Vtok', lt)])
                if smp and lt >= 2:
                    f = stf[0]
                    P.copy('dve', f, ps[bank][:, :], reads=[psk(bank)], writes=['stf'])
                    out_rows(nvp_d.rearrange("p (k d) -> p k d", k=4),
                             lambda b: nvs_d[b, 120:128, :].rearrange("p (k d) -> p k d", k=4), f, lt, kview)
            inproj(1, tiles_all, evac_v)
            if CUT == 6:
                P.barrier()
                continue

            for kvh in range(4 if stages >= 3 else 0):
                def evac_q(lt, bank):
                    gt = 3 * g + lt
                    f = stf[1]
                    P.copy('act', f, ps[bank][:, :], reads=[psk(bank)], writes=['stf'])
                    rope_inplace(f, gt)
                    P.copy('act', stb[1], f, reads=['stf'], writes=['stb'])
                    pb = ps[4 + lt % 2][:].bitcast(BF16)
                    for pc in range(4):
                        P.tr(pb[:, pc * 128:(pc + 1) * 128], stb[1][:, pc * 128:(pc + 1) * 128], ident,
                             reads=['stb', 'cmat'], writes=[psk(4 + lt % 2)])
                    for c in range(2):
                        hs = slice(c * 64, (c + 1) * 64)
                        P.copy('dve', QTz[hs, c, :, (lt - 1) * 128:lt * 128], pb[hs, 0:512].rearrange("p (k t) -> p k t", k=4),
                               reads=[psk(4 + lt % 2)], writes=['QT'])
                inproj(2 + kvh, [1, 2, 3], evac_q)
                if smp:
                    P.dma('pool', cK, cKT_d[:, :, kvh, :], writes=['cK'], slot='cK')
                    P.dma('pool', cVt, cV_d[:, :, kvh, :], writes=['cV'], slot='cV')
                for lt in [1, 2, 3]:
                    if CUT == 7:
                        break
                    qc = (lt - 1) * 128
                    is_s = smp and lt == 3
                    vsl = slice(kvh * 128, (kvh + 1) * 128)
                    if not is_s:
                        for kt, ktile in enumerate([lt - 1, lt]):
                            for c in range(2):
                                bank = kt * 2 + c
                                hs = slice(c * 64, (c + 1) * 64)
                                P.mm(ps[bank][:, :].rearrange("p (a b) -> p a b", a=4),
                                     KT[:, kvh, ktile * 128:(ktile + 1) * 128], QTz[:, c, :, qc:qc + 128], True, True,
                                     reads=['KT', 'QT'], writes=[psk(bank)])
                                if CUT == 8:
                                    continue
                                P.actf(Ef[:, bank * 512:(bank + 1) * 512], ps[bank][:, :], AF.Exp, scale=0.125,
                                       reads=[psk(bank)], writes=[('Ef', bank)])
                            if CUT in (8, 9):
                                continue
                            midx = MC if kt == 1 else (MFP if (g == 0 and lt == 1) else MP)
                            ev = Eb[:, kt * 1024:(kt + 1) * 1024].rearrange("p (a b) -> p a b", a=8)
                            efv = Ef[:, kt * 1024:(kt + 1) * 1024].rearrange("p (a b) -> p a b", a=8)
                            P.tt('dve', ev, efv, cmat[:, midx, :].unsqueeze(1).to_broadcast([128, 8, 128]), ALU.mult,
                                 reads=[('Ef', kt * 2), ('Ef', kt * 2 + 1), 'cmat'], writes=[('Eb', kt * 2), ('Eb', kt * 2 + 1)])
                        for c in range(2):
                            if CUT in (8, 9, 10):
                                continue
                            P.mm(ps[6 + c][:, :], ones_b[:], Eb[:, c * 512:(c + 1) * 512], True, False,
                                 reads=[('Eb', c), 'ones_b'], writes=[psk(6 + c)])
                            P.mm(ps[6 + c][:, :], ones_b[:], Eb[:, (2 + c) * 512:(3 + c) * 512], False, True,
                                 reads=[('Eb', 2 + c)], writes=[psk(6 + c)])
                            P.mm(ps[4 + c][:, :], Vtok[:, lt - 1, vsl], Eb[:, c * 512:(c + 1) * 512], True, False,
                                 reads=[('Eb', c), ('Vtok', lt - 1 if not is_s else 3)], writes=[psk(4 + c)])
                            P.mm(ps[4 + c][:, :], Vtok[:, lt, vsl], Eb[:, (2 + c) * 512:(3 + c) * 512], False, True,
                                 reads=[('Eb', 2 + c), ('Vtok', lt)], writes=[psk(4 + c)])
                    else:
                        for c in range(2):
                            hs = slice(c * 64, (c + 1) * 64)
                            P.mm(ps[c][:, :].rearrange("p (a b) -> p a b", a=4), KT[:, kvh, 384:512], QTz[:, c, :, qc:qc + 128],
                                 True, True, reads=['KT', 'QT'], writes=[psk(c)])
                            P.actf(Ef[:, c * 512:(c + 1) * 512], ps[c][:, :], AF.Exp, scale=0.125,
                                   reads=[psk(c)], writes=[('Ef', c)])
                        ev = Eb[:, 0:1024].rearrange("p (a b) -> p a b", a=8)
                        efv = Ef[:, 0:1024].rearrange("p (a b) -> p a b", a=8)
                        P.tt('dve', ev, efv, cmat[:, MSN, :].unsqueeze(1).to_broadcast([128, 8, 128]), ALU.mult,
                             reads=[('Ef', 0), ('Ef', 1), 'cmat'], writes=[('Eb', 0), ('Eb', 1)])
                        for b in range(16):
                            for c in range(2):
                                hs = slice(c * 64, (c + 1) * 64)
                                off = ((b % 8) * 2 + c) * 32
                                P.mm(ps[2 + b // 8][:, off:off + 32].rearrange("p (a t) -> p a t", a=4), cK[:, b, :],
                                     QTz[:, c, :, qc + b * 8:qc + b * 8 + 8], True, True,
                                     reads=['cK', 'QT'], writes=[psk(2 + b // 8)])
                        for hb in range(2):
                            P.actf(Ef[:, 1024 + hb * 512:1024 + (hb + 1) * 512], ps[2 + hb][:, :], AF.Exp, scale=0.125,
                                   reads=[psk(2 + hb)], writes=[('Ef', 2 + hb)])
                        ecv = Ec.rearrange("p (a t) -> p a t", t=8)
                        efv = Ef[:, 1024:2048].rearrange("p (a t) -> p a t", t=8)
                        P.tt('dve', ecv, efv, cmat[:, MSC, 0:8].unsqueeze(1).to_broadcast([128, 128, 8]), ALU.mult,
                             reads=[('Ef', 2), ('Ef', 3), 'cmat'], writes=['Ec'])
                        Ec4 = Ec.rearrange("p (b c a t) -> p b c a t", b=16, c=2, a=4)
                        for c in range(2):
                            pperm = lambda bk: ps[bk][:, :].rearrange("p (a b t) -> p b a t", a=4, b=16)
                            P.mm(ps[6 + c][:, :], ones_b[:], Eb[:, c * 512:(c + 1) * 512], True, False,
                                 reads=[('Eb', c), 'ones_b'], writes=[psk(6 + c)])
                            P.mm(pperm(6 + c), ones_b[:], Ec4[:, :, c, :, :], False, True,
                                 reads=['Ec'], writes=[psk(6 + c)])
                            P.mm(ps[4 + c][:, :], Vtok[:, 3, vsl], Eb[:, c * 512:(c + 1) * 512], True, False,
                                 reads=[('Eb', c), ('Vtok', lt - 1 if not is_s else 3)], writes=[psk(4 + c)])
                            for b in range(16):
                                P.mm(pperm(4 + c)[:, b, :, :], cVt[:, b, :], Ec4[:, b, c, :, :], False, b == 15,
                                     reads=['Ec', 'cV'], writes=[psk(4 + c)])
                    for c in range(2):
                        if CUT in (8, 9, 10, 11):
                            continue
                        hs = slice(c * 64, (c + 1) * 64)
                        rv = Rr[hs, c, :].rearrange("p (a b) -> p a b", a=4)
                        sk = sinkE[hs, c * 16 + kvh * 4: c * 16 + kvh * 4 + 4].unsqueeze(2).to_broadcast([64, 4, 128])
                        P.tt('dve', rv, ps[6 + c][hs, :].rearrange("p (a b) -> p a b", a=4), sk, ALU.add,
                             reads=[psk(6 + c), 'sinkE'], writes=['Rr'])
                        P.op('dve', lambda h, rv=rv: h.reciprocal(rv, rv), reads=['Rr'], writes=['Rr'])
                        P.tt('dve', mixT[hs, kvh * 4:(kvh + 1) * 4, qc:qc + 128],
                             ps[4 + c][hs, :].rearrange("p (a b) -> p a b", a=4), rv, ALU.mult,
                             reads=[psk(4 + c), 'Rr'], writes=['mixT'])

            if CUT in (7, 8, 9, 10, 11, 12):
                P.barrier()
                continue
            cv.off = off_pool
            Utok = cv.get([128, 4, 512], BF16)
            dT = cv.get([128, 4, 384], BF16)
            SPt = cv.get([128, 2, 512], BF16)
            for g4 in range(4 if stages >= 3 else (4 if stages >= 2 else 0)):
                def evac_u(lt, bank):
                    P.copy('dve', Utok[:, lt, :], ps[bank][:, :], reads=[psk(bank)], writes=[('Utok', lt)])
                    if smp and lt >= 2:
                        f = stf[0]
                        P.copy('dve', f, ps[bank][:, :], reads=[psk(bank)], writes=['stf'])
                        if lt == 2:
                            P.dma('sp', npp_d[:, g4 * 512:(g4 + 1) * 512], f[113:128, :], reads=['stf'], slot='o_misc', is_out=True)
                        else:
                            for b in range(16):
                                P.dma('sp', nps_d[b, 7:15, g4 * 512:(g4 + 1) * 512], f[b * 8:(b + 1) * 8, :], reads=['stf'],
                                      slot='o_misc', is_out=True)
                inproj(6 + g4, tiles_all, evac_u)
                if stages < 3:
                    continue
                Wp, wpk = wload(poolw_d[:, g4], [128, 4, 512])
                if smp:
                    P.dma('pool', SPt[0:120], sptm_d[:, :, g4 * 512:(g4 + 1) * 512], writes=['SPt'], slot='SPt')
                for lt in [1, 2, 3]:
                    bank = nextbank(0, 2)
                    first = (g == 0 and lt == 1)
                    for cc in range(4):
                        o = ps[bank][:, cc * 128:(cc + 1) * 128]
                        csl = slice(cc * 128, (cc + 1) * 128)
                        if smp and lt == 3:
                            P.mm(o, SPt[0:120, 0, csl], cmat[0:120, BSA + g4, :], True, False, reads=['SPt', 'cmat'], writes=[psk(bank)])
                            P.mm(o, SPt[0:120, 1, csl], cmat[0:120, BSB + g4, :], False, False, reads=['SPt'], writes=[psk(bank)])
                            P.mm(o, Utok[:, 3, csl], cmat[:, BSM + g4, :], False, True, reads=[('Utok', 3)], writes=[psk(bank)])
                        else:
                            P.mm(o, Utok[:, lt - 1, csl], cmat[:, (BPF if first else BP) + g4, :], True, False,
                                 reads=[('Utok', lt - 1), 'cmat'], writes=[psk(bank)])
                            P.mm(o, Utok[:, lt, csl], cmat[:, (BCF if first else BC) + g4, :], False, True,
                                 reads=[('Utok', lt)], writes=[psk(bank)])
                    P.copy('dve', dT[:, :, (lt - 1) * 128:lt * 128], ps[bank][:, :].rearrange("p (a b) -> p a b", a=4),
                           reads=[psk(bank)], writes=['dT'])
                for dc in range(4):
                    bank = nextbank(2, 4)
                    for cc in range(4):
                        P.mm(ps[bank][:, 0:384], Wp[:, cc, dc * 128:(dc + 1) * 128], dT[:, cc, :], cc == 0, cc == 3,
                             reads=['dT', wpk], writes=[psk(bank)])
                    fcm = 16 + g4 * 4 + dc
                    P.ts('dve', mixT[:, fcm, :], ps[bank][:, 0:384], pscT[:, g4 * 4 + dc:g4 * 4 + dc + 1], None, ALU.mult,
                         reads=[psk(bank), 'vecs'], writes=['mixT'])
            if stages < 4:
                P.barrier()
                continue

            P.barrier()
            z = big[:, :].rearrange("p (a b) -> p a b", a=32)
            h2T = R2[:, :].rearrange("p (a b) -> p a b", a=32)
            cv = Carver(S2)
            sqt = [cv.get([128, 384], F32) for _ in range(2)]
            tmpz = cv.get([128, 128], F32)
            oc0 = 3 * g * 128
            for kq in range(8):
                P.dma('sp', z[:, kq * 4:(kq + 1) * 4, :], xv[:, kq * 4:(kq + 1) * 4, c0 + 128:c0 + 512],
                      writes=[('z', kq * 4 + j) for j in range(4)], slot=('zl', kq))
                for j in range(4):
                    fc = kq * 4 + j
                    P.op('act', lambda h, fc=fc: h.mul(z[:, fc, :], z[:, fc, :], ALPHA), reads=[('z', fc)], writes=[('z', fc)])

            def z_accum(fc, src, gbase, srckey):
                zk = ('z', fc)
                P.stt('dve', z[:, fc, 0:ncp - 128], src[:, 0:ncp - 128], modP[:, gbase + fc:gbase + fc + 1], z[:, fc, 0:ncp - 128],
                      ALU.mult, ALU.add, reads=[srckey, zk, 'mod'], writes=[zk])
                if smp:
                    tv = tmpz.rearrange("p (b t) -> p b t", b=16)
                    P.tt('dve', tv, src[:, 256:384].rearrange("p (b t) -> p b t", b=16), bc_s(modS[:, gbase + fc, :]), ALU.mult,
                         reads=[srckey, 'mod'], writes=['tmpz'])
                    P.tt('dve', z[:, fc, 256:384], z[:, fc, 256:384], tmpz, ALU.add, reads=['tmpz', zk], writes=[zk])
                sq = sqt[fc % 2]
                P.actf(sq, z[:, fc, :], AF.Square, reads=[zk], writes=[('sqt', fc % 2)])
                P.mm(ps[6][:, 0:384], ones_f[:], z[:, fc, :], fc == 0, fc == 31, reads=[zk, 'ones_f'], writes=[psk(6)])
                P.mm(ps[7][:, 0:384], ones_f[:], sq, fc == 0, fc == 31, reads=[('sqt', fc % 2)], writes=[psk(7)])

            for wt in range(16):
                a, k = wload(wout_v[:, :, wt * 256:(wt + 1) * 256], [128, 32, 256])
                for j in range(2):
                    fc = wt * 2 + j
                    bank = nextbank(0, 4)
                    for kc in range(32):
                        P.mm(ps[bank][:, 0:384], a[:, kc, j * 128:(j + 1) * 128], mixT[:, kc, :], kc == 0, kc == 31,
                             reads=['mixT', k], writes=[psk(bank)])
                    z_accum(fc, ps[bank], 64, psk(bank))

            def post1(fc):
                zk = ('z', fc)
                P.actf(h2T[:, fc, 0:ncp - 128], z[:, fc, 0:ncp - 128], AF.Identity, bias=modP[:, 96 + fc:97 + fc],
                       scale=modP[:, 128 + fc:129 + fc], reads=[zk, 'mod'], writes=[('h2T', fc)])
                if smp:
                    tv = tmpz.rearrange("p (b t) -> p b t", b=16)
                    P.tt('dve', tv, z[:, fc, 256:384].rearrange("p (b t) -> p b t", b=16), bc_s(modS[:, 128 + fc, :]), ALU.mult,
                         reads=[zk, 'mod'], writes=['tmpz'])
                    P.tt('dve', h2T[:, fc, 256:384].rearrange("p (b t) -> p b t", b=16), tv, bc_s(modS[:, 96 + fc, :]), ALU.add,
                         reads=['tmpz', 'mod'], writes=[('h2T', fc)])
                P.op('act', lambda h: h.mul(z[:, fc, :], z[:, fc, :], ALPHA), reads=[zk], writes=[zk])

            def stash1(fc0):
                P.dma('sp', yv[:, fc0:fc0 + 4, oc0:oc0 + 384], z[:, fc0:fc0 + 4, :], reads=[('z', fc0 + j) for j in range(4)],
                      writes=[('yst', fc0)], slot=('yst', fc0))
            ln_block(z, ln1g, ln1b, g, post1, stash1)
            P.barrier()
            if stages < 5:
                continue

            y2T = big[:, :].rearrange("p (a b) -> p a b", a=32)
            cv = Carver(S2)
            s_sb = cv.get([128, 3, 16, 128], F32)
            tau = cv.get([128, 3, 8], F32)
            off_tmp = cv.off
            qT = cv.get([128, 16, 384], BF16)
            t16 = cv.get([128, 16, 16], F32)
            wk = cv.get([128, 256], F32)
            cand = cv.get([128, 8, 256], F32)
            b16 = cv.get([128, 8, 16], F32)
            smal = cv.get([128, 8, 8], F32)
            for wt in range(8):
                a, k = wload(wq_v[:, :, wt * 256:(wt + 1) * 256], [128, 32, 256])
                for j in range(2):
                    pc = wt * 2 + j
                    bank = nextbank(0, 4)
                    for kc in range(32):
                        P.mm(ps[bank][:, 0:384], a[:, kc, j * 128:(j + 1) * 128], h2T[:, kc, :], kc == 0, kc == 31,
                             reads=[('h2T', kc), k], writes=[psk(bank)])
                    P.copy('dve', qT[:, pc, :], ps[bank][:, 0:384], reads=[psk(bank)], writes=['qT'])
            subk, subk_key = wload(subk_d, [128, 16, 128])
            for t in range(3):
                for pc in range(16):
                    bk = 4 + pc // 4
                    P.mm(ps[bk][:, (pc % 4) * 128:(pc % 4 + 1) * 128], qT[:, pc, t * 128:(t + 1) * 128], subk[:, pc, :], True, True,
                         reads=['qT', subk_key], writes=[psk(bk)])
                for q4 in range(4):
                    P.copy('act' if q4 % 2 else 'dve', s_sb[:, t, q4 * 4:(q4 + 1) * 4, :],
                           ps[4 + q4][:, :].rearrange("p (a b) -> p a b", a=4), reads=[psk(4 + q4)], writes=['s_sb'])
                for pc in range(16):
                    P.op('dve', lambda h, pc=pc, t=t: h.max(t16[:, pc, 0:8], s_sb[:, t, pc, :]), reads=['s_sb'], writes=['t16'])
                    P.op('dve', lambda h, pc=pc, t=t: h.match_replace(wk[:, 0:128], t16[:, pc, 0:8], s_sb[:, t, pc, :], -1e30),
                         reads=['s_sb', 't16'], writes=['wk'])
                    P.op('dve', lambda h, pc=pc: h.max(t16[:, pc, 8:16], wk[:, 0:128]), reads=['wk'], writes=['t16'])
                t16v = t16.rearrange("p (h q) k -> p h q k", q=2)
                s_v = s_sb[:, t].rearrange("p (h q) k -> p h q k", q=2)

                def cand_top(tag):
                    P.tt('dve', cand.rearrange("p h (i j) -> p h i j", i=16),
                         t16v[:, :, 0, :].unsqueeze(3).to_broadcast([128, 8, 16, 16]),
                         t16v[:, :, 1, :].unsqueeze(2).to_broadcast([128, 8, 16, 16]), ALU.add, reads=['t16'], writes=['cand'])
                    for hh in range(8):
                        P.op('dve', lambda h, hh=hh: h.max(b16[:, hh, 0:8], cand[:, hh, :]), reads=['cand'], writes=['b16'])
                        P.op('dve', lambda h, hh=hh: h.match_replace(wk[:, :], b16[:, hh, 0:8], cand[:, hh, :], -1e30),
                             reads=['cand', 'b16'], writes=['wk'])
                        P.op('dve', lambda h, hh=hh: h.max(b16[:, hh, 8:16], wk[:, :]), reads=['wk'], writes=['b16'])
                cand_top(0)
                mcol = smal[:, 0, :]
                zcol = smal[:, 1, :]
                P.copy('dve', mcol, b16[:, :, 0], reads=['b16'], writes=['smal'])
                P.tt('dve', b16[:, :, :], b16[:, :, :], mcol.unsqueeze(2).to_broadcast([128, 8, 16]), ALU.subtract,
                     reads=['b16', 'smal'], writes=['b16'])
                P.actf(b16[:, :, :], b16[:, :, :], AF.Exp, reads=['b16'], writes=['b16'])
                P.op('dve', lambda h: h.reduce_sum(zcol, b16[:, :, :], AX.X), reads=['b16'], writes=['smal'])
                P.actf(zcol, zcol, AF.Ln, reads=['smal'], writes=['smal'])
                P.tt('dve', zcol, zcol, mcol, ALU.add, reads=['smal'], writes=['smal'])
                P.tt('dve', s_v[:, :, 1, :], s_v[:, :, 1, :], zcol.unsqueeze(2).to_broadcast([128, 8, 128]), ALU.subtract,
                     reads=['s_sb', 'smal'], writes=['s_sb'])
                P.tt('dve', t16v[:, :, 1, :], t16v[:, :, 1, :], zcol.unsqueeze(2).to_broadcast([128, 8, 16]), ALU.subtract,
                     reads=['t16', 'smal'], writes=['t16'])
                cand_top(1)
                P.copy('dve', tau[:, t, :], b16[:, :, 15], reads=['b16'], writes=['tau'])
            cv.off = off_tmp
            P.barrier()
            actT = [cv.get([128, 8, 384], BF16) for _ in range(2)]
            gel = [cv.get([128, 384], F32) for _ in range(2)]
            valb = [cv.get([128, 8, 128], F32) for _ in range(2)]
            Ebf = [cv.get([128, 8, 128], BF16) for _ in range(2)]
            Gb = [cv.get([128, 8, 128], BF16) for _ in range(2)]
            Uslot = [W[0][:, :].rearrange("p (a b) -> p a b", a=32), W[1][:, :].rearrange("p (a b) -> p a b", a=32)]
            Vslot = [W[2][:, 0:4096].rearrange("p (a b) -> p a b", a=8), W[2][:, 4096:8192].rearrange("p (a b) -> p a b", a=8)]
            NCH = 128
            ucount = [0]
            pend = [None]
            finp = [None]

            def stage1(i, t, sl):
                s_v = s_sb[:, t].rearrange("p (h q) k -> p h q k", q=2)
                if VAL_ENG == 'act':
                    for hh in range(8):
                        P.actf(valb[sl][:, hh, :], s_v[:, hh, 1, :], AF.Identity, bias=s_v[:, hh, 0, i:i + 1],
                               reads=['s_sb'], writes=[('valb', sl)])
                else:
                    P.tt(VAL_ENG, valb[sl], s_v[:, :, 1, :], s_v[:, :, 0, i:i + 1].to_broadcast([128, 8, 128]), ALU.add,
                         reads=['s_sb'], writes=[('valb', sl)])
                P.actf(Ebf[sl], valb[sl], AF.Exp, reads=[('valb', sl)], writes=[('Ebf', sl)])

            def stage2(i, t, sl):
                gb = 2 + i % 2
                P.tt('dve', valb[sl], valb[sl], tau[:, t, :].unsqueeze(2).to_broadcast([128, 8, 128]), ALU.is_ge,
                     reads=[('valb', sl), 'tau'], writes=[('valb', sl)])
                P.tt('dve', Gb[sl], valb[sl], Ebf[sl], ALU.mult, reads=[('valb', sl), ('Ebf', sl)], writes=[('Gb', sl)])
                for hh in range(8):
                    P.mm(ps[gb][:, t * 128:(t + 1) * 128], Gb[sl][:, hh, :], ident, hh == 0, hh == 7,
                         reads=[('Gb', sl), 'cmat'], writes=[psk(gb)])

            def finalize(i):
                EB, ci = i // 8, i % 8
                P.tt('dve', actT[EB % 2][:, ci, :], ps[2 + i % 2][:, 0:384], gel[i % 2], ALU.mult,
                     reads=[psk(2 + i % 2), ('gel', i % 2)], writes=[('actT', EB % 2, ci)])

            def step_unit(i, t):
                sl = ucount[0] % 2
                ucount[0] += 1
                stage1(i, t, sl)
                if finp[0] is not None and t == 2:
                    finalize(finp[0])
                    finp[0] = None
                if pend[0] is not None:
                    pi, pt, psl = pend[0]
                    stage2(pi, pt, psl)
                    if pt == 2:
                        finp[0] = pi
                pend[0] = (i, t, sl)
                emit_one_add()

            def flush_units():
                if finp[0] is not None:
                    finalize(finp[0])
                    finp[0] = None
                if pend[0] is not None:
                    pi, pt, psl = pend[0]
                    stage2(pi, pt, psl)
                    if pt == 2:
                        finalize(pi)
                    pend[0] = None

            vcount = [0]
            ypend = []

            def emit_one_add():
                if not ypend:
                    return
                fc, yb, first = ypend.pop(0)
                if first:
                    P.copy('dve', y2T[:, fc, :], ps[yb][:, 0:384], reads=[psk(yb)], writes=[('z', fc)])
                else:
                    P.tt('dve', y2T[:, fc, :], y2T[:, fc, :], ps[yb][:, 0:384], ALU.add, reads=[psk(yb), ('z', fc)],
                         writes=[('z', fc)])

            def vpart(v):
                EB, fq = v // 8, v % 8
                while ypend:
                    emit_one_add()
                vs = vcount[0] % 2
                vcount[0] += 1
                va = Vslot[vs]
                P.dma('pool', va, V_v[:, EB * 8:(EB + 1) * 8, fq * 512:(fq + 1) * 512], writes=[('Wv', vs)], slot=('Wv', vs))
                for fj in range(4):
                    fc = fq * 4 + fj
                    yb = 4 + fj
                    for ci in range(8):
                        P.mm(ps[yb][:, 0:384], va[:, ci, fj * 128:(fj + 1) * 128], actT[EB % 2][:, ci, :], ci == 0, ci == 7,
                             reads=[('actT', EB % 2, ci), ('Wv', vs)], writes=[psk(yb)])
                    ypend.append((fc, yb, EB == 0))

            VLAG = 9
            abank = [0, 1]
            for i in range(NCH):
                if i % 2 == 0:
                    us = (i // 2) % 2
                    P.dma('pool', Uslot[us], UT_v[:, :, i * 128:(i + 2) * 128], writes=[('W', us)], slot=('W', us))
                us = (i // 2) % 2
                j = i % 2
                ab = nextbank(0, 2)
                for kc in range(32):
                    P.mm(ps[ab][:, 0:384], Uslot[us][:, kc, j * 128:(j + 1) * 128], h2T[:, kc, :], kc == 0, kc == 31,
                         reads=[('h2T', kc), ('W', us)], writes=[psk(ab)])
                abank[i % 2] = ab
                if i % 2 == 1:
                    for ii in (i - 1, i):
                        P.actf(gel[ii % 2], ps[abank[ii % 2]][:, 0:384], AF.Gelu, reads=[psk(abank[ii % 2])], writes=[('gel', ii % 2)])
                if i >= VLAG:
                    vpart(i - VLAG)
                for t in range(3):
                    step_unit(i, t)
            flush_units()
            for v in range(NCH - VLAG, NCH):
                vpart(v)
            while ypend:
                emit_one_add()
            P.barrier()
            cv = Carver(S2)
            sqt = [cv.get([128, 384], F32) for _ in range(2)]
            tmpz = cv.get([128, 128], F32)
            xst = [cv.get([128, 4, 384], F32) for _ in range(2)]
            for kq in range(8):
                sl = kq % 2
                P.dma('sp', xst[sl], yv[:, kq * 4:(kq + 1) * 4, oc0:oc0 + 384], reads=[('yst', kq * 4)], writes=[('xst', sl)],
                      slot=('xst', sl))
                for j in range(4):
                    fc = kq * 4 + j
                    zk = ('z', fc)
                    P.stt('dve', y2T[:, fc, 0:ncp - 128], y2T[:, fc, 0:ncp - 128], modP[:, 160 + fc:161 + fc], xst[sl][:, j, 0:ncp - 128],
                          ALU.mult, ALU.add, reads=[zk, ('xst', sl), 'mod'], writes=[zk])
                    if smp:
                        tv = tmpz.rearrange("p (b t) -> p b t", b=16)
                        P.tt('dve', tv, y2T[:, fc, 256:384].rearrange("p (b t) -> p b t", b=16), bc_s(modS[:, 160 + fc, :]), ALU.mult,
                             reads=[zk, 'mod'], writes=['tmpz'])
                        P.tt('dve', y2T[:, fc, 256:384], tmpz, xst[sl][:, j, 256:384], ALU.add, reads=['tmpz', ('xst', sl)], writes=[zk])
                    sq = sqt[fc % 2]
                    P.actf(sq, y2T[:, fc, :], AF.Square, reads=[zk], writes=[('sqt', fc % 2)])
                    P.mm(ps[6][:, 0:384], ones_f[:], y2T[:, fc, :], fc == 0, fc == 31, reads=[zk, 'ones_f'], writes=[psk(6)])
                    P.mm(ps[7][:, 0:384], ones_f[:], sq, fc == 0, fc == 31, reads=[('sqt', fc % 2)], writes=[psk(7)])

            def stash2(fc0):
                P.dma('sp', yv[:, fc0:fc0 + 4, oc0:oc0 + 384], y2T[:, fc0:fc0 + 4, :], reads=[('z', fc0 + j) for j in range(4)],
                      writes=[('yst', fc0)], slot='o_y', is_out=True)
            ln_block(y2T, ln2g, ln2b, g, None, stash2)
            P.barrier()
        P.emit()
    return nc


def _bf(x):
    return np.ascontiguousarray(x, dtype=np.float32)


def _consts(hf):
    cm = np.zeros((NMAT, 128, 128), np.float32)
    k = np.arange(128)[:, None]
    q = np.arange(128)[None, :]
    cm[MP] = (k >= q)
    cm[MC] = (k <= q)
    cm[MFP] = cm[MP] if hf == 1 else 0.0
    cm[MSN] = ((k // 8) == (q // 8)) & ((k % 8) <= (q % 8))
    cm[IDM] = np.eye(128)
    cm[MSC, :, 0:8] = (np.arange(128)[:, None] >= np.arange(8)[None, :])
    for g4, w in enumerate((2, 4, 8, 16)):
        tp = np.arange(128)[:, None]
        t = np.arange(128)[None, :]
        cur = ((tp <= t) & (tp > t - w)).astype(np.float32)
        prv = (tp - 128 > t - w).astype(np.float32)
        cm[BP + g4] = prv / w
        cm[BC + g4] = cur / w - np.eye(128)
        if hf == 1:
            cm[BPF + g4] = cm[BP + g4]
            cm[BCF + g4] = cm[BC + g4]
        else:
            cnt = np.minimum(w, t + 1).astype(np.float32)
            cm[BPF + g4] = 0.0
            cm[BCF + g4] = cur / cnt - np.eye(128)
        bq, tq = np.arange(128)[None, :] // 8, np.arange(128)[None, :] % 8
        r_b, r_r = np.arange(120)[:, None] // 15, np.arange(120)[:, None] % 15
        for half, idx in ((0, BSA), (1, BSB)):
            m = ((r_b + 8 * half) == bq) & (r_r > 15 + tq - w)
            cm[idx + g4, 0:120] = m / w
        bk, tk = np.arange(128)[:, None] // 8, np.arange(128)[:, None] % 8
        m = (bk == bq) & (tk <= tq) & (tk > tq - w)
        cm[BSM + g4] = m / w - np.eye(128)
    return np.ascontiguousarray(cm.transpose(1, 0, 2))


def _rope_tab(hf):
    half = 8
    inv = (np.float32(500000.0) ** (-np.arange(half, dtype=np.float32) / np.float32(half))).astype(np.float32)
    tab = np.zeros((128, 10, 2, 8), np.float32)
    for gt in range(10):
        if gt < 9:
            pos = hf * 1024 + (gt - 1) * 128 + np.arange(128)
        else:
            pos = 8192 + (np.arange(128) % 8)
        ang = pos.astype(np.float32)[:, None] * inv[None, :]
        tab[:, gt, 0, :] = np.cos(ang)
        tab[:, gt, 1, :] = np.sin(ang)
    return tab


_NC_CACHE = {}


def _prep(x_prompt, x_sample, cache_k, cache_v, state_pool, c_prompt, c_sample, w_ada, b_ada, w_in,
          sinks, pool_w, pool_scale, w_out, ln1_g, ln1_b, peer_wq, peer_subkeys, peer_u, peer_v,
          ln2_g, ln2_b):
    f = lambda a: np.asarray(a, dtype=np.float32)
    x_prompt, x_sample, cache_k, cache_v, state_pool = map(f, (x_prompt, x_sample, cache_k, cache_v, state_pool))
    c_prompt, c_sample = f(c_prompt), f(c_sample)
    wi = f(w_in)[0]
    kcol = wi[:, 2048:2304].reshape(D, 4, 1, 64)
    vcol = wi[:, 2304:2560].reshape(D, 4, 1, 64)
    win_r = np.concatenate([np.broadcast_to(kcol, (D, 4, 2, 64)).reshape(D, 512),
                            np.broadcast_to(vcol, (D, 4, 2, 64)).reshape(D, 512),
                            wi[:, 0:2048], wi[:, 2560:4608]], axis=1)
    vecs = np.zeros((128, 384), np.float32)
    vecs[:, 0:192] = f(b_ada)[0].reshape(192, 128).T
    vecs[:, 192:208] = f(pool_scale)[0].reshape(16, 128).T
    for i, a in enumerate((ln1_g, ln1_b, ln2_g, ln2_b)):
        vecs[:, 208 + 32 * i:240 + 32 * i] = f(a)[0].reshape(32, 128).T
    vecs[:, 336:368] = np.broadcast_to(f(sinks)[0].reshape(16, 2).T.reshape(1, 32), (128, 32))
    common = {
        "w_ada": _bf(f(w_ada)[0]), "w_in": _bf(win_r), "w_out": _bf(f(w_out)[0]), "wq": _bf(f(peer_wq)[0]),
        "subk": _bf(f(peer_subkeys)[0].transpose(3, 0, 1, 2).reshape(128, 16, 128)),
        "UT": _bf(f(peer_u)[0].T), "V": _bf(f(peer_v)[0]),
        "poolw": _bf(f(pool_w)[0].reshape(4, 4, 128, 512).transpose(2, 0, 1, 3)),
        "vecs": vecs,
    }
    in_maps = []
    for r in range(8):
        b, hf = r // 2, r % 2
        xs = x_sample[16 * r:16 * r + 16].reshape(128, D)
        t0 = hf * 1024
        halo = x_prompt[b, t0 - 128:t0] if hf == 1 else np.zeros((128, D), np.float32)
        xT = np.concatenate([halo, x_prompt[b, t0:t0 + 1024], xs], axis=0).T
        cc = np.concatenate([c_prompt[b:b + 1], c_sample[16 * r:16 * r + 16]], axis=0)
        ck = cache_k[0, 16 * r:16 * r + 16]
        cvv = cache_v[0, 16 * r:16 * r + 16]
        ckt = ck.transpose(3, 0, 2, 1)
        cvt = cvv.transpose(1, 0, 2, 3)
        sp = state_pool[0, 16 * r:16 * r + 16]
        m = dict(common)
        m.update({
            "xT": _bf(xT),
            "cT": _bf(cc.T.reshape(32, 128, 17).transpose(1, 0, 2)),
            "cmat": _consts(hf), "rope": _rope_tab(hf),
            "cKT": _bf(np.concatenate([ckt, ckt], axis=0)),
            "cV": _bf(np.concatenate([cvt, cvt], axis=3)),
            "ckn": _bf(ck.reshape(16, 128, 256)), "cvn": _bf(cvv.reshape(16, 128, 256)),
            "sptm": _bf(sp.reshape(2, 120, 2048).transpose(1, 0, 2)), "spn": _bf(sp),
        })
        in_maps.append(m)
    return in_maps


def _assemble(res, cores=tuple(range(8))):
    y_p = np.zeros((4, 2048, D), np.float32)
    y_s = np.zeros((128, 8, D), np.float32)
    nkp = np.zeros((1, 4, 128, 4, 64), np.float32)
    nvp = np.zeros_like(nkp)
    npp = np.zeros((1, 4, 15, 2048), np.float32)
    nks = np.zeros((1, 128, 128, 4, 64), np.float32)
    nvs = np.zeros_like(nks)
    nps = np.zeros((1, 128, 15, 2048), np.float32)
    for r in cores:
        b, hf = r // 2, r % 2
        o = res[cores.index(r)]
        yT = o["yT"]
        y_p[b, hf * 1024:(hf + 1) * 1024] = yT[:, 0:1024].T
        y_s[16 * r:16 * r + 16] = yT[:, 1024:1152].T.reshape(16, 8, D)
        if hf == 1:
            nkp[0, b] = o["nkp"].reshape(128, 4, 64)
            nvp[0, b] = o["nvp"].reshape(128, 4, 64)
            npp[0, b] = o["npp"]
        nks[0, 16 * r:16 * r + 16] = o["nks"].reshape(16, 128, 4, 64)
        nvs[0, 16 * r:16 * r + 16] = o["nvs"].reshape(16, 128, 4, 64)
        nps[0, 16 * r:16 * r + 16] = o["nps"]
    return (y_p, y_s, nkp, nvp, npp, nks, nvs, nps)


def kernel(**inputs):
    in_maps = _prep(**inputs)
    if 'nc' not in _NC_CACHE:
        _NC_CACHE['nc'] = build_nc()
    res = run_bass_kernel_spmd(_NC_CACHE['nc'], in_maps, core_ids=list(range(8))).results
    return _assemble(res)
```

```python
import numpy as np
from contextlib import ExitStack
import concourse.bass as bass
import concourse.mybir as mybir
from concourse.bass_utils import run_bass_kernel_spmd

F32 = mybir.dt.float32
BF16 = mybir.dt.bfloat16
ALU = mybir.AluOpType
AF = mybir.ActivationFunctionType
AX = mybir.AxisListType
ENG = ('pe', 'act', 'dve', 'pool', 'sp')


class Prog:
    def __init__(self, nc, es):
        self.nc = nc
        self.es = es
        self.ops = {e: [] for e in ENG}
        self.n = {e: 0 for e in ENG}
        self.seen = {e: {} for e in ENG}
        self.res = {}
        self.dcount = {}
        self.targets = {e: set() for e in ENG}
        self.out_slots = set()

    def _deps(self, reads, writes):
        d = []
        for k in reads:
            r = self.res.get(k)
            if r and r[0] is not None:
                d.append(r[0])
        for k in writes:
            r = self.res.get(k)
            if r:
                if r[0] is not None:
                    d.append(r[0])
                for sk, v in r[1].items():
                    d.append((sk[0], sk[1], v))
        return d

    def _waits(self, eng, deps, skip_same=False):
        for kind, key, val in deps:
            if kind == 'e' and key == eng and skip_same:
                continue
            if kind == 'd':
                val = self.dcount[key]
            sk = (kind, key)
            if self.seen[eng].get(sk, -1) >= val:
                continue
            self.seen[eng][sk] = val
            self.ops[eng].append(('w', kind, key, val))
            if kind == 'e':
                self.targets[key].add(val)

    def _update(self, me, reads, writes):
        for k in writes:
            self.res[k] = [me, {}]
        for k in reads:
            r = self.res.setdefault(k, [None, {}])
            sk = (me[0], me[1])
            if r[1].get(sk, -1) < me[2]:
                r[1][sk] = me[2]

    def op(self, eng, fn, reads=(), writes=(), skip_same=False):
        self._waits(eng, self._deps(reads, writes), skip_same)
        idx = self.n[eng]
        self.n[eng] += 1
        self.ops[eng].append(('i', fn, idx))
        self._update(('e', eng, idx), reads, writes)

    def dma(self, eng, out, in_, reads=(), writes=(), slot=None, is_out=False):
        self._waits(eng, self._deps(reads, writes))
        c = self.dcount.get(slot, 0) + 16
        self.dcount[slot] = c
        self.ops[eng].append(('d', out, in_, slot))
        self._update(('d', slot, c), reads, writes)
        if is_out:
            self.out_slots.add(slot)

    def barrier(self):
        for e in ENG:
            deps = []
            for f in ENG:
                if f != e and self.n[f] > 0:
                    deps.append(('e', f, self.n[f] - 1))
            for s, c in self.dcount.items():
                deps.append(('d', s, c))
            self._waits(e, deps)

    def mm(self, out, lhsT, rhs, start, stop, reads=(), writes=()):
        self.op('pe', lambda h: h.matmul(out, lhsT, rhs, start=start, stop=stop), reads, writes, skip_same=True)

    def tr(self, out, in_, ident, reads=(), writes=()):
        self.op('pe', lambda h: h.transpose(out, in_, ident), reads, writes, skip_same=True)

    def actf(self, out, in_, func, bias=None, scale=None, reads=(), writes=(), eng='act'):
        kw = {}
        if bias is not None:
            kw['bias'] = bias
        if scale is not None:
            kw['scale'] = scale
        self.op(eng, lambda h: h.activation(out, in_, func, **kw), reads, writes)

    def tt(self, eng, out, in0, in1, op, reads=(), writes=()):
        self.op(eng, lambda h: h.tensor_tensor(out, in0, in1, op), reads, writes)

    def ts(self, eng, out, in0, s1, s2, op0, op1=None, reads=(), writes=()):
        if op1 is None:
            self.op(eng, lambda h: h.tensor_scalar(out, in0, s1, None, op0), reads, writes)
        else:
            self.op(eng, lambda h: h.tensor_scalar(out, in0, s1, s2, op0, op1), reads, writes)

    def stt(self, eng, out, in0, scalar, in1, op0, op1, reads=(), writes=()):
        self.op(eng, lambda h: h.scalar_tensor_tensor(out, in0, scalar, in1, op0, op1), reads, writes)

    def copy(self, eng, out, in_, reads=(), writes=()):
        if eng == 'act':
            self.op(eng, lambda h: h.copy(out, in_), reads, writes)
        else:
            self.op(eng, lambda h: h.tensor_copy(out, in_), reads, writes)

    def emit(self):
        nc = self.nc
        es = self.es
        sem = {e: es.enter_context(nc.semaphore("sem_" + e)) for e in ENG}
        dsem = {}
        for i, s in enumerate(self.dcount):
            dsem[s] = es.enter_context(nc.semaphore("dsem%d" % i))
        rank = {e: {idx: i + 1 for i, idx in enumerate(sorted(self.targets[e]))} for e in ENG}
        for s in sorted(self.out_slots, key=str):
            self.ops['sp'].append(('w', 'd', s, self.dcount[s]))
        block = es.enter_context(nc.Block())

        def run(e, h):
            for rec in self.ops[e]:
                if rec[0] == 'w':
                    _, kind, key, val = rec
                    if kind == 'e':
                        h.wait_ge(sem[key], rank[key][val])
                    else:
                        h.wait_ge(dsem[key], val)
                elif rec[0] == 'i':
                    ins = rec[1](h)
                    if rec[2] in rank[e]:
                        ins.then_inc(sem[e], 1)
                else:
                    _, out, in_, slot = rec
                    h.dma_start(out=out, in_=in_).then_inc(dsem[slot], 16)

        block.tensor(lambda h: run('pe', h))
        block.scalar(lambda h: run('act', h))
        block.vector(lambda h: run('dve', h))
        block.gpsimd(lambda h: run('pool', h))
        block.sync(lambda h: run('sp', h))
        print("PROG ops:", {e: len(self.ops[e]) for e in ENG}, "dsems", len(dsem))


CUT = 99
D = 4096
ALPHA = 2.0 ** 0.25
LN_EPS = 1e-5
MP, MC, MFP, MSN, IDM, BP, BC, BPF, BCF, BSA, BSB, BSM, MSC = 0, 1, 2, 3, 4, 5, 9, 13, 17, 21, 25, 29, 33
NMAT = 34
STAGES = 99
VAL_ENG = 'act'


def build_nc(stages=STAGES, groups=(0, 1, 2), skipA=False):
    nc = bass.Bass("TRN2", target_bir_lowering=False)
    dt_in = lambda n, s: nc.dram_tensor(n, s, F32, kind="ExternalInput").ap()
    dt_out = lambda n, s: nc.dram_tensor(n, s, F32, kind="ExternalOutput").ap()
    xT_d = dt_in("xT", [D, 1280])
    cT_d = dt_in("cT", [128, 32, 17])
    cmat_d = dt_in("cmat", [128, NMAT, 128])
    rope_d = dt_in("rope", [128, 10, 2, 8])
    vecs_d = dt_in("vecs", [128, 384])
    cKT_d = dt_in("cKT", [128, 16, 4, 128])
    cV_d = dt_in("cV", [128, 16, 4, 128])
    ckn_d = dt_in("ckn", [16, 128, 256])
    cvn_d = dt_in("cvn", [16, 128, 256])
    sptm_d = dt_in("sptm", [120, 2, 2048])
    spn_d = dt_in("spn", [16, 15, 2048])
    wada_d = dt_in("w_ada", [D, 24576] if not skipA else [128, 128])
    win_d = dt_in("w_in", [D, 5120])
    wout_d = dt_in("w_out", [D, D] if stages >= 4 else [128, 128])
    wq_d = dt_in("wq", [D, 2048] if stages >= 5 else [128, 128])
    subk_d = dt_in("subk", [128, 16, 128])
    UT_d = dt_in("UT", [D, 16384] if stages >= 5 else [128, 128])
    V_d = dt_in("V", [16384, D] if stages >= 5 else [128, 128])
    poolw_d = dt_in("poolw", [128, 4, 4, 512])
    yT_d = dt_out("yT", [D, 1152])
    nkp_d = dt_out("nkp", [128, 256])
    nvp_d = dt_out("nvp", [128, 256])
    npp_d = dt_out("npp", [15, 2048])
    nks_d = dt_out("nks", [16, 128, 256])
    nvs_d = dt_out("nvs", [16, 128, 256])
    nps_d = dt_out("nps", [16, 15, 2048])

    es = ExitStack()
    with es:
        P = Prog(nc, es)
        sb = lambda name, shape, dt: es.enter_context(nc.sbuf_tensor("s_" + name, shape, dt))
        ps = [es.enter_context(nc.psum_tensor("ps%d" % i, [128, 512], F32)) for i in range(8)]
        psk = lambda i: ('ps', i)

        cmat = sb("cmat", [128, NMAT, 128], BF16)
        rope = sb("rope", [128, 10, 2, 8], F32)
        vecs = sb("vecs", [128, 384], F32)
        modP = sb("modP", [128, 192], F32)
        modS = sb("modS", [128, 192, 16], F32)
        cT = sb("cTs", [128, 32, 17], F32)
        siluT = sb("siluT", [128, 32, 17], BF16)
        sinkE = sb("sinkE", [128, 32], F32)
        ones_b = sb("ones_b", [128, 128], BF16)
        ones_f = sb("ones_f", [128, 128], F32)
        W = [sb("W%d" % i, [128, 8192], BF16) for i in range(3)]
        R2 = sb("R2", [128, 12288], BF16)
        big = sb("big", [128, 12288], F32)
        S2 = sb("S2", [128, 14848], F32)

        b_adaT = vecs[:, 0:192]
        pscT = vecs[:, 192:208]
        ln1g, ln1b, ln2g, ln2b = (vecs[:, 208 + 32 * i:240 + 32 * i] for i in range(4))
        ident = cmat[:, IDM, :]

        class Carver:
            def __init__(self, t):
                self.t = t
                self.off = 0

            def get(self, shape, dt):
                n = int(np.prod(shape[1:]))
                nb = n * (2 if dt == BF16 else 4)
                nw = (nb + 3) // 4
                a = self.t[:, self.off:self.off + nw]
                self.off += nw
                assert self.off <= 14848, self.off
                if dt == BF16:
                    a = a.bitcast(BF16)[:, 0:n]
                if len(shape) == 3:
                    a = a.rearrange("p (a b) -> p a b", a=shape[1])
                elif len(shape) == 4:
                    a = a.rearrange("p (a b c) -> p a b c", a=shape[1], b=shape[2])
                return a

        wslot = [0]

        def wload(src_ap, view_shape, key_extra=None):
            s = wslot[0] % 3
            wslot[0] += 1
            n = int(np.prod(view_shape[1:]))
            a = W[s][0:view_shape[0], 0:n]
            if len(view_shape) == 3:
                a = a.rearrange("p (a b) -> p a b", a=view_shape[1])
            P.dma('pool', a, src_ap, writes=[('W', s)], slot=('W', s))
            return a, ('W', s)

        P.dma('pool', cmat[:], cmat_d, writes=['cmat'], slot='c_cmat')
        P.dma('sp', rope[:], rope_d, writes=['rope'], slot='c_rope')
        P.dma('sp', vecs[:], vecs_d, writes=['vecs'], slot='c_vecs')
        P.dma('sp', cT[:], cT_d, writes=['cT'], slot='c_cT')
        P.op('dve', lambda h: h.memset(ones_b[:], 1.0), writes=['ones_b'])
        P.op('dve', lambda h: h.memset(ones_f[:], 1.0), writes=['ones_f'])
        P.actf(siluT[:], cT[:], AF.Silu, reads=['cT'], writes=['siluT'])
        P.actf(sinkE[:], vecs[:, 336:368], AF.Exp, reads=['vecs'], writes=['sinkE'])
        P.dma('sp', nks_d[:, 0:120, :], ckn_d[:, 8:128, :], slot='o_misc', is_out=True)
        P.dma('sp', nvs_d[:, 0:120, :], cvn_d[:, 8:128, :], slot='o_misc', is_out=True)
        P.dma('sp', nps_d[:, 0:7, :], spn_d[:, 8:15, :], slot='o_misc', is_out=True)

        wada_v = wada_d.rearrange("(kc p) c -> p kc c", p=128)
        if skipA:
            P.op('dve', lambda h: h.memset(modP[:], 0.25), writes=['mod'])
            P.op('dve', lambda h: h.memset(modS[:], 0.25), writes=['mod'])
        WA = [big[:, 0:8192].bitcast(BF16).rearrange("p (a b) -> p a b", a=32),
              S2[:, 0:8192].bitcast(BF16).rearrange("p (a b) -> p a b", a=32)]
        for cg in range(0 if skipA else 48):
            s = cg % 2
            P.dma('pool', WA[s], wada_v[:, :, cg * 512:(cg + 1) * 512], writes=[('WA', s)], slot=('WA', s))
            bank = cg % 2
            pv = ps[bank][:, 0:68].rearrange("p (j n) -> p j n", j=4)
            for j in range(4):
                for kc in range(32):
                    P.mm(pv[:, j, :], WA[s][:, kc, j * 128:(j + 1) * 128], siluT[:, kc, :], kc == 0, kc == 31,
                         reads=[('WA', s), 'siluT'], writes=[psk(bank)])
            m0 = cg * 4
            P.tt('dve', modP[:, m0:m0 + 4].unsqueeze(2), pv[:, :, 0:1], b_adaT[:, m0:m0 + 4].unsqueeze(2), ALU.add,
                 reads=[psk(bank), 'vecs'], writes=['mod'])
            P.tt('dve', modS[:, m0:m0 + 4, :], pv[:, :, 1:17],
                 b_adaT[:, m0:m0 + 4].unsqueeze(2).to_broadcast([128, 4, 16]), ALU.add,
                 reads=[psk(bank), 'vecs'], writes=['mod'])
        for m in (1, 4):
            P.ts('dve', modP[:, m * 32:(m + 1) * 32], modP[:, m * 32:(m + 1) * 32], 1.0, None, ALU.add,
                 reads=['mod'], writes=['mod'])
            P.ts('dve', modS[:, m * 32:(m + 1) * 32, :], modS[:, m * 32:(m + 1) * 32, :], 1.0, None, ALU.add,
                 reads=['mod'], writes=['mod'])
        P.barrier()

        xv = xT_d.rearrange("(kc p) t -> p kc t", p=128)
        yv = yT_d.rearrange("(kc p) t -> p kc t", p=128)
        win_v = win_d.rearrange("(kc p) c -> p kc c", p=128)
        wout_v = wout_d.rearrange("(kc p) c -> p kc c", p=128)
        wq_v = wq_d.rearrange("(kc p) c -> p kc c", p=128)
        UT_v = UT_d.rearrange("(kc p) c -> p kc c", p=128)
        V_v = V_d.rearrange("(c p) f -> p c f", p=128)

        def bc_s(ap_b16, n=8):
            return ap_b16.unsqueeze(2).to_broadcast([ap_b16.shape[0], 16, n])

        pbank = [0]

        def nextbank(lo, hi):
            b = lo + pbank[0] % (hi - lo)
            pbank[0] += 1
            return b

        def ln_block(z, gcol, bcol, g, mod_post, stash):
            cv = Carver(S2)
            cv.off = 11000
            mean = cv.get([128, 384], F32)
            rstd = cv.get([128, 384], F32)
            tmpn = cv.get([128, 384], F32)
            P.op('act', lambda h: h.mul(mean, ps[6][:, 0:384], 1.0 / D), reads=[psk(6)], writes=['mean'])
            P.op('act', lambda h: h.mul(rstd, ps[7][:, 0:384], 1.0 / D), reads=[psk(7)], writes=['rstd'])
            P.tt('dve', tmpn, mean, mean, ALU.mult, reads=['mean'], writes=['tmpn'])
            P.tt('dve', rstd, rstd, tmpn, ALU.subtract, reads=['rstd', 'tmpn'], writes=['rstd'])
            P.ts('dve', rstd, rstd, LN_EPS, None, ALU.add, reads=['rstd'], writes=['rstd'])
            P.actf(rstd, rstd, AF.Sqrt, reads=['rstd'], writes=['rstd'])
            P.op('dve', lambda h: h.reciprocal(rstd, rstd), reads=['rstd'], writes=['rstd'])
            for fc in range(32):
                zk = ('z', fc)
                P.tt('dve', z[:, fc, :], z[:, fc, :], mean, ALU.subtract, reads=[zk, 'mean'], writes=[zk])
                P.tt('dve', z[:, fc, :], z[:, fc, :], rstd, ALU.mult, reads=[zk, 'rstd'], writes=[zk])
                P.ts('dve', z[:, fc, :], z[:, fc, :], gcol[:, fc:fc + 1], bcol[:, fc:fc + 1], ALU.mult, ALU.add,
                     reads=[zk, 'vecs'], writes=[zk])
                if mod_post is not None:
                    mod_post(fc)
                if stash is not None and fc % 4 == 3:
                    stash(fc - 3)

        for g in (groups if stages >= 2 else ()):
            c0 = 3 * g * 128
            smp = (g == 2)
            ncp = 384 if smp else 512
            own = [1, 2] if smp else [1, 2, 3]
            tiles_all = [0, 1, 2, 3]
            hT = big[:, 0:8192].bitcast(BF16).rearrange("p (a b) -> p a b", a=32)
            xs = big[:, 8192:12288].rearrange("p (s j t) -> p s j t", s=2, j=4)
            mixT = R2[:, :].rearrange("p (a b) -> p a b", a=32)
            cv = Carver(S2)
            KT = cv.get([128, 4, 512], BF16)
            Vtok = cv.get([128, 4, 512], BF16)
            stf = [cv.get([128, 512], F32) for _ in range(2)]
            stb = [cv.get([128, 512], BF16) for _ in range(2)]
            rt = cv.get([128, 4, 64], F32)
            QTz = cv.get([128, 2, 4, 384], BF16)
            Eb = cv.get([128, 2048], BF16)
            Ef = cv.get([128, 2048], F32)
            Rr = cv.get([128, 2, 512], F32)
            cK = cv.get([128, 16, 128], BF16)
            cVt = cv.get([128, 16, 128], BF16)
            Ec = cv.get([128, 1024], BF16)
            tmpS = cv.get([128, 128], F32)
            off_pool = cv.off
            P.op('dve', lambda h: h.memset(QTz, 0.0), writes=['QT'])
            for kq in range(8):
                sl = kq % 2
                P.dma('sp', xs[:, sl], xv[:, kq * 4:(kq + 1) * 4, c0:c0 + 512], writes=[('xs', sl)], slot=('xs', sl))
                for j in range(4):
                    kc = kq * 4 + j
                    if kc % 2 == 0:
                        P.ts('dve', hT[:, kc, 0:ncp], xs[:, sl, j, 0:ncp], modP[:, 32 + kc:33 + kc], modP[:, kc:kc + 1],
                             ALU.mult, ALU.add, reads=[('xs', sl), 'mod'], writes=[('hT', kc)])
                    else:
                        P.actf(hT[:, kc, 0:ncp], xs[:, sl, j, 0:ncp], AF.Identity, bias=modP[:, kc:kc + 1],
                               scale=modP[:, 32 + kc:33 + kc], reads=[('xs', sl), 'mod'], writes=[('hT', kc)])
                    if smp:
                        tv = tmpS.rearrange("p (b t) -> p b t", b=16)
                        P.tt('dve', tv, xs[:, sl, j, 384:512].rearrange("p (b t) -> p b t", b=16),
                             bc_s(modS[:, 32 + kc, :]), ALU.mult, reads=[('xs', sl), 'mod'], writes=['tmpS'])
                        P.tt('dve', hT[:, kc, 384:512].rearrange("p (b t) -> p b t", b=16), tv,
                             bc_s(modS[:, kc, :]), ALU.add, reads=['tmpS', 'mod'], writes=[('hT', kc)])
            if CUT == 1:
                P.barrier()
                continue

            def rope_inplace(f, gt):
                f3 = f.rearrange("p (h d) -> p h d", h=8)
                x1 = f3[:, :, 0:8]
                x2 = f3[:, :, 8:16]
                cs = rope[:, gt, 0, :].unsqueeze(1).to_broadcast([128, 8, 8])
                sn = rope[:, gt, 1, :].unsqueeze(1).to_broadcast([128, 8, 8])
                r = [rt[:, i, :].rearrange("p (h d) -> p h d", h=8) for i in range(4)]
                P.tt('dve', r[0], x1, cs, ALU.mult, reads=['stf', 'rope'], writes=['rt'])
                P.tt('dve', r[1], x2, sn, ALU.mult, reads=['stf', 'rope'], writes=['rt'])
                P.tt('dve', r[2], x2, cs, ALU.mult, reads=['stf', 'rope'], writes=['rt'])
                P.tt('dve', r[3], x1, sn, ALU.mult, reads=['stf', 'rope'], writes=['rt'])
                P.tt('dve', x1, r[0], r[1], ALU.subtract, reads=['rt'], writes=['stf'])
                P.tt('dve', x2, r[2], r[3], ALU.add, reads=['rt'], writes=['stf'])

            def inproj(cg, lts, evac):
                tl = []
                for half in range(2):
                    a, k = wload(win_v[:, :, cg * 512 + half * 256: cg * 512 + half * 256 + 256], [128, 32, 256])
                    tl.append((a, k))
                for lt in lts:
                    bank = nextbank(0, 4)
                    for half in range(2):
                        a, k = tl[half]
                        for kc in range(32):
                            P.mm(ps[bank][:, half * 256:(half + 1) * 256], hT[:, kc, lt * 128:(lt + 1) * 128], a[:, kc, :],
                                 kc == 0, kc == 31, reads=[('hT', kc), k], writes=[psk(bank)])
                    if CUT >= 3:
                        evac(lt, bank)

            def out_rows(dst_p, dst_s, src, lt, view=None):
                if not smp:
                    return
                if lt == 2 and dst_p is not None:
                    P.dma('sp', dst_p, src if view is None else view(src), reads=['stf'], slot='o_misc', is_out=True)
                if lt == 3:
                    for b in range(16):
                        s_ = src[b * 8:(b + 1) * 8]
                        P.dma('sp', dst_s(b), s_ if view is None else view(s_), reads=['stf'], slot='o_misc', is_out=True)

            kview = lambda a: a.rearrange("p (k c d) -> p k c d", k=4, c=2)[:, :, 0, :]

            def evac_k(lt, bank):
                gt = 3 * g + lt
                f = stf[0]
                P.copy('act', f, ps[bank][:, :], reads=[psk(bank)], writes=['stf'])
                if CUT == 3:
                    return
                rope_inplace(f, gt)
                P.copy('act', stb[0], f, reads=['stf'], writes=['stb'])
                if CUT == 4:
                    return
                pb = ps[4 + lt % 2][:].bitcast(BF16)
                for kv in range(4):
                    P.tr(pb[:, kv * 128:(kv + 1) * 128], stb[0][:, kv * 128:(kv + 1) * 128], ident,
                         reads=['stb', 'cmat'], writes=[psk(4 + lt % 2)])
                P.copy('dve', KT[:, :, lt * 128:(lt + 1) * 128], pb[:, 0:512].rearrange("p (k t) -> p k t", k=4),
                       reads=[psk(4 + lt % 2)], writes=['KT'])
                out_rows(nkp_d.rearrange("p (k d) -> p k d", k=4),
                         lambda b: nks_d[b, 120:128, :].rearrange("p (k d) -> p k d", k=4), f, lt, kview)
            inproj(0, tiles_all, evac_k)
            if CUT <= 5:
                P.barrier()
                continue

            def evac_v(lt, bank):
                P.copy('dve', Vtok[:, lt, :], ps[bank][:, :], reads=[psk(bank)], writes=[('Vtok', lt)])
                if smp and lt >= 2:
                    f = stf[0]
                    P.copy('dve', f, ps[bank][:, :], reads=[psk(bank)], writes=['stf'])
                    out_rows(nvp_d.rearrange("p (k d) -> p k d", k=4),
                             lambda b: nvs_d[b, 120:128, :].rearrange("p (k d) -> p k d", k=4), f, lt, kview)
            inproj(1, tiles_all, evac_v)
            if CUT == 6:
                P.barrier()
                continue

            for kvh in range(4 if stages >= 3 else 0):
                def evac_q(lt, bank):
                    gt = 3 * g + lt
                    f = stf[1]
                    P.copy('act', f, ps[bank][:, :], reads=[psk(bank)], writes=['stf'])
                    rope_inplace(f, gt)
                    P.copy('act', stb[1], f, reads=['stf'], writes=['stb'])
                    pb = ps[4 + lt % 2][:].bitcast(BF16)
                    for pc in range(4):
                        P.tr(pb[:, pc * 128:(pc + 1) * 128], stb[1][:, pc * 128:(pc + 1) * 128], ident,
                             reads=['stb', 'cmat'], writes=[psk(4 + lt % 2)])
                    for c in range(2):
                        hs = slice(c * 64, (c + 1) * 64)
                        P.copy('dve', QTz[hs, c, :, (lt - 1) * 128:lt * 128], pb[hs, 0:512].rearrange("p (k t) -> p k t", k=4),
                               reads=[psk(4 + lt % 2)], writes=['QT'])
                inproj(2 + kvh, [1, 2, 3], evac_q)
                if smp:
                    P.dma('pool', cK, cKT_d[:, :, kvh, :], writes=['cK'], slot='cK')
                    P.dma('pool', cVt, cV_d[:, :, kvh, :], writes=['cV'], slot='cV')
                for lt in [1, 2, 3]:
                    if CUT == 7:
                        break
                    qc = (lt - 1) * 128
                    is_s = smp and lt == 3
                    vsl = slice(kvh * 128, (kvh + 1) * 128)
                    if not is_s:
                        for kt, ktile in enumerate([lt - 1, lt]):
                            for c in range(2):
                                bank = kt * 2 + c
                                hs = slice(c * 64, (c + 1) * 64)
                                P.mm(ps[bank][:, :].rearrange("p (a b) -> p a b", a=4),
                                     KT[:, kvh, ktile * 128:(ktile + 1) * 128], QTz[:, c, :, qc:qc + 128], True, True,
                                     reads=['KT', 'QT'], writes=[psk(bank)])
                                if CUT == 8:
                                    continue
                                P.actf(Ef[:, bank * 512:(bank + 1) * 512], ps[bank][:, :], AF.Exp, scale=0.125,
                                       reads=[psk(bank)], writes=[('Ef', bank)])
                            if CUT in (8, 9):
                                continue
                            midx = MC if kt == 1 else (MFP if (g == 0 and lt == 1) else MP)
                            ev = Eb[:, kt * 1024:(kt + 1) * 1024].rearrange("p (a b) -> p a b", a=8)
                            efv = Ef[:, kt * 1024:(kt + 1) * 1024].rearrange("p (a b) -> p a b", a=8)
                            P.tt('dve', ev, efv, cmat[:, midx, :].unsqueeze(1).to_broadcast([128, 8, 128]), ALU.mult,
                                 reads=[('Ef', kt * 2), ('Ef', kt * 2 + 1), 'cmat'], writes=[('Eb', kt * 2), ('Eb', kt * 2 + 1)])
                        for c in range(2):
                            if CUT in (8, 9, 10):
                                continue
                            P.mm(ps[6 + c][:, :], ones_b[:], Eb[:, c * 512:(c + 1) * 512], True, False,
                                 reads=[('Eb', c), 'ones_b'], writes=[psk(6 + c)])
                            P.mm(ps[6 + c][:, :], ones_b[:], Eb[:, (2 + c) * 512:(3 + c) * 512], False, True,
                                 reads=[('Eb', 2 + c)], writes=[psk(6 + c)])
                            P.mm(ps[4 + c][:, :], Vtok[:, lt - 1, vsl], Eb[:, c * 512:(c + 1) * 512], True, False,
                                 reads=[('Eb', c), ('Vtok', lt - 1 if not is_s else 3)], writes=[psk(4 + c)])
                            P.mm(ps[4 + c][:, :], Vtok[:, lt, vsl], Eb[:, (2 + c) * 512:(3 + c) * 512], False, True,
                                 reads=[('Eb', 2 + c), ('Vtok', lt)], writes=[psk(4 + c)])
                    else:
                        for c in range(2):
                            hs = slice(c * 64, (c + 1) * 64)
                            P.mm(ps[c][:, :].rearrange("p (a b) -> p a b", a=4), KT[:, kvh, 384:512], QTz[:, c, :, qc:qc + 128],
                                 True, True, reads=['KT', 'QT'], writes=[psk(c)])
                            P.actf(Ef[:, c * 512:(c + 1) * 512], ps[c][:, :], AF.Exp, scale=0.125,
                                   reads=[psk(c)], writes=[('Ef', c)])
                        ev = Eb[:, 0:1024].rearrange("p (a b) -> p a b", a=8)
                        efv = Ef[:, 0:1024].rearrange("p (a b) -> p a b", a=8)
                        P.tt('dve', ev, efv, cmat[:, MSN, :].unsqueeze(1).to_broadcast([128, 8, 128]), ALU.mult,
                             reads=[('Ef', 0), ('Ef', 1), 'cmat'], writes=[('Eb', 0), ('Eb', 1)])
                        for b in range(16):
                            for c in range(2):
                                hs = slice(c * 64, (c + 1) * 64)
                                off = ((b % 8) * 2 + c) * 32
                                P.mm(ps[2 + b // 8][:, off:off + 32].rearrange("p (a t) -> p a t", a=4), cK[:, b, :],
                                     QTz[:, c, :, qc + b * 8:qc + b * 8 + 8], True, True,
                                     reads=['cK', 'QT'], writes=[psk(2 + b // 8)])
                        for hb in range(2):
                            P.actf(Ef[:, 1024 + hb * 512:1024 + (hb + 1) * 512], ps[2 + hb][:, :], AF.Exp, scale=0.125,
                                   reads=[psk(2 + hb)], writes=[('Ef', 2 + hb)])
                        ecv = Ec.rearrange("p (a t) -> p a t", t=8)
                        efv = Ef[:, 1024:2048].rearrange("p (a t) -> p a t", t=8)
                        P.tt('dve', ecv, efv, cmat[:, MSC, 0:8].unsqueeze(1).to_broadcast([128, 128, 8]), ALU.mult,
                             reads=[('Ef', 2), ('Ef', 3), 'cmat'], writes=['Ec'])
                        Ec4 = Ec.rearrange("p (b c a t) -> p b c a t", b=16, c=2, a=4)
                        for c in range(2):
                            pperm = lambda bk: ps[bk][:, :].rearrange("p (a b t) -> p b a t", a=4, b=16)
                            P.mm(ps[6 + c][:, :], ones_b[:], Eb[:, c * 512:(c + 1) * 512], True, False,
                                 reads=[('Eb', c), 'ones_b'], writes=[psk(6 + c)])
                            P.mm(pperm(6 + c), ones_b[:], Ec4[:, :, c, :, :], False, True,
                                 reads=['Ec'], writes=[psk(6 + c)])
                            P.mm(ps[4 + c][:, :], Vtok[:, 3, vsl], Eb[:, c * 512:(c + 1) * 512], True, False,
                                 reads=[('Eb', c), ('Vtok', lt - 1 if not is_s else 3)], writes=[psk(4 + c)])
                            for b in range(16):
                                P.mm(pperm(4 + c)[:, b, :, :], cVt[:, b, :], Ec4[:, b, c, :, :], False, b == 15,
                                     reads=['Ec', 'cV'], writes=[psk(4 + c)])
                    for c in range(2):
                        if CUT in (8, 9, 10, 11):
                            continue
                        hs = slice(c * 64, (c + 1) * 64)
                        rv = Rr[hs, c, :].rearrange("p (a b) -> p a b", a=4)
                        sk = sinkE[hs, c * 16 + kvh * 4: c * 16 + kvh * 4 + 4].unsqueeze(2).to_broadcast([64, 4, 128])
                        P.tt('dve', rv, ps[6 + c][hs, :].rearrange("p (a b) -> p a b", a=4), sk, ALU.add,
                             reads=[psk(6 + c), 'sinkE'], writes=['Rr'])
                        P.op('dve', lambda h, rv=rv: h.reciprocal(rv, rv), reads=['Rr'], writes=['Rr'])
                        P.tt('dve', mixT[hs, kvh * 4:(kvh + 1) * 4, qc:qc + 128],
                             ps[4 + c][hs, :].rearrange("p (a b) -> p a b", a=4), rv, ALU.mult,
                             reads=[psk(4 + c), 'Rr'], writes=['mixT'])

            if CUT in (7, 8, 9, 10, 11, 12):
                P.barrier()
                continue
            cv.off = off_pool
            Utok = cv.get([128, 4, 512], BF16)
            dT = cv.get([128, 4, 384], BF16)
            SPt = cv.get([128, 2, 512], BF16)
            for g4 in range(4 if stages >= 3 else (4 if stages >= 2 else 0)):
                def evac_u(lt, bank):
                    P.copy('dve', Utok[:, lt, :], ps[bank][:, :], reads=[psk(bank)], writes=[('Utok', lt)])
                    if smp and lt >= 2:
                        f = stf[0]
                        P.copy('dve', f, ps[bank][:, :], reads=[psk(bank)], writes=['stf'])
                        if lt == 2:
                            P.dma('sp', npp_d[:, g4 * 512:(g4 + 1) * 512], f[113:128, :], reads=['stf'], slot='o_misc', is_out=True)
                        else:
                            for b in range(16):
                                P.dma('sp', nps_d[b, 7:15, g4 * 512:(g4 + 1) * 512], f[b * 8:(b + 1) * 8, :], reads=['stf'],
                                      slot='o_misc', is_out=True)
                inproj(6 + g4, tiles_all, evac_u)
                if stages < 3:
                    continue
                Wp, wpk = wload(poolw_d[:, g4], [128, 4, 512])
                if smp:
                    P.dma('pool', SPt[0:120], sptm_d[:, :, g4 * 512:(g4 + 1) * 512], writes=['SPt'], slot='SPt')
                for lt in [1, 2, 3]:
                    bank = nextbank(0, 2)
                    first = (g == 0 and lt == 1)
                    for cc in range(4):
                        o = ps[bank][:, cc * 128:(cc + 1) * 128]
                        csl = slice(cc * 128, (cc + 1) * 128)
                        if smp and lt == 3:
                            P.mm(o, SPt[0:120, 0, csl], cmat[0:120, BSA + g4, :], True, False, reads=['SPt', 'cmat'], writes=[psk(bank)])
                            P.mm(o, SPt[0:120, 1, csl], cmat[0:120, BSB + g4, :], False, False, reads=['SPt'], writes=[psk(bank)])
                            P.mm(o, Utok[:, 3, csl], cmat[:, BSM + g4, :], False, True, reads=[('Utok', 3)], writes=[psk(bank)])
                        else:
                            P.mm(o, Utok[:, lt - 1, csl], cmat[:, (BPF if first else BP) + g4, :], True, False,
                                 reads=[('Utok', lt - 1), 'cmat'], writes=[psk(bank)])
                            P.mm(o, Utok[:, lt, csl], cmat[:, (BCF if first else BC) + g4, :], False, True,
                                 reads=[('Utok', lt)], writes=[psk(bank)])
                    P.copy('dve', dT[:, :, (lt - 1) * 128:lt * 128], ps[bank][:, :].rearrange("p (a b) -> p a b", a=4),
                           reads=[psk(bank)], writes=['dT'])
                for dc in range(4):
                    bank = nextbank(2, 4)
                    for cc in range(4):
                        P.mm(ps[bank][:, 0:384], Wp[:, cc, dc * 128:(dc + 1) * 128], dT[:, cc, :], cc == 0, cc == 3,
                             reads=['dT', wpk], writes=[psk(bank)])
                    fcm = 16 + g4 * 4 + dc
                    P.ts('dve', mixT[:, fcm, :], ps[bank][:, 0:384], pscT[:, g4 * 4 + dc:g4 * 4 + dc + 1], None, ALU.mult,
                         reads=[psk(bank), 'vecs'], writes=['mixT'])
            if stages < 4:
                P.barrier()
                continue

            P.barrier()
            z = big[:, :].rearrange("p (a b) -> p a b", a=32)
            h2T = R2[:, :].rearrange("p (a b) -> p a b", a=32)
            cv = Carver(S2)
            sqt = [cv.get([128, 384], F32) for _ in range(2)]
            tmpz = cv.get([128, 128], F32)
            oc0 = 3 * g * 128
            for kq in range(8):
                P.dma('sp', z[:, kq * 4:(kq + 1) * 4, :], xv[:, kq * 4:(kq + 1) * 4, c0 + 128:c0 + 512],
                      writes=[('z', kq * 4 + j) for j in range(4)], slot=('zl', kq))
                for j in range(4):
                    fc = kq * 4 + j
                    P.op('act', lambda h, fc=fc: h.mul(z[:, fc, :], z[:, fc, :], ALPHA), reads=[('z', fc)], writes=[('z', fc)])

            def z_accum(fc, src, gbase, srckey):
                zk = ('z', fc)
                P.stt('dve', z[:, fc, 0:ncp - 128], src[:, 0:ncp - 128], modP[:, gbase + fc:gbase + fc + 1], z[:, fc, 0:ncp - 128],
                      ALU.mult, ALU.add, reads=[srckey, zk, 'mod'], writes=[zk])
                if smp:
                    tv = tmpz.rearrange("p (b t) -> p b t", b=16)
                    P.tt('dve', tv, src[:, 256:384].rearrange("p (b t) -> p b t", b=16), bc_s(modS[:, gbase + fc, :]), ALU.mult,
                         reads=[srckey, 'mod'], writes=['tmpz'])
                    P.tt('dve', z[:, fc, 256:384], z[:, fc, 256:384], tmpz, ALU.add, reads=['tmpz', zk], writes=[zk])
                sq = sqt[fc % 2]
                P.actf(sq, z[:, fc, :], AF.Square, reads=[zk], writes=[('sqt', fc % 2)])
                P.mm(ps[6][:, 0:384], ones_f[:], z[:, fc, :], fc == 0, fc == 31, reads=[zk, 'ones_f'], writes=[psk(6)])
                P.mm(ps[7][:, 0:384], ones_f[:], sq, fc == 0, fc == 31, reads=[('sqt', fc % 2)], writes=[psk(7)])

            for wt in range(16):
                a, k = wload(wout_v[:, :, wt * 256:(wt + 1) * 256], [128, 32, 256])
                for j in range(2):
                    fc = wt * 2 + j
                    bank = nextbank(0, 4)
                    for kc in range(32):
                        P.mm(ps[bank][:, 0:384], a[:, kc, j * 128:(j + 1) * 128], mixT[:, kc, :], kc == 0, kc == 31,
                             reads=['mixT', k], writes=[psk(bank)])
                    z_accum(fc, ps[bank], 64, psk(bank))

            def post1(fc):
                zk = ('z', fc)
                P.actf(h2T[:, fc, 0:ncp - 128], z[:, fc, 0:ncp - 128], AF.Identity, bias=modP[:, 96 + fc:97 + fc],
                       scale=modP[:, 128 + fc:129 + fc], reads=[zk, 'mod'], writes=[('h2T', fc)])
                if smp:
                    tv = tmpz.rearrange("p (b t) -> p b t", b=16)
                    P.tt('dve', tv, z[:, fc, 256:384].rearrange("p (b t) -> p b t", b=16), bc_s(modS[:, 128 + fc, :]), ALU.mult,
                         reads=[zk, 'mod'], writes=['tmpz'])
                    P.tt('dve', h2T[:, fc, 256:384].rearrange("p (b t) -> p b t", b=16), tv, bc_s(modS[:, 96 + fc, :]), ALU.add,
                         reads=['tmpz', 'mod'], writes=[('h2T', fc)])
                P.op('act', lambda h: h.mul(z[:, fc, :], z[:, fc, :], ALPHA), reads=[zk], writes=[zk])

            def stash1(fc0):
                P.dma('sp', yv[:, fc0:fc0 + 4, oc0:oc0 + 384], z[:, fc0:fc0 + 4, :], reads=[('z', fc0 + j) for j in range(4)],
                      writes=[('yst', fc0)], slot=('yst', fc0))
            ln_block(z, ln1g, ln1b, g, post1, stash1)
            P.barrier()
            if stages < 5:
                continue

            y2T = big[:, :].rearrange("p (a b) -> p a b", a=32)
            cv = Carver(S2)
            s_sb = cv.get([128, 3, 16, 128], F32)
            tau = cv.get([128, 3, 8], F32)
            off_tmp = cv.off
            qT = cv.get([128, 16, 384], BF16)
            t16 = cv.get([128, 16, 16], F32)
            wk = cv.get([128, 256], F32)
            cand = cv.get([128, 8, 256], F32)
            b16 = cv.get([128, 8, 16], F32)
            smal = cv.get([128, 8, 8], F32)
            for wt in range(8):
                a, k = wload(wq_v[:, :, wt * 256:(wt + 1) * 256], [128, 32, 256])
                for j in range(2):
                    pc = wt * 2 + j
                    bank = nextbank(0, 4)
                    for kc in range(32):
                        P.mm(ps[bank][:, 0:384], a[:, kc, j * 128:(j + 1) * 128], h2T[:, kc, :], kc == 0, kc == 31,
                             reads=[('h2T', kc), k], writes=[psk(bank)])
                    P.copy('dve', qT[:, pc, :], ps[bank][:, 0:384], reads=[psk(bank)], writes=['qT'])
            subk, subk_key = wload(subk_d, [128, 16, 128])
            for t in range(3):
                for pc in range(16):
                    bk = 4 + pc // 4
                    P.mm(ps[bk][:, (pc % 4) * 128:(pc % 4 + 1) * 128], qT[:, pc, t * 128:(t + 1) * 128], subk[:, pc, :], True, True,
                         reads=['qT', subk_key], writes=[psk(bk)])
                for q4 in range(4):
                    P.copy('act' if q4 % 2 else 'dve', s_sb[:, t, q4 * 4:(q4 + 1) * 4, :],
                           ps[4 + q4][:, :].rearrange("p (a b) -> p a b", a=4), reads=[psk(4 + q4)], writes=['s_sb'])
                for pc in range(16):
                    P.op('dve', lambda h, pc=pc, t=t: h.max(t16[:, pc, 0:8], s_sb[:, t, pc, :]), reads=['s_sb'], writes=['t16'])
                    P.op('dve', lambda h, pc=pc, t=t: h.match_replace(wk[:, 0:128], t16[:, pc, 0:8], s_sb[:, t, pc, :], -1e30),
                         reads=['s_sb', 't16'], writes=['wk'])
                    P.op('dve', lambda h, pc=pc: h.max(t16[:, pc, 8:16], wk[:, 0:128]), reads=['wk'], writes=['t16'])
                t16v = t16.rearrange("p (h q) k -> p h q k", q=2)
                s_v = s_sb[:, t].rearrange("p (h q) k -> p h q k", q=2)

                def cand_top(tag):
                    P.tt('dve', cand.rearrange("p h (i j) -> p h i j", i=16),
                         t16v[:, :, 0, :].unsqueeze(3).to_broadcast([128, 8, 16, 16]),
                         t16v[:, :, 1, :].unsqueeze(2).to_broadcast([128, 8, 16, 16]), ALU.add, reads=['t16'], writes=['cand'])
                    for hh in range(8):
                        P.op('dve', lambda h, hh=hh: h.max(b16[:, hh, 0:8], cand[:, hh, :]), reads=['cand'], writes=['b16'])
                        P.op('dve', lambda h, hh=hh: h.match_replace(wk[:, :], b16[:, hh, 0:8], cand[:, hh, :], -1e30),
                             reads=['cand', 'b16'], writes=['wk'])
                        P.op('dve', lambda h, hh=hh: h.max(b16[:, hh, 8:16], wk[:, :]), reads=['wk'], writes=['b16'])
                cand_top(0)
                mcol = smal[:, 0, :]
                zcol = smal[:, 1, :]
                P.copy('dve', mcol, b16[:, :, 0], reads=['b16'], writes=['smal'])
                P.tt('dve', b16[:, :, :], b16[:, :, :], mcol.unsqueeze(2).to_broadcast([128, 8, 16]), ALU.subtract,
                     reads=['b16', 'smal'], writes=['b16'])
                P.actf(b16[:, :, :], b16[:, :, :], AF.Exp, reads=['b16'], writes=['b16'])
                P.op('dve', lambda h: h.reduce_sum(zcol, b16[:, :, :], AX.X), reads=['b16'], writes=['smal'])
                P.actf(zcol, zcol, AF.Ln, reads=['smal'], writes=['smal'])
                P.tt('dve', zcol, zcol, mcol, ALU.add, reads=['smal'], writes=['smal'])
                P.tt('dve', s_v[:, :, 1, :], s_v[:, :, 1, :], zcol.unsqueeze(2).to_broadcast([128, 8, 128]), ALU.subtract,
                     reads=['s_sb', 'smal'], writes=['s_sb'])
                P.tt('dve', t16v[:, :, 1, :], t16v[:, :, 1, :], zcol.unsqueeze(2).to_broadcast([128, 8, 16]), ALU.subtract,
                     reads=['t16', 'smal'], writes=['t16'])
                cand_top(1)
                P.copy('dve', tau[:, t, :], b16[:, :, 15], reads=['b16'], writes=['tau'])
            cv.off = off_tmp
            P.barrier()
            actT = [cv.get([128, 8, 384], BF16) for _ in range(2)]
            gel = [cv.get([128, 384], F32) for _ in range(2)]
            valb = [cv.get([128, 8, 128], F32) for _ in range(2)]
            Ebf = [cv.get([128, 8, 128], BF16) for _ in range(2)]
            maskb = [cv.get([128, 8, 128], BF16) for _ in range(2)]
            Gb = Ebf
            Uslot = [W[0][:, :].rearrange("p (a b) -> p a b", a=32), W[1][:, :].rearrange("p (a b) -> p a b", a=32)]
            Vslot = [W[2][:, 0:4096].rearrange("p (a b) -> p a b", a=8), W[2][:, 4096:8192].rearrange("p (a b) -> p a b", a=8)]
            NCH = 128
            ucount = [0]
            pend = [None]
            finp = [None]

            def stage1(i, t, sl):
                s_v = s_sb[:, t].rearrange("p (h q) k -> p h q k", q=2)
                if t == 2:
                    for hh in range(8):
                        P.actf(valb[sl][:, hh, :], s_v[:, hh, 1, :], AF.Identity, bias=s_v[:, hh, 0, i:i + 1],
                               reads=['s_sb'], writes=[('valb', sl)])
                else:
                    P.tt('dve', valb[sl], s_v[:, :, 1, :], s_v[:, :, 0, i:i + 1].to_broadcast([128, 8, 128]), ALU.add,
                         reads=['s_sb'], writes=[('valb', sl)])
                P.actf(Ebf[sl], valb[sl], AF.Exp, reads=[('valb', sl)], writes=[('Ebf', sl)])

            def stage2(i, t, sl):
                gb = 2 + i % 2
                P.tt('dve', maskb[sl], valb[sl], tau[:, t, :].unsqueeze(2).to_broadcast([128, 8, 128]), ALU.is_ge,
                     reads=[('valb', sl), 'tau'], writes=[('maskb', sl)])
                P.tt('dve', Gb[sl], maskb[sl], Ebf[sl], ALU.mult, reads=[('maskb', sl), ('Ebf', sl)], writes=[('Ebf', sl)])
                for hh in range(8):
                    P.mm(ps[gb][:, t * 128:(t + 1) * 128], Gb[sl][:, hh, :], ident, hh == 0, hh == 7,
                         reads=[('Ebf', sl), 'cmat'], writes=[psk(gb)])

            def finalize(i):
                EB, ci = i // 8, i % 8
                P.tt('dve', actT[EB % 2][:, ci, :], ps[2 + i % 2][:, 0:384], gel[i % 2], ALU.mult,
                     reads=[psk(2 + i % 2), ('gel', i % 2)], writes=[('actT', EB % 2, ci)])

            def step_unit(i, t):
                sl = ucount[0] % 2
                ucount[0] += 1
                stage1(i, t, sl)
                if finp[0] is not None and t == 2:
                    finalize(finp[0])
                    finp[0] = None
                if pend[0] is not None:
                    pi, pt, psl = pend[0]
                    stage2(pi, pt, psl)
                    if pt == 2:
                        finp[0] = pi
                pend[0] = (i, t, sl)
                emit_one_add()

            def flush_units():
                if finp[0] is not None:
                    finalize(finp[0])
                    finp[0] = None
                if pend[0] is not None:
                    pi, pt, psl = pend[0]
                    stage2(pi, pt, psl)
                    if pt == 2:
                        finalize(pi)
                    pend[0] = None

            vcount = [0]
            ypend = []

            def emit_one_add():
                if not ypend:
                    return
                fc, yb, first = ypend.pop(0)
                if first:
                    P.copy('dve', y2T[:, fc, :], ps[yb][:, 0:384], reads=[psk(yb)], writes=[('z', fc)])
                else:
                    P.tt('dve', y2T[:, fc, :], y2T[:, fc, :], ps[yb][:, 0:384], ALU.add, reads=[psk(yb), ('z', fc)],
                         writes=[('z', fc)])

            def vpart(v):
                EB, fq = v // 8, v % 8
                while ypend:
                    emit_one_add()
                vs = vcount[0] % 2
                vcount[0] += 1
                va = Vslot[vs]
                P.dma('pool', va, V_v[:, EB * 8:(EB + 1) * 8, fq * 512:(fq + 1) * 512], writes=[('Wv', vs)], slot=('Wv', vs))
                for fj in range(4):
                    fc = fq * 4 + fj
                    yb = 4 + fj
                    for ci in range(8):
                        P.mm(ps[yb][:, 0:384], va[:, ci, fj * 128:(fj + 1) * 128], actT[EB % 2][:, ci, :], ci == 0, ci == 7,
                             reads=[('actT', EB % 2, ci), ('Wv', vs)], writes=[psk(yb)])
                    ypend.append((fc, yb, EB == 0))

            VLAG = 9
            abank = [0, 1]
            for i in range(NCH):
                if i % 2 == 0:
                    us = (i // 2) % 2
                    P.dma('pool', Uslot[us], UT_v[:, :, i * 128:(i + 2) * 128], writes=[('W', us)], slot=('W', us))
                us = (i // 2) % 2
                j = i % 2
                ab = nextbank(0, 2)
                for kc in range(32):
                    P.mm(ps[ab][:, 0:384], Uslot[us][:, kc, j * 128:(j + 1) * 128], h2T[:, kc, :], kc == 0, kc == 31,
                         reads=[('h2T', kc), ('W', us)], writes=[psk(ab)])
                abank[i % 2] = ab
                if i % 2 == 1:
                    for ii in (i - 1, i):
                        P.actf(gel[ii % 2], ps[abank[ii % 2]][:, 0:384], AF.Gelu, reads=[psk(abank[ii % 2])], writes=[('gel', ii % 2)])
                if i >= VLAG:
                    vpart(i - VLAG)
                for t in range(3):
                    step_unit(i, t)
            flush_units()
            for v in range(NCH - VLAG, NCH):
                vpart(v)
            while ypend:
                emit_one_add()
            P.barrier()
            cv = Carver(S2)
            sqt = [cv.get([128, 384], F32) for _ in range(2)]
            tmpz = cv.get([128, 128], F32)
            xst = [cv.get([128, 4, 384], F32) for _ in range(2)]
            for kq in range(8):
                sl = kq % 2
                P.dma('sp', xst[sl], yv[:, kq * 4:(kq + 1) * 4, oc0:oc0 + 384], reads=[('yst', kq * 4)], writes=[('xst', sl)],
                      slot=('xst', sl))
                for j in range(4):
                    fc = kq * 4 + j
                    zk = ('z', fc)
                    P.stt('dve', y2T[:, fc, 0:ncp - 128], y2T[:, fc, 0:ncp - 128], modP[:, 160 + fc:161 + fc], xst[sl][:, j, 0:ncp - 128],
                          ALU.mult, ALU.add, reads=[zk, ('xst', sl), 'mod'], writes=[zk])
                    if smp:
                        tv = tmpz.rearrange("p (b t) -> p b t", b=16)
                        P.tt('dve', tv, y2T[:, fc, 256:384].rearrange("p (b t) -> p b t", b=16), bc_s(modS[:, 160 + fc, :]), ALU.mult,
                             reads=[zk, 'mod'], writes=['tmpz'])
                        P.tt('dve', y2T[:, fc, 256:384], tmpz, xst[sl][:, j, 256:384], ALU.add, reads=['tmpz', ('xst', sl)], writes=[zk])
                    sq = sqt[fc % 2]
                    P.actf(sq, y2T[:, fc, :], AF.Square, reads=[zk], writes=[('sqt', fc % 2)])
                    P.mm(ps[6][:, 0:384], ones_f[:], y2T[:, fc, :], fc == 0, fc == 31, reads=[zk, 'ones_f'], writes=[psk(6)])
                    P.mm(ps[7][:, 0:384], ones_f[:], sq, fc == 0, fc == 31, reads=[('sqt', fc % 2)], writes=[psk(7)])

            def stash2(fc0):
                P.dma('sp', yv[:, fc0:fc0 + 4, oc0:oc0 + 384], y2T[:, fc0:fc0 + 4, :], reads=[('z', fc0 + j) for j in range(4)],
                      writes=[('yst', fc0)], slot='o_y', is_out=True)
            ln_block(y2T, ln2g, ln2b, g, None, stash2)
            P.barrier()
        P.emit()
    return nc


def _bf(x):
    return np.ascontiguousarray(x, dtype=np.float32)


def _consts(hf):
    cm = np.zeros((NMAT, 128, 128), np.float32)
    k = np.arange(128)[:, None]
    q = np.arange(128)[None, :]
    cm[MP] = (k >= q)
    cm[MC] = (k <= q)
    cm[MFP] = cm[MP] if hf == 1 else 0.0
    cm[MSN] = ((k // 8) == (q // 8)) & ((k % 8) <= (q % 8))
    cm[IDM] = np.eye(128)
    cm[MSC, :, 0:8] = (np.arange(128)[:, None] >= np.arange(8)[None, :])
    for g4, w in enumerate((2, 4, 8, 16)):
        tp = np.arange(128)[:, None]
        t = np.arange(128)[None, :]
        cur = ((tp <= t) & (tp > t - w)).astype(np.float32)
        prv = (tp - 128 > t - w).astype(np.float32)
        cm[BP + g4] = prv / w
        cm[BC + g4] = cur / w - np.eye(128)
        if hf == 1:
            cm[BPF + g4] = cm[BP + g4]
            cm[BCF + g4] = cm[BC + g4]
        else:
            cnt = np.minimum(w, t + 1).astype(np.float32)
            cm[BPF + g4] = 0.0
            cm[BCF + g4] = cur / cnt - np.eye(128)
        bq, tq = np.arange(128)[None, :] // 8, np.arange(128)[None, :] % 8
        r_b, r_r = np.arange(120)[:, None] // 15, np.arange(120)[:, None] % 15
        for half, idx in ((0, BSA), (1, BSB)):
            m = ((r_b + 8 * half) == bq) & (r_r > 15 + tq - w)
            cm[idx + g4, 0:120] = m / w
        bk, tk = np.arange(128)[:, None] // 8, np.arange(128)[:, None] % 8
        m = (bk == bq) & (tk <= tq) & (tk > tq - w)
        cm[BSM + g4] = m / w - np.eye(128)
    return np.ascontiguousarray(cm.transpose(1, 0, 2))


def _rope_tab(hf):
    half = 8
    inv = (np.float32(500000.0) ** (-np.arange(half, dtype=np.float32) / np.float32(half))).astype(np.float32)
    tab = np.zeros((128, 10, 2, 8), np.float32)
    for gt in range(10):
        if gt < 9:
            pos = hf * 1024 + (gt - 1) * 128 + np.arange(128)
        else:
            pos = 8192 + (np.arange(128) % 8)
        ang = pos.astype(np.float32)[:, None] * inv[None, :]
        tab[:, gt, 0, :] = np.cos(ang)
        tab[:, gt, 1, :] = np.sin(ang)
    return tab


_NC_CACHE = {}


def _prep(x_prompt, x_sample, cache_k, cache_v, state_pool, c_prompt, c_sample, w_ada, b_ada, w_in,
          sinks, pool_w, pool_scale, w_out, ln1_g, ln1_b, peer_wq, peer_subkeys, peer_u, peer_v,
          ln2_g, ln2_b):
    f = lambda a: np.asarray(a, dtype=np.float32)
    x_prompt, x_sample, cache_k, cache_v, state_pool = map(f, (x_prompt, x_sample, cache_k, cache_v, state_pool))
    c_prompt, c_sample = f(c_prompt), f(c_sample)
    wi = f(w_in)[0]
    kcol = wi[:, 2048:2304].reshape(D, 4, 1, 64)
    vcol = wi[:, 2304:2560].reshape(D, 4, 1, 64)
    win_r = np.concatenate([np.broadcast_to(kcol, (D, 4, 2, 64)).reshape(D, 512),
                            np.broadcast_to(vcol, (D, 4, 2, 64)).reshape(D, 512),
                            wi[:, 0:2048], wi[:, 2560:4608]], axis=1)
    vecs = np.zeros((128, 384), np.float32)
    vecs[:, 0:192] = f(b_ada)[0].reshape(192, 128).T
    vecs[:, 192:208] = f(pool_scale)[0].reshape(16, 128).T
    for i, a in enumerate((ln1_g, ln1_b, ln2_g, ln2_b)):
        vecs[:, 208 + 32 * i:240 + 32 * i] = f(a)[0].reshape(32, 128).T
    vecs[:, 336:368] = np.broadcast_to(f(sinks)[0].reshape(16, 2).T.reshape(1, 32), (128, 32))
    common = {
        "w_ada": _bf(f(w_ada)[0]), "w_in": _bf(win_r), "w_out": _bf(f(w_out)[0]), "wq": _bf(f(peer_wq)[0]),
        "subk": _bf(f(peer_subkeys)[0].transpose(3, 0, 1, 2).reshape(128, 16, 128)),
        "UT": _bf(f(peer_u)[0].T), "V": _bf(f(peer_v)[0]),
        "poolw": _bf(f(pool_w)[0].reshape(4, 4, 128, 512).transpose(2, 0, 1, 3)),
        "vecs": vecs,
    }
    in_maps = []
    for r in range(8):
        b, hf = r // 2, r % 2
        xs = x_sample[16 * r:16 * r + 16].reshape(128, D)
        t0 = hf * 1024
        halo = x_prompt[b, t0 - 128:t0] if hf == 1 else np.zeros((128, D), np.float32)
        xT = np.concatenate([halo, x_prompt[b, t0:t0 + 1024], xs], axis=0).T
        cc = np.concatenate([c_prompt[b:b + 1], c_sample[16 * r:16 * r + 16]], axis=0)
        ck = cache_k[0, 16 * r:16 * r + 16]
        cvv = cache_v[0, 16 * r:16 * r + 16]
        ckt = ck.transpose(3, 0, 2, 1)
        cvt = cvv.transpose(1, 0, 2, 3)
        sp = state_pool[0, 16 * r:16 * r + 16]
        m = dict(common)
        m.update({
            "xT": _bf(xT),
            "cT": _bf(cc.T.reshape(32, 128, 17).transpose(1, 0, 2)),
            "cmat": _consts(hf), "rope": _rope_tab(hf),
            "cKT": _bf(np.concatenate([ckt, ckt], axis=0)),
            "cV": _bf(np.concatenate([cvt, cvt], axis=3)),
            "ckn": _bf(ck.reshape(16, 128, 256)), "cvn": _bf(cvv.reshape(16, 128, 256)),
            "sptm": _bf(sp.reshape(2, 120, 2048).transpose(1, 0, 2)), "spn": _bf(sp),
        })
        in_maps.append(m)
    return in_maps


def _assemble(res, cores=tuple(range(8))):
    y_p = np.zeros((4, 2048, D), np.float32)
    y_s = np.zeros((128, 8, D), np.float32)
    nkp = np.zeros((1, 4, 128, 4, 64), np.float32)
    nvp = np.zeros_like(nkp)
    npp = np.zeros((1, 4, 15, 2048), np.float32)
    nks = np.zeros((1, 128, 128, 4, 64), np.float32)
    nvs = np.zeros_like(nks)
    nps = np.zeros((1, 128, 15, 2048), np.float32)
    for r in cores:
        b, hf = r // 2, r % 2
        o = res[cores.index(r)]
        yT = o["yT"]
        y_p[b, hf * 1024:(hf + 1) * 1024] = yT[:, 0:1024].T
        y_s[16 * r:16 * r + 16] = yT[:, 1024:1152].T.reshape(16, 8, D)
        if hf == 1:
            nkp[0, b] = o["nkp"].reshape(128, 4, 64)
            nvp[0, b] = o["nvp"].reshape(128, 4, 64)
            npp[0, b] = o["npp"]
        nks[0, 16 * r:16 * r + 16] = o["nks"].reshape(16, 128, 4, 64)
        nvs[0, 16 * r:16 * r + 16] = o["nvs"].reshape(16, 128, 4, 64)
        nps[0, 16 * r:16 * r + 16] = o["nps"]
    return (y_p, y_s, nkp, nvp, npp, nks, nvs, nps)


def kernel(**inputs):
    in_maps = _prep(**inputs)
    if 'nc' not in _NC_CACHE:
        _NC_CACHE['nc'] = build_nc()
    res = run_bass_kernel_spmd(_NC_CACHE['nc'], in_maps, core_ids=list(range(8))).results
    return _assemble(res)
```

```python
import numpy as np
from contextlib import ExitStack
import concourse.bass as bass
import concourse.mybir as mybir
from concourse.bass_utils import run_bass_kernel_spmd

F32 = mybir.dt.float32
BF16 = mybir.dt.bfloat16
ALU = mybir.AluOpType
AF = mybir.ActivationFunctionType
AX = mybir.AxisListType
ENG = ('pe', 'act', 'dve', 'pool', 'sp')


class Prog:
    def __init__(self, nc, es):
        self.nc = nc
        self.es = es
        self.ops = {e: [] for e in ENG}
        self.n = {e: 0 for e in ENG}
        self.seen = {e: {} for e in ENG}
        self.res = {}
        self.dcount = {}
        self.targets = {e: set() for e in ENG}
        self.out_slots = set()

    def _deps(self, reads, writes):
        d = []
        for k in reads:
            r = self.res.get(k)
            if r and r[0] is not None:
                d.append(r[0])
        for k in writes:
            r = self.res.get(k)
            if r:
                if r[0] is not None:
                    d.append(r[0])
                for sk, v in r[1].items():
                    d.append((sk[0], sk[1], v))
        return d

    def _waits(self, eng, deps, skip_same=False):
        for kind, key, val in deps:
            if kind == 'e' and key == eng and skip_same:
                continue
            if kind == 'd':
                val = self.dcount[key]
            sk = (kind, key)
            if self.seen[eng].get(sk, -1) >= val:
                continue
            self.seen[eng][sk] = val
            self.ops[eng].append(('w', kind, key, val))
            if kind == 'e':
                self.targets[key].add(val)

    def _update(self, me, reads, writes):
        for k in writes:
            self.res[k] = [me, {}]
        for k in reads:
            r = self.res.setdefault(k, [None, {}])
            sk = (me[0], me[1])
            if r[1].get(sk, -1) < me[2]:
                r[1][sk] = me[2]

    def op(self, eng, fn, reads=(), writes=(), skip_same=False):
        self._waits(eng, self._deps(reads, writes), skip_same)
        idx = self.n[eng]
        self.n[eng] += 1
        self.ops[eng].append(('i', fn, idx))
        self._update(('e', eng, idx), reads, writes)

    def dma(self, eng, out, in_, reads=(), writes=(), slot=None, is_out=False):
        self._waits(eng, self._deps(reads, writes))
        c = self.dcount.get(slot, 0) + 16
        self.dcount[slot] = c
        self.ops[eng].append(('d', out, in_, slot))
        self._update(('d', slot, c), reads, writes)
        if is_out:
            self.out_slots.add(slot)

    def barrier(self):
        for e in ENG:
            deps = []
            for f in ENG:
                if f != e and self.n[f] > 0:
                    deps.append(('e', f, self.n[f] - 1))
            for s, c in self.dcount.items():
                deps.append(('d', s, c))
            self._waits(e, deps)

    def mm(self, out, lhsT, rhs, start, stop, reads=(), writes=()):
        self.op('pe', lambda h: h.matmul(out, lhsT, rhs, start=start, stop=stop), reads, writes, skip_same=True)

    def tr(self, out, in_, ident, reads=(), writes=()):
        self.op('pe', lambda h: h.transpose(out, in_, ident), reads, writes, skip_same=True)

    def actf(self, out, in_, func, bias=None, scale=None, reads=(), writes=(), eng='act'):
        kw = {}
        if bias is not None:
            kw['bias'] = bias
        if scale is not None:
            kw['scale'] = scale
        self.op(eng, lambda h: h.activation(out, in_, func, **kw), reads, writes)

    def tt(self, eng, out, in0, in1, op, reads=(), writes=()):
        self.op(eng, lambda h: h.tensor_tensor(out, in0, in1, op), reads, writes)

    def ts(self, eng, out, in0, s1, s2, op0, op1=None, reads=(), writes=()):
        if op1 is None:
            self.op(eng, lambda h: h.tensor_scalar(out, in0, s1, None, op0), reads, writes)
        else:
            self.op(eng, lambda h: h.tensor_scalar(out, in0, s1, s2, op0, op1), reads, writes)

    def stt(self, eng, out, in0, scalar, in1, op0, op1, reads=(), writes=()):
        self.op(eng, lambda h: h.scalar_tensor_tensor(out, in0, scalar, in1, op0, op1), reads, writes)

    def copy(self, eng, out, in_, reads=(), writes=()):
        if eng == 'act':
            self.op(eng, lambda h: h.copy(out, in_), reads, writes)
        else:
            self.op(eng, lambda h: h.tensor_copy(out, in_), reads, writes)

    def emit(self):
        nc = self.nc
        es = self.es
        sem = {e: es.enter_context(nc.semaphore("sem_" + e)) for e in ENG}
        dsem = {}
        for i, s in enumerate(self.dcount):
            dsem[s] = es.enter_context(nc.semaphore("dsem%d" % i))
        rank = {e: {idx: i + 1 for i, idx in enumerate(sorted(self.targets[e]))} for e in ENG}
        for s in sorted(self.out_slots, key=str):
            self.ops['sp'].append(('w', 'd', s, self.dcount[s]))
        block = es.enter_context(nc.Block())

        def run(e, h):
            for rec in self.ops[e]:
                if rec[0] == 'w':
                    _, kind, key, val = rec
                    if kind == 'e':
                        h.wait_ge(sem[key], rank[key][val])
                    else:
                        h.wait_ge(dsem[key], val)
                elif rec[0] == 'i':
                    ins = rec[1](h)
                    if rec[2] in rank[e]:
                        ins.then_inc(sem[e], 1)
                else:
                    _, out, in_, slot = rec
                    h.dma_start(out=out, in_=in_).then_inc(dsem[slot], 16)

        block.tensor(lambda h: run('pe', h))
        block.scalar(lambda h: run('act', h))
        block.vector(lambda h: run('dve', h))
        block.gpsimd(lambda h: run('pool', h))
        block.sync(lambda h: run('sp', h))
        print("PROG ops:", {e: len(self.ops[e]) for e in ENG}, "dsems", len(dsem))


CUT = 99
D = 4096
ALPHA = 2.0 ** 0.25
LN_EPS = 1e-5
MP, MC, MFP, MSN, IDM, BP, BC, BPF, BCF, BSA, BSB, BSM, MSC = 0, 1, 2, 3, 4, 5, 9, 13, 17, 21, 25, 29, 33
NMAT = 34
STAGES = 99
VAL_ENG = 'act'


def build_nc(stages=STAGES, groups=(0, 1, 2), skipA=False):
    nc = bass.Bass("TRN2", target_bir_lowering=False)
    dt_in = lambda n, s: nc.dram_tensor(n, s, F32, kind="ExternalInput").ap()
    dt_out = lambda n, s: nc.dram_tensor(n, s, F32, kind="ExternalOutput").ap()
    xT_d = dt_in("xT", [D, 1280])
    cT_d = dt_in("cT", [128, 32, 17])
    cmat_d = dt_in("cmat", [128, NMAT, 128])
    rope_d = dt_in("rope", [128, 10, 2, 8])
    vecs_d = dt_in("vecs", [128, 384])
    cKT_d = dt_in("cKT", [128, 16, 4, 128])
    cV_d = dt_in("cV", [128, 16, 4, 128])
    ckn_d = dt_in("ckn", [16, 128, 256])
    cvn_d = dt_in("cvn", [16, 128, 256])
    sptm_d = dt_in("sptm", [120, 2, 2048])
    spn_d = dt_in("spn", [16, 15, 2048])
    wada_d = dt_in("w_ada", [D, 24576] if not skipA else [128, 128])
    win_d = dt_in("w_in", [D, 5120])
    wout_d = dt_in("w_out", [D, D] if stages >= 4 else [128, 128])
    wq_d = dt_in("wq", [D, 2048] if stages >= 5 else [128, 128])
    subk_d = dt_in("subk", [128, 16, 128])
    UT_d = dt_in("UT", [D, 16384] if stages >= 5 else [128, 128])
    V_d = dt_in("V", [16384, D] if stages >= 5 else [128, 128])
    poolw_d = dt_in("poolw", [128, 4, 4, 512])
    yT_d = dt_out("yT", [D, 1152])
    nkp_d = dt_out("nkp", [128, 256])
    nvp_d = dt_out("nvp", [128, 256])
    npp_d = dt_out("npp", [15, 2048])
    nks_d = dt_out("nks", [16, 128, 256])
    nvs_d = dt_out("nvs", [16, 128, 256])
    nps_d = dt_out("nps", [16, 15, 2048])

    es = ExitStack()
    with es:
        P = Prog(nc, es)
        sb = lambda name, shape, dt: es.enter_context(nc.sbuf_tensor("s_" + name, shape, dt))
        ps = [es.enter_context(nc.psum_tensor("ps%d" % i, [128, 512], F32)) for i in range(8)]
        psk = lambda i: ('ps', i)

        cmat = sb("cmat", [128, NMAT, 128], BF16)
        rope = sb("rope", [128, 10, 2, 8], F32)
        vecs = sb("vecs", [128, 384], F32)
        modP = sb("modP", [128, 192], F32)
        modS = sb("modS", [128, 192, 16], F32)
        cT = sb("cTs", [128, 32, 17], F32)
        siluT = sb("siluT", [128, 32, 17], BF16)
        sinkE = sb("sinkE", [128, 32], F32)
        ones_b = sb("ones_b", [128, 128], BF16)
        ones_f = sb("ones_f", [128, 128], F32)
        W = [sb("W%d" % i, [128, 8192], BF16) for i in range(3)]
        R2 = sb("R2", [128, 12288], BF16)
        big = sb("big", [128, 12288], F32)
        S2 = sb("S2", [128, 14848], F32)

        b_adaT = vecs[:, 0:192]
        pscT = vecs[:, 192:208]
        ln1g, ln1b, ln2g, ln2b = (vecs[:, 208 + 32 * i:240 + 32 * i] for i in range(4))
        ident = cmat[:, IDM, :]

        class Carver:
            def __init__(self, t):
                self.t = t
                self.off = 0

            def get(self, shape, dt):
                n = int(np.prod(shape[1:]))
                nb = n * (2 if dt == BF16 else 4)
                nw = (nb + 3) // 4
                a = self.t[:, self.off:self.off + nw]
                self.off += nw
                assert self.off <= 14848, self.off
                if dt == BF16:
                    a = a.bitcast(BF16)[:, 0:n]
                if len(shape) == 3:
                    a = a.rearrange("p (a b) -> p a b", a=shape[1])
                elif len(shape) == 4:
                    a = a.rearrange("p (a b c) -> p a b c", a=shape[1], b=shape[2])
                return a

        wslot = [0]

        def wload(src_ap, view_shape, key_extra=None):
            s = wslot[0] % 3
            wslot[0] += 1
            n = int(np.prod(view_shape[1:]))
            a = W[s][0:view_shape[0], 0:n]
            if len(view_shape) == 3:
                a = a.rearrange("p (a b) -> p a b", a=view_shape[1])
            P.dma('pool', a, src_ap, writes=[('W', s)], slot=('W', s))
            return a, ('W', s)

        P.dma('pool', cmat[:], cmat_d, writes=['cmat'], slot='c_cmat')
        P.dma('sp', rope[:], rope_d, writes=['rope'], slot='c_rope')
        P.dma('sp', vecs[:], vecs_d, writes=['vecs'], slot='c_vecs')
        P.dma('sp', cT[:], cT_d, writes=['cT'], slot='c_cT')
        P.op('dve', lambda h: h.memset(ones_b[:], 1.0), writes=['ones_b'])
        P.op('dve', lambda h: h.memset(ones_f[:], 1.0), writes=['ones_f'])
        P.actf(siluT[:], cT[:], AF.Silu, reads=['cT'], writes=['siluT'])
        P.actf(sinkE[:], vecs[:, 336:368], AF.Exp, reads=['vecs'], writes=['sinkE'])
        P.dma('sp', nks_d[:, 0:120, :], ckn_d[:, 8:128, :], slot='o_misc', is_out=True)
        P.dma('sp', nvs_d[:, 0:120, :], cvn_d[:, 8:128, :], slot='o_misc', is_out=True)
        P.dma('sp', nps_d[:, 0:7, :], spn_d[:, 8:15, :], slot='o_misc', is_out=True)

        wada_v = wada_d.rearrange("(kc p) c -> p kc c", p=128)
        if skipA:
            P.op('dve', lambda h: h.memset(modP[:], 0.25), writes=['mod'])
            P.op('dve', lambda h: h.memset(modS[:], 0.25), writes=['mod'])
        WA = [big[:, 0:8192].bitcast(BF16).rearrange("p (a b) -> p a b", a=32),
              S2[:, 0:8192].bitcast(BF16).rearrange("p (a b) -> p a b", a=32)]
        for cg in range(0 if skipA else 48):
            s = cg % 2
            P.dma('pool', WA[s], wada_v[:, :, cg * 512:(cg + 1) * 512], writes=[('WA', s)], slot=('WA', s))
            bank = cg % 2
            pv = ps[bank][:, 0:68].rearrange("p (j n) -> p j n", j=4)
            for j in range(4):
                for kc in range(32):
                    P.mm(pv[:, j, :], WA[s][:, kc, j * 128:(j + 1) * 128], siluT[:, kc, :], kc == 0, kc == 31,
                         reads=[('WA', s), 'siluT'], writes=[psk(bank)])
            m0 = cg * 4
            P.tt('dve', modP[:, m0:m0 + 4].unsqueeze(2), pv[:, :, 0:1], b_adaT[:, m0:m0 + 4].unsqueeze(2), ALU.add,
                 reads=[psk(bank), 'vecs'], writes=['mod'])
            P.tt('dve', modS[:, m0:m0 + 4, :], pv[:, :, 1:17],
                 b_adaT[:, m0:m0 + 4].unsqueeze(2).to_broadcast([128, 4, 16]), ALU.add,
                 reads=[psk(bank), 'vecs'], writes=['mod'])
        for m in (1, 4):
            P.ts('dve', modP[:, m * 32:(m + 1) * 32], modP[:, m * 32:(m + 1) * 32], 1.0, None, ALU.add,
                 reads=['mod'], writes=['mod'])
            P.ts('dve', modS[:, m * 32:(m + 1) * 32, :], modS[:, m * 32:(m + 1) * 32, :], 1.0, None, ALU.add,
                 reads=['mod'], writes=['mod'])
        P.barrier()

        xv = xT_d.rearrange("(kc p) t -> p kc t", p=128)
        yv = yT_d.rearrange("(kc p) t -> p kc t", p=128)
        win_v = win_d.rearrange("(kc p) c -> p kc c", p=128)
        wout_v = wout_d.rearrange("(kc p) c -> p kc c", p=128)
        wq_v = wq_d.rearrange("(kc p) c -> p kc c", p=128)
        UT_v = UT_d.rearrange("(kc p) c -> p kc c", p=128)
        V_v = V_d.rearrange("(c p) f -> p c f", p=128)

        def bc_s(ap_b16, n=8):
            return ap_b16.unsqueeze(2).to_broadcast([ap_b16.shape[0], 16, n])

        pbank = [0]

        def nextbank(lo, hi):
            b = lo + pbank[0] % (hi - lo)
            pbank[0] += 1
            return b

        def ln_block(z, gcol, bcol, g, mod_post, stash):
            cv = Carver(S2)
            cv.off = 11000
            mean = cv.get([128, 384], F32)
            rstd = cv.get([128, 384], F32)
            tmpn = cv.get([128, 384], F32)
            P.op('act', lambda h: h.mul(mean, ps[6][:, 0:384], 1.0 / D), reads=[psk(6)], writes=['mean'])
            P.op('act', lambda h: h.mul(rstd, ps[7][:, 0:384], 1.0 / D), reads=[psk(7)], writes=['rstd'])
            P.tt('dve', tmpn, mean, mean, ALU.mult, reads=['mean'], writes=['tmpn'])
            P.tt('dve', rstd, rstd, tmpn, ALU.subtract, reads=['rstd', 'tmpn'], writes=['rstd'])
            P.ts('dve', rstd, rstd, LN_EPS, None, ALU.add, reads=['rstd'], writes=['rstd'])
            P.actf(rstd, rstd, AF.Sqrt, reads=['rstd'], writes=['rstd'])
            P.op('dve', lambda h: h.reciprocal(rstd, rstd), reads=['rstd'], writes=['rstd'])
            for fc in range(32):
                zk = ('z', fc)
                P.tt('dve', z[:, fc, :], z[:, fc, :], mean, ALU.subtract, reads=[zk, 'mean'], writes=[zk])
                P.tt('dve', z[:, fc, :], z[:, fc, :], rstd, ALU.mult, reads=[zk, 'rstd'], writes=[zk])
                P.ts('dve', z[:, fc, :], z[:, fc, :], gcol[:, fc:fc + 1], bcol[:, fc:fc + 1], ALU.mult, ALU.add,
                     reads=[zk, 'vecs'], writes=[zk])
                if mod_post is not None:
                    mod_post(fc)
                if stash is not None and fc % 4 == 3:
                    stash(fc - 3)

        for g in (groups if stages >= 2 else ()):
            c0 = 3 * g * 128
            smp = (g == 2)
            ncp = 384 if smp else 512
            own = [1, 2] if smp else [1, 2, 3]
            tiles_all = [0, 1, 2, 3]
            hT = big[:, 0:8192].bitcast(BF16).rearrange("p (a b) -> p a b", a=32)
            xs = big[:, 8192:12288].rearrange("p (s j t) -> p s j t", s=2, j=4)
            mixT = R2[:, :].rearrange("p (a b) -> p a b", a=32)
            cv = Carver(S2)
            KT = cv.get([128, 4, 512], BF16)
            Vtok = cv.get([128, 4, 512], BF16)
            stf = [cv.get([128, 512], F32) for _ in range(2)]
            stb = [cv.get([128, 512], BF16) for _ in range(2)]
            rt = cv.get([128, 4, 64], F32)
            QTz = cv.get([128, 2, 4, 384], BF16)
            Eb = cv.get([128, 2048], BF16)
            Ef = cv.get([128, 2048], F32)
            Rr = cv.get([128, 2, 512], F32)
            cK = cv.get([128, 16, 128], BF16)
            cVt = cv.get([128, 16, 128], BF16)
            Ec = cv.get([128, 1024], BF16)
            tmpS = cv.get([128, 128], F32)
            off_pool = cv.off
            P.op('dve', lambda h: h.memset(QTz, 0.0), writes=['QT'])
            for kq in range(8):
                sl = kq % 2
                P.dma('sp', xs[:, sl], xv[:, kq * 4:(kq + 1) * 4, c0:c0 + 512], writes=[('xs', sl)], slot=('xs', sl))
                for j in range(4):
                    kc = kq * 4 + j
                    if kc % 2 == 0:
                        P.ts('dve', hT[:, kc, 0:ncp], xs[:, sl, j, 0:ncp], modP[:, 32 + kc:33 + kc], modP[:, kc:kc + 1],
                             ALU.mult, ALU.add, reads=[('xs', sl), 'mod'], writes=[('hT', kc)])
                    else:
                        P.actf(hT[:, kc, 0:ncp], xs[:, sl, j, 0:ncp], AF.Identity, bias=modP[:, kc:kc + 1],
                               scale=modP[:, 32 + kc:33 + kc], reads=[('xs', sl), 'mod'], writes=[('hT', kc)])
                    if smp:
                        tv = tmpS.rearrange("p (b t) -> p b t", b=16)
                        P.tt('dve', tv, xs[:, sl, j, 384:512].rearrange("p (b t) -> p b t", b=16),
                             bc_s(modS[:, 32 + kc, :]), ALU.mult, reads=[('xs', sl), 'mod'], writes=['tmpS'])
                        P.tt('dve', hT[:, kc, 384:512].rearrange("p (b t) -> p b t", b=16), tv,
                             bc_s(modS[:, kc, :]), ALU.add, reads=['tmpS', 'mod'], writes=[('hT', kc)])
            if CUT == 1:
                P.barrier()
                continue

            def rope_inplace(f, gt):
                f3 = f.rearrange("p (h d) -> p h d", h=8)
                x1 = f3[:, :, 0:8]
                x2 = f3[:, :, 8:16]
                cs = rope[:, gt, 0, :].unsqueeze(1).to_broadcast([128, 8, 8])
                sn = rope[:, gt, 1, :].unsqueeze(1).to_broadcast([128, 8, 8])
                r = [rt[:, i, :].rearrange("p (h d) -> p h d", h=8) for i in range(4)]
                P.tt('dve', r[0], x1, cs, ALU.mult, reads=['stf', 'rope'], writes=['rt'])
                P.tt('dve', r[1], x2, sn, ALU.mult, reads=['stf', 'rope'], writes=['rt'])
                P.tt('dve', r[2], x2, cs, ALU.mult, reads=['stf', 'rope'], writes=['rt'])
                P.tt('dve', r[3], x1, sn, ALU.mult, reads=['stf', 'rope'], writes=['rt'])
                P.tt('dve', x1, r[0], r[1], ALU.subtract, reads=['rt'], writes=['stf'])
                P.tt('dve', x2, r[2], r[3], ALU.add, reads=['rt'], writes=['stf'])

            def inproj(cg, lts, evac):
                tl = []
                for half in range(2):
                    a, k = wload(win_v[:, :, cg * 512 + half * 256: cg * 512 + half * 256 + 256], [128, 32, 256])
                    tl.append((a, k))
                for lt in lts:
                    bank = nextbank(0, 4)
                    for half in range(2):
                        a, k = tl[half]
                        for kc in range(32):
                            P.mm(ps[bank][:, half * 256:(half + 1) * 256], hT[:, kc, lt * 128:(lt + 1) * 128], a[:, kc, :],
                                 kc == 0, kc == 31, reads=[('hT', kc), k], writes=[psk(bank)])
                    if CUT >= 3:
                        evac(lt, bank)

            def out_rows(dst_p, dst_s, src, lt, view=None):
                if not smp:
                    return
                if lt == 2 and dst_p is not None:
                    P.dma('sp', dst_p, src if view is None else view(src), reads=['stf'], slot='o_misc', is_out=True)
                if lt == 3:
                    for b in range(16):
                        s_ = src[b * 8:(b + 1) * 8]
                        P.dma('sp', dst_s(b), s_ if view is None else view(s_), reads=['stf'], slot='o_misc', is_out=True)

            kview = lambda a: a.rearrange("p (k c d) -> p k c d", k=4, c=2)[:, :, 0, :]

            def evac_k(lt, bank):
                gt = 3 * g + lt
                f = stf[0]
                P.copy('act', f, ps[bank][:, :], reads=[psk(bank)], writes=['stf'])
                if CUT == 3:
                    return
                rope_inplace(f, gt)
                P.copy('act', stb[0], f, reads=['stf'], writes=['stb'])
                if CUT == 4:
                    return
                pb = ps[4 + lt % 2][:].bitcast(BF16)
                for kv in range(4):
                    P.tr(pb[:, kv * 128:(kv + 1) * 128], stb[0][:, kv * 128:(kv + 1) * 128], ident,
                         reads=['stb', 'cmat'], writes=[psk(4 + lt % 2)])
                P.copy('dve', KT[:, :, lt * 128:(lt + 1) * 128], pb[:, 0:512].rearrange("p (k t) -> p k t", k=4),
                       reads=[psk(4 + lt % 2)], writes=['KT'])
                out_rows(nkp_d.rearrange("p (k d) -> p k d", k=4),
                         lambda b: nks_d[b, 120:128, :].rearrange("p (k d) -> p k d", k=4), f, lt, kview)
            inproj(0, tiles_all, evac_k)
            if CUT <= 5:
                P.barrier()
                continue

            def evac_v(lt, bank):
                P.copy('dve', Vtok[:, lt, :], ps[bank][:, :], reads=[psk(bank)], writes=[('Vtok', lt)])
                if smp and lt >= 2:
                    f = stf[0]
                    P.copy('dve', f, ps[bank][:, :], reads=[psk(bank)], writes=['stf'])
                    out_rows(nvp_d.rearrange("p (k d) -> p k d", k=4),
                             lambda b: nvs_d[b, 120:128, :].rearrange("p (k d) -> p k d", k=4), f, lt, kview)
            inproj(1, tiles_all, evac_v)
            if CUT == 6:
                P.barrier()
                continue

            for kvh in range(4 if stages >= 3 else 0):
                def evac_q(lt, bank):
                    gt = 3 * g + lt
                    f = stf[1]
                    P.copy('act', f, ps[bank][:, :], reads=[psk(bank)], writes=['stf'])
                    rope_inplace(f, gt)
                    P.copy('act', stb[1], f, reads=['stf'], writes=['stb'])
                    pb = ps[4 + lt % 2][:].bitcast(BF16)
                    for pc in range(4):
                        P.tr(pb[:, pc * 128:(pc + 1) * 128], stb[1][:, pc * 128:(pc + 1) * 128], ident,
                             reads=['stb', 'cmat'], writes=[psk(4 + lt % 2)])
                    for c in range(2):
                        hs = slice(c * 64, (c + 1) * 64)
                        P.copy('dve', QTz[hs, c, :, (lt - 1) * 128:lt * 128], pb[hs, 0:512].rearrange("p (k t) -> p k t", k=4),
                               reads=[psk(4 + lt % 2)], writes=['QT'])
                inproj(2 + kvh, [1, 2, 3], evac_q)
                if smp:
                    P.dma('pool', cK, cKT_d[:, :, kvh, :], writes=['cK'], slot='cK')
                    P.dma('pool', cVt, cV_d[:, :, kvh, :], writes=['cV'], slot='cV')
                for lt in [1, 2, 3]:
                    if CUT == 7:
                        break
                    qc = (lt - 1) * 128
                    is_s = smp and lt == 3
                    vsl = slice(kvh * 128, (kvh + 1) * 128)
                    if not is_s:
                        for kt, ktile in enumerate([lt - 1, lt]):
                            for c in range(2):
                                bank = kt * 2 + c
                                hs = slice(c * 64, (c + 1) * 64)
                                P.mm(ps[bank][:, :].rearrange("p (a b) -> p a b", a=4),
                                     KT[:, kvh, ktile * 128:(ktile + 1) * 128], QTz[:, c, :, qc:qc + 128], True, True,
                                     reads=['KT', 'QT'], writes=[psk(bank)])
                                if CUT == 8:
                                    continue
                                P.actf(Ef[:, bank * 512:(bank + 1) * 512], ps[bank][:, :], AF.Exp, scale=0.125,
                                       reads=[psk(bank)], writes=[('Ef', bank)])
                            if CUT in (8, 9):
                                continue
                            midx = MC if kt == 1 else (MFP if (g == 0 and lt == 1) else MP)
                            ev = Eb[:, kt * 1024:(kt + 1) * 1024].rearrange("p (a b) -> p a b", a=8)
                            efv = Ef[:, kt * 1024:(kt + 1) * 1024].rearrange("p (a b) -> p a b", a=8)
                            P.tt('dve', ev, efv, cmat[:, midx, :].unsqueeze(1).to_broadcast([128, 8, 128]), ALU.mult,
                                 reads=[('Ef', kt * 2), ('Ef', kt * 2 + 1), 'cmat'], writes=[('Eb', kt * 2), ('Eb', kt * 2 + 1)])
                        for c in range(2):
                            if CUT in (8, 9, 10):
                                continue
                            P.mm(ps[6 + c][:, :], ones_b[:], Eb[:, c * 512:(c + 1) * 512], True, False,
                                 reads=[('Eb', c), 'ones_b'], writes=[psk(6 + c)])
                            P.mm(ps[6 + c][:, :], ones_b[:], Eb[:, (2 + c) * 512:(3 + c) * 512], False, True,
                                 reads=[('Eb', 2 + c)], writes=[psk(6 + c)])
                            P.mm(ps[4 + c][:, :], Vtok[:, lt - 1, vsl], Eb[:, c * 512:(c + 1) * 512], True, False,
                                 reads=[('Eb', c), ('Vtok', lt - 1 if not is_s else 3)], writes=[psk(4 + c)])
                            P.mm(ps[4 + c][:, :], Vtok[:, lt, vsl], Eb[:, (2 + c) * 512:(3 + c) * 512], False, True,
                                 reads=[('Eb', 2 + c), ('Vtok', lt)], writes=[psk(4 + c)])
                    else:
                        for c in range(2):
                            hs = slice(c * 64, (c + 1) * 64)
                            P.mm(ps[c][:, :].rearrange("p (a b) -> p a b", a=4), KT[:, kvh, 384:512], QTz[:, c, :, qc:qc + 128],
                                 True, True, reads=['KT', 'QT'], writes=[psk(c)])
                            P.actf(Ef[:, c * 512:(c + 1) * 512], ps[c][:, :], AF.Exp, scale=0.125,
                                   reads=[psk(c)], writes=[('Ef', c)])
                        ev = Eb[:, 0:1024].rearrange("p (a b) -> p a b", a=8)
                        efv = Ef[:, 0:1024].rearrange("p (a b) -> p a b", a=8)
                        P.tt('dve', ev, efv, cmat[:, MSN, :].unsqueeze(1).to_broadcast([128, 8, 128]), ALU.mult,
                             reads=[('Ef', 0), ('Ef', 1), 'cmat'], writes=[('Eb', 0), ('Eb', 1)])
                        for b in range(16):
                            for c in range(2):
                                hs = slice(c * 64, (c + 1) * 64)
                                off = ((b % 8) * 2 + c) * 32
                                P.mm(ps[2 + b // 8][:, off:off + 32].rearrange("p (a t) -> p a t", a=4), cK[:, b, :],
                                     QTz[:, c, :, qc + b * 8:qc + b * 8 + 8], True, True,
                                     reads=['cK', 'QT'], writes=[psk(2 + b // 8)])
                        for hb in range(2):
                            P.actf(Ef[:, 1024 + hb * 512:1024 + (hb + 1) * 512], ps[2 + hb][:, :], AF.Exp, scale=0.125,
                                   reads=[psk(2 + hb)], writes=[('Ef', 2 + hb)])
                        ecv = Ec.rearrange("p (a t) -> p a t", t=8)
                        efv = Ef[:, 1024:2048].rearrange("p (a t) -> p a t", t=8)
                        P.tt('dve', ecv, efv, cmat[:, MSC, 0:8].unsqueeze(1).to_broadcast([128, 128, 8]), ALU.mult,
                             reads=[('Ef', 2), ('Ef', 3), 'cmat'], writes=['Ec'])
                        Ec4 = Ec.rearrange("p (b c a t) -> p b c a t", b=16, c=2, a=4)
                        for c in range(2):
                            pperm = lambda bk: ps[bk][:, :].rearrange("p (a b t) -> p b a t", a=4, b=16)
                            P.mm(ps[6 + c][:, :], ones_b[:], Eb[:, c * 512:(c + 1) * 512], True, False,
                                 reads=[('Eb', c), 'ones_b'], writes=[psk(6 + c)])
                            P.mm(pperm(6 + c), ones_b[:], Ec4[:, :, c, :, :], False, True,
                                 reads=['Ec'], writes=[psk(6 + c)])
                            P.mm(ps[4 + c][:, :], Vtok[:, 3, vsl], Eb[:, c * 512:(c + 1) * 512], True, False,
                                 reads=[('Eb', c), ('Vtok', lt - 1 if not is_s else 3)], writes=[psk(4 + c)])
                            for b in range(16):
                                P.mm(pperm(4 + c)[:, b, :, :], cVt[:, b, :], Ec4[:, b, c, :, :], False, b == 15,
                                     reads=['Ec', 'cV'], writes=[psk(4 + c)])
                    for c in range(2):
                        if CUT in (8, 9, 10, 11):
                            continue
                        hs = slice(c * 64, (c + 1) * 64)
                        rv = Rr[hs, c, :].rearrange("p (a b) -> p a b", a=4)
                        sk = sinkE[hs, c * 16 + kvh * 4: c * 16 + kvh * 4 + 4].unsqueeze(2).to_broadcast([64, 4, 128])
                        P.tt('dve', rv, ps[6 + c][hs, :].rearrange("p (a b) -> p a b", a=4), sk, ALU.add,
                             reads=[psk(6 + c), 'sinkE'], writes=['Rr'])
                        P.op('dve', lambda h, rv=rv: h.reciprocal(rv, rv), reads=['Rr'], writes=['Rr'])
                        P.tt('dve', mixT[hs, kvh * 4:(kvh + 1) * 4, qc:qc + 128],
                             ps[4 + c][hs, :].rearrange("p (a b) -> p a b", a=4), rv, ALU.mult,
                             reads=[psk(4 + c), 'Rr'], writes=['mixT'])

            if CUT in (7, 8, 9, 10, 11, 12):
                P.barrier()
                continue
            cv.off = off_pool
            Utok = cv.get([128, 4, 512], BF16)
            dT = cv.get([128, 4, 384], BF16)
            SPt = cv.get([128, 2, 512], BF16)
            for g4 in range(4 if stages >= 3 else (4 if stages >= 2 else 0)):
                def evac_u(lt, bank):
                    P.copy('dve', Utok[:, lt, :], ps[bank][:, :], reads=[psk(bank)], writes=[('Utok', lt)])
                    if smp and lt >= 2:
                        f = stf[0]
                        P.copy('dve', f, ps[bank][:, :], reads=[psk(bank)], writes=['stf'])
                        if lt == 2:
                            P.dma('sp', npp_d[:, g4 * 512:(g4 + 1) * 512], f[113:128, :], reads=['stf'], slot='o_misc', is_out=True)
                        else:
                            for b in range(16):
                                P.dma('sp', nps_d[b, 7:15, g4 * 512:(g4 + 1) * 512], f[b * 8:(b + 1) * 8, :], reads=['stf'],
                                      slot='o_misc', is_out=True)
                inproj(6 + g4, tiles_all, evac_u)
                if stages < 3:
                    continue
                Wp, wpk = wload(poolw_d[:, g4], [128, 4, 512])
                if smp:
                    P.dma('pool', SPt[0:120], sptm_d[:, :, g4 * 512:(g4 + 1) * 512], writes=['SPt'], slot='SPt')
                for lt in [1, 2, 3]:
                    bank = nextbank(0, 2)
                    first = (g == 0 and lt == 1)
                    for cc in range(4):
                        o = ps[bank][:, cc * 128:(cc + 1) * 128]
                        csl = slice(cc * 128, (cc + 1) * 128)
                        if smp and lt == 3:
                            P.mm(o, SPt[0:120, 0, csl], cmat[0:120, BSA + g4, :], True, False, reads=['SPt', 'cmat'], writes=[psk(bank)])
                            P.mm(o, SPt[0:120, 1, csl], cmat[0:120, BSB + g4, :], False, False, reads=['SPt'], writes=[psk(bank)])
                            P.mm(o, Utok[:, 3, csl], cmat[:, BSM + g4, :], False, True, reads=[('Utok', 3)], writes=[psk(bank)])
                        else:
                            P.mm(o, Utok[:, lt - 1, csl], cmat[:, (BPF if first else BP) + g4, :], True, False,
                                 reads=[('Utok', lt - 1), 'cmat'], writes=[psk(bank)])
                            P.mm(o, Utok[:, lt, csl], cmat[:, (BCF if first else BC) + g4, :], False, True,
                                 reads=[('Utok', lt)], writes=[psk(bank)])
                    P.copy('dve', dT[:, :, (lt - 1) * 128:lt * 128], ps[bank][:, :].rearrange("p (a b) -> p a b", a=4),
                           reads=[psk(bank)], writes=['dT'])
                for dc in range(4):
                    bank = nextbank(2, 4)
                    for cc in range(4):
                        P.mm(ps[bank][:, 0:384], Wp[:, cc, dc * 128:(dc + 1) * 128], dT[:, cc, :], cc == 0, cc == 3,
                             reads=['dT', wpk], writes=[psk(bank)])
                    fcm = 16 + g4 * 4 + dc
                    P.ts('dve', mixT[:, fcm, :], ps[bank][:, 0:384], pscT[:, g4 * 4 + dc:g4 * 4 + dc + 1], None, ALU.mult,
                         reads=[psk(bank), 'vecs'], writes=['mixT'])
            if stages < 4:
                P.barrier()
                continue

            P.barrier()
            z = big[:, :].rearrange("p (a b) -> p a b", a=32)
            h2T = R2[:, :].rearrange("p (a b) -> p a b", a=32)
            cv = Carver(S2)
            sqt = [cv.get([128, 384], F32) for _ in range(2)]
            tmpz = cv.get([128, 128], F32)
            oc0 = 3 * g * 128
            for kq in range(8):
                P.dma('sp', z[:, kq * 4:(kq + 1) * 4, :], xv[:, kq * 4:(kq + 1) * 4, c0 + 128:c0 + 512],
                      writes=[('z', kq * 4 + j) for j in range(4)], slot=('zl', kq))
                for j in range(4):
                    fc = kq * 4 + j
                    P.op('act', lambda h, fc=fc: h.mul(z[:, fc, :], z[:, fc, :], ALPHA), reads=[('z', fc)], writes=[('z', fc)])

            def z_accum(fc, src, gbase, srckey):
                zk = ('z', fc)
                P.stt('dve', z[:, fc, 0:ncp - 128], src[:, 0:ncp - 128], modP[:, gbase + fc:gbase + fc + 1], z[:, fc, 0:ncp - 128],
                      ALU.mult, ALU.add, reads=[srckey, zk, 'mod'], writes=[zk])
                if smp:
                    tv = tmpz.rearrange("p (b t) -> p b t", b=16)
                    P.tt('dve', tv, src[:, 256:384].rearrange("p (b t) -> p b t", b=16), bc_s(modS[:, gbase + fc, :]), ALU.mult,
                         reads=[srckey, 'mod'], writes=['tmpz'])
                    P.tt('dve', z[:, fc, 256:384], z[:, fc, 256:384], tmpz, ALU.add, reads=['tmpz', zk], writes=[zk])
                sq = sqt[fc % 2]
                P.actf(sq, z[:, fc, :], AF.Square, reads=[zk], writes=[('sqt', fc % 2)])
                P.mm(ps[6][:, 0:384], ones_f[:], z[:, fc, :], fc == 0, fc == 31, reads=[zk, 'ones_f'], writes=[psk(6)])
                P.mm(ps[7][:, 0:384], ones_f[:], sq, fc == 0, fc == 31, reads=[('sqt', fc % 2)], writes=[psk(7)])

            for wt in range(16):
                a, k = wload(wout_v[:, :, wt * 256:(wt + 1) * 256], [128, 32, 256])
                for j in range(2):
                    fc = wt * 2 + j
                    bank = nextbank(0, 4)
                    for kc in range(32):
                        P.mm(ps[bank][:, 0:384], a[:, kc, j * 128:(j + 1) * 128], mixT[:, kc, :], kc == 0, kc == 31,
                             reads=['mixT', k], writes=[psk(bank)])
                    z_accum(fc, ps[bank], 64, psk(bank))

            def post1(fc):
                zk = ('z', fc)
                P.actf(h2T[:, fc, 0:ncp - 128], z[:, fc, 0:ncp - 128], AF.Identity, bias=modP[:, 96 + fc:97 + fc],
                       scale=modP[:, 128 + fc:129 + fc], reads=[zk, 'mod'], writes=[('h2T', fc)])
                if smp:
                    tv = tmpz.rearrange("p (b t) -> p b t", b=16)
                    P.tt('dve', tv, z[:, fc, 256:384].rearrange("p (b t) -> p b t", b=16), bc_s(modS[:, 128 + fc, :]), ALU.mult,
                         reads=[zk, 'mod'], writes=['tmpz'])
                    P.tt('dve', h2T[:, fc, 256:384].rearrange("p (b t) -> p b t", b=16), tv, bc_s(modS[:, 96 + fc, :]), ALU.add,
                         reads=['tmpz', 'mod'], writes=[('h2T', fc)])
                P.op('act', lambda h: h.mul(z[:, fc, :], z[:, fc, :], ALPHA), reads=[zk], writes=[zk])

            def stash1(fc0):
                P.dma('sp', yv[:, fc0:fc0 + 4, oc0:oc0 + 384], z[:, fc0:fc0 + 4, :], reads=[('z', fc0 + j) for j in range(4)],
                      writes=[('yst', fc0)], slot=('yst', fc0))
            ln_block(z, ln1g, ln1b, g, post1, stash1)
            P.barrier()
            if stages < 5:
                continue

            y2T = big[:, :].rearrange("p (a b) -> p a b", a=32)
            cv = Carver(S2)
            s_sb = cv.get([128, 3, 16, 128], F32)
            tau = cv.get([128, 3, 8], F32)
            off_tmp = cv.off
            qT = cv.get([128, 16, 384], BF16)
            t16 = cv.get([128, 16, 16], F32)
            wk = cv.get([128, 256], F32)
            cand = cv.get([128, 8, 256], F32)
            b16 = cv.get([128, 8, 16], F32)
            smal = cv.get([128, 8, 8], F32)
            for wt in range(8):
                a, k = wload(wq_v[:, :, wt * 256:(wt + 1) * 256], [128, 32, 256])
                for j in range(2):
                    pc = wt * 2 + j
                    bank = nextbank(0, 4)
                    for kc in range(32):
                        P.mm(ps[bank][:, 0:384], a[:, kc, j * 128:(j + 1) * 128], h2T[:, kc, :], kc == 0, kc == 31,
                             reads=[('h2T', kc), k], writes=[psk(bank)])
                    P.copy('dve', qT[:, pc, :], ps[bank][:, 0:384], reads=[psk(bank)], writes=['qT'])
            subk, subk_key = wload(subk_d, [128, 16, 128])
            for t in range(3):
                for pc in range(16):
                    bk = 4 + pc // 4
                    P.mm(ps[bk][:, (pc % 4) * 128:(pc % 4 + 1) * 128], qT[:, pc, t * 128:(t + 1) * 128], subk[:, pc, :], True, True,
                         reads=['qT', subk_key], writes=[psk(bk)])
                for q4 in range(4):
                    P.copy('act' if q4 % 2 else 'dve', s_sb[:, t, q4 * 4:(q4 + 1) * 4, :],
                           ps[4 + q4][:, :].rearrange("p (a b) -> p a b", a=4), reads=[psk(4 + q4)], writes=['s_sb'])
                for pc in range(16):
                    P.op('dve', lambda h, pc=pc, t=t: h.max(t16[:, pc, 0:8], s_sb[:, t, pc, :]), reads=['s_sb'], writes=['t16'])
                    P.op('dve', lambda h, pc=pc, t=t: h.match_replace(wk[:, 0:128], t16[:, pc, 0:8], s_sb[:, t, pc, :], -1e30),
                         reads=['s_sb', 't16'], writes=['wk'])
                    P.op('dve', lambda h, pc=pc: h.max(t16[:, pc, 8:16], wk[:, 0:128]), reads=['wk'], writes=['t16'])
                t16v = t16.rearrange("p (h q) k -> p h q k", q=2)
                s_v = s_sb[:, t].rearrange("p (h q) k -> p h q k", q=2)

                def cand_top(tag):
                    P.tt('dve', cand.rearrange("p h (i j) -> p h i j", i=16),
                         t16v[:, :, 0, :].unsqueeze(3).to_broadcast([128, 8, 16, 16]),
                         t16v[:, :, 1, :].unsqueeze(2).to_broadcast([128, 8, 16, 16]), ALU.add, reads=['t16'], writes=['cand'])
                    for hh in range(8):
                        P.op('dve', lambda h, hh=hh: h.max(b16[:, hh, 0:8], cand[:, hh, :]), reads=['cand'], writes=['b16'])
                        P.op('dve', lambda h, hh=hh: h.match_replace(wk[:, :], b16[:, hh, 0:8], cand[:, hh, :], -1e30),
                             reads=['cand', 'b16'], writes=['wk'])
                        P.op('dve', lambda h, hh=hh: h.max(b16[:, hh, 8:16], wk[:, :]), reads=['wk'], writes=['b16'])
                cand_top(0)
                mcol = smal[:, 0, :]
                zcol = smal[:, 1, :]
                P.copy('dve', mcol, b16[:, :, 0], reads=['b16'], writes=['smal'])
                P.tt('dve', b16[:, :, :], b16[:, :, :], mcol.unsqueeze(2).to_broadcast([128, 8, 16]), ALU.subtract,
                     reads=['b16', 'smal'], writes=['b16'])
                P.actf(b16[:, :, :], b16[:, :, :], AF.Exp, reads=['b16'], writes=['b16'])
                P.op('dve', lambda h: h.reduce_sum(zcol, b16[:, :, :], AX.X), reads=['b16'], writes=['smal'])
                P.actf(zcol, zcol, AF.Ln, reads=['smal'], writes=['smal'])
                P.tt('dve', zcol, zcol, mcol, ALU.add, reads=['smal'], writes=['smal'])
                P.tt('dve', s_v[:, :, 1, :], s_v[:, :, 1, :], zcol.unsqueeze(2).to_broadcast([128, 8, 128]), ALU.subtract,
                     reads=['s_sb', 'smal'], writes=['s_sb'])
                P.tt('dve', t16v[:, :, 1, :], t16v[:, :, 1, :], zcol.unsqueeze(2).to_broadcast([128, 8, 16]), ALU.subtract,
                     reads=['t16', 'smal'], writes=['t16'])
                cand_top(1)
                P.copy('dve', tau[:, t, :], b16[:, :, 15], reads=['b16'], writes=['tau'])
            cv.off = off_tmp
            P.barrier()
            actT = [cv.get([128, 8, 384], BF16) for _ in range(2)]
            gel = [cv.get([128, 384], F32) for _ in range(2)]
            valb = [cv.get([128, 8, 128], F32) for _ in range(2)]
            Ebf = [cv.get([128, 8, 128], BF16) for _ in range(2)]
            maskb = [cv.get([128, 8, 128], BF16) for _ in range(2)]
            Gb = Ebf
            Uslot = [W[0][:, :].rearrange("p (a b) -> p a b", a=32), W[1][:, :].rearrange("p (a b) -> p a b", a=32)]
            Vslot = [W[2][:, 0:4096].rearrange("p (a b) -> p a b", a=8), W[2][:, 4096:8192].rearrange("p (a b) -> p a b", a=8)]
            NCH = 128
            ucount = [0]
            pend = [None]
            finp = [None]

            def stage1(i, t, sl):
                s_v = s_sb[:, t].rearrange("p (h q) k -> p h q k", q=2)
                if t == 2:
                    for hh in range(8):
                        P.actf(valb[sl][:, hh, :], s_v[:, hh, 1, :], AF.Identity, bias=s_v[:, hh, 0, i:i + 1],
                               reads=['s_sb'], writes=[('valb', sl)])
                else:
                    P.tt('dve', valb[sl], s_v[:, :, 1, :], s_v[:, :, 0, i:i + 1].to_broadcast([128, 8, 128]), ALU.add,
                         reads=['s_sb'], writes=[('valb', sl)])
                P.actf(Ebf[sl], valb[sl], AF.Exp, reads=[('valb', sl)], writes=[('Ebf', sl)])

            def stage2(i, t, sl):
                gb = 2 + i % 2
                P.tt('dve', maskb[sl], valb[sl], tau[:, t, :].unsqueeze(2).to_broadcast([128, 8, 128]), ALU.is_ge,
                     reads=[('valb', sl), 'tau'], writes=[('maskb', sl)])
                P.tt('dve', Gb[sl], maskb[sl], Ebf[sl], ALU.mult, reads=[('maskb', sl), ('Ebf', sl)], writes=[('Ebf', sl)])
                for n in (4, 2, 1):
                    P.tt('dve', Gb[sl][:, 0:n, :], Gb[sl][:, 0:n, :], Gb[sl][:, n:2 * n, :], ALU.add,
                         reads=[('Ebf', sl)], writes=[('Ebf', sl)])
                P.mm(ps[gb][:, t * 128:(t + 1) * 128], Gb[sl][:, 0, :], ident, True, True,
                     reads=[('Ebf', sl), 'cmat'], writes=[psk(gb)])

            def finalize(i):
                EB, ci = i // 8, i % 8
                P.tt('dve', actT[EB % 2][:, ci, :], ps[2 + i % 2][:, 0:384], gel[i % 2], ALU.mult,
                     reads=[psk(2 + i % 2), ('gel', i % 2)], writes=[('actT', EB % 2, ci)])

            def step_unit(i, t):
                sl = ucount[0] % 2
                ucount[0] += 1
                stage1(i, t, sl)
                if finp[0] is not None and t == 2:
                    finalize(finp[0])
                    finp[0] = None
                if pend[0] is not None:
                    pi, pt, psl = pend[0]
                    stage2(pi, pt, psl)
                    if pt == 2:
                        finp[0] = pi
                pend[0] = (i, t, sl)
                emit_one_add()

            def flush_units():
                if finp[0] is not None:
                    finalize(finp[0])
                    finp[0] = None
                if pend[0] is not None:
                    pi, pt, psl = pend[0]
                    stage2(pi, pt, psl)
                    if pt == 2:
                        finalize(pi)
                    pend[0] = None

            vcount = [0]
            ypend = []

            def emit_one_add():
                if not ypend:
                    return
                fc, yb, first = ypend.pop(0)
                if first:
                    P.copy('dve', y2T[:, fc, :], ps[yb][:, 0:384], reads=[psk(yb)], writes=[('z', fc)])
                else:
                    P.tt('dve', y2T[:, fc, :], y2T[:, fc, :], ps[yb][:, 0:384], ALU.add, reads=[psk(yb), ('z', fc)],
                         writes=[('z', fc)])

            def vpart(v):
                EB, fq = v // 8, v % 8
                while ypend:
                    emit_one_add()
                vs = vcount[0] % 2
                vcount[0] += 1
                va = Vslot[vs]
                P.dma('pool', va, V_v[:, EB * 8:(EB + 1) * 8, fq * 512:(fq + 1) * 512], writes=[('Wv', vs)], slot=('Wv', vs))
                for fj in range(4):
                    fc = fq * 4 + fj
                    yb = 4 + fj
                    for ci in range(8):
                        P.mm(ps[yb][:, 0:384], va[:, ci, fj * 128:(fj + 1) * 128], actT[EB % 2][:, ci, :], ci == 0, ci == 7,
                             reads=[('actT', EB % 2, ci), ('Wv', vs)], writes=[psk(yb)])
                    ypend.append((fc, yb, EB == 0))

            VLAG = 9
            abank = [0, 1]
            for i in range(NCH):
                if i % 2 == 0:
                    us = (i // 2) % 2
                    P.dma('pool', Uslot[us], UT_v[:, :, i * 128:(i + 2) * 128], writes=[('W', us)], slot=('W', us))
                us = (i // 2) % 2
                j = i % 2
                ab = nextbank(0, 2)
                for kc in range(32):
                    P.mm(ps[ab][:, 0:384], Uslot[us][:, kc, j * 128:(j + 1) * 128], h2T[:, kc, :], kc == 0, kc == 31,
                         reads=[('h2T', kc), ('W', us)], writes=[psk(ab)])
                abank[i % 2] = ab
                if i % 2 == 1:
                    for ii in (i - 1, i):
                        P.actf(gel[ii % 2], ps[abank[ii % 2]][:, 0:384], AF.Gelu, reads=[psk(abank[ii % 2])], writes=[('gel', ii % 2)])
                if i >= VLAG:
                    vpart(i - VLAG)
                for t in range(3):
                    step_unit(i, t)
            flush_units()
            for v in range(NCH - VLAG, NCH):
                vpart(v)
            while ypend:
                emit_one_add()
            P.barrier()
            cv = Carver(S2)
            sqt = [cv.get([128, 384], F32) for _ in range(2)]
            tmpz = cv.get([128, 128], F32)
            xst = [cv.get([128, 4, 384], F32) for _ in range(2)]
            for kq in range(8):
                sl = kq % 2
                P.dma('sp', xst[sl], yv[:, kq * 4:(kq + 1) * 4, oc0:oc0 + 384], reads=[('yst', kq * 4)], writes=[('xst', sl)],
                      slot=('xst', sl))
                for j in range(4):
                    fc = kq * 4 + j
                    zk = ('z', fc)
                    P.stt('dve', y2T[:, fc, 0:ncp - 128], y2T[:, fc, 0:ncp - 128], modP[:, 160 + fc:161 + fc], xst[sl][:, j, 0:ncp - 128],
                          ALU.mult, ALU.add, reads=[zk, ('xst', sl), 'mod'], writes=[zk])
                    if smp:
                        tv = tmpz.rearrange("p (b t) -> p b t", b=16)
                        P.tt('dve', tv, y2T[:, fc, 256:384].rearrange("p (b t) -> p b t", b=16), bc_s(modS[:, 160 + fc, :]), ALU.mult,
                             reads=[zk, 'mod'], writes=['tmpz'])
                        P.tt('dve', y2T[:, fc, 256:384], tmpz, xst[sl][:, j, 256:384], ALU.add, reads=['tmpz', ('xst', sl)], writes=[zk])
                    sq = sqt[fc % 2]
                    P.actf(sq, y2T[:, fc, :], AF.Square, reads=[zk], writes=[('sqt', fc % 2)])
                    P.mm(ps[6][:, 0:384], ones_f[:], y2T[:, fc, :], fc == 0, fc == 31, reads=[zk, 'ones_f'], writes=[psk(6)])
                    P.mm(ps[7][:, 0:384], ones_f[:], sq, fc == 0, fc == 31, reads=[('sqt', fc % 2)], writes=[psk(7)])

            def stash2(fc0):
                P.dma('sp', yv[:, fc0:fc0 + 4, oc0:oc0 + 384], y2T[:, fc0:fc0 + 4, :], reads=[('z', fc0 + j) for j in range(4)],
                      writes=[('yst', fc0)], slot='o_y', is_out=True)
            ln_block(y2T, ln2g, ln2b, g, None, stash2)
            P.barrier()
        P.emit()
    return nc


def _bf(x):
    return np.ascontiguousarray(x, dtype=np.float32)


def _consts(hf):
    cm = np.zeros((NMAT, 128, 128), np.float32)
    k = np.arange(128)[:, None]
    q = np.arange(128)[None, :]
    cm[MP] = (k >= q)
    cm[MC] = (k <= q)
    cm[MFP] = cm[MP] if hf == 1 else 0.0
    cm[MSN] = ((k // 8) == (q // 8)) & ((k % 8) <= (q % 8))
    cm[IDM] = np.eye(128)
    cm[MSC, :, 0:8] = (np.arange(128)[:, None] >= np.arange(8)[None, :])
    for g4, w in enumerate((2, 4, 8, 16)):
        tp = np.arange(128)[:, None]
        t = np.arange(128)[None, :]
        cur = ((tp <= t) & (tp > t - w)).astype(np.float32)
        prv = (tp - 128 > t - w).astype(np.float32)
        cm[BP + g4] = prv / w
        cm[BC + g4] = cur / w - np.eye(128)
        if hf == 1:
            cm[BPF + g4] = cm[BP + g4]
            cm[BCF + g4] = cm[BC + g4]
        else:
            cnt = np.minimum(w, t + 1).astype(np.float32)
            cm[BPF + g4] = 0.0
            cm[BCF + g4] = cur / cnt - np.eye(128)
        bq, tq = np.arange(128)[None, :] // 8, np.arange(128)[None, :] % 8
        r_b, r_r = np.arange(120)[:, None] // 15, np.arange(120)[:, None] % 15
        for half, idx in ((0, BSA), (1, BSB)):
            m = ((r_b + 8 * half) == bq) & (r_r > 15 + tq - w)
            cm[idx + g4, 0:120] = m / w
        bk, tk = np.arange(128)[:, None] // 8, np.arange(128)[:, None] % 8
        m = (bk == bq) & (tk <= tq) & (tk > tq - w)
        cm[BSM + g4] = m / w - np.eye(128)
    return np.ascontiguousarray(cm.transpose(1, 0, 2))


def _rope_tab(hf):
    half = 8
    inv = (np.float32(500000.0) ** (-np.arange(half, dtype=np.float32) / np.float32(half))).astype(np.float32)
    tab = np.zeros((128, 10, 2, 8), np.float32)
    for gt in range(10):
        if gt < 9:
            pos = hf * 1024 + (gt - 1) * 128 + np.arange(128)
        else:
            pos = 8192 + (np.arange(128) % 8)
        ang = pos.astype(np.float32)[:, None] * inv[None, :]
        tab[:, gt, 0, :] = np.cos(ang)
        tab[:, gt, 1, :] = np.sin(ang)
    return tab


_NC_CACHE = {}


def _prep(x_prompt, x_sample, cache_k, cache_v, state_pool, c_prompt, c_sample, w_ada, b_ada, w_in,
          sinks, pool_w, pool_scale, w_out, ln1_g, ln1_b, peer_wq, peer_subkeys, peer_u, peer_v,
          ln2_g, ln2_b):
    f = lambda a: np.asarray(a, dtype=np.float32)
    x_prompt, x_sample, cache_k, cache_v, state_pool = map(f, (x_prompt, x_sample, cache_k, cache_v, state_pool))
    c_prompt, c_sample = f(c_prompt), f(c_sample)
    wi = f(w_in)[0]
    kcol = wi[:, 2048:2304].reshape(D, 4, 1, 64)
    vcol = wi[:, 2304:2560].reshape(D, 4, 1, 64)
    win_r = np.concatenate([np.broadcast_to(kcol, (D, 4, 2, 64)).reshape(D, 512),
                            np.broadcast_to(vcol, (D, 4, 2, 64)).reshape(D, 512),
                            wi[:, 0:2048], wi[:, 2560:4608]], axis=1)
    vecs = np.zeros((128, 384), np.float32)
    vecs[:, 0:192] = f(b_ada)[0].reshape(192, 128).T
    vecs[:, 192:208] = f(pool_scale)[0].reshape(16, 128).T
    for i, a in enumerate((ln1_g, ln1_b, ln2_g, ln2_b)):
        vecs[:, 208 + 32 * i:240 + 32 * i] = f(a)[0].reshape(32, 128).T
    vecs[:, 336:368] = np.broadcast_to(f(sinks)[0].reshape(16, 2).T.reshape(1, 32), (128, 32))
    common = {
        "w_ada": _bf(f(w_ada)[0]), "w_in": _bf(win_r), "w_out": _bf(f(w_out)[0]), "wq": _bf(f(peer_wq)[0]),
        "subk": _bf(f(peer_subkeys)[0].transpose(3, 0, 1, 2).reshape(128, 16, 128)),
        "UT": _bf(f(peer_u)[0].T), "V": _bf(f(peer_v)[0]),
        "poolw": _bf(f(pool_w)[0].reshape(4, 4, 128, 512).transpose(2, 0, 1, 3)),
        "vecs": vecs,
    }
    in_maps = []
    for r in range(8):
        b, hf = r // 2, r % 2
        xs = x_sample[16 * r:16 * r + 16].reshape(128, D)
        t0 = hf * 1024
        halo = x_prompt[b, t0 - 128:t0] if hf == 1 else np.zeros((128, D), np.float32)
        xT = np.concatenate([halo, x_prompt[b, t0:t0 + 1024], xs], axis=0).T
        cc = np.concatenate([c_prompt[b:b + 1], c_sample[16 * r:16 * r + 16]], axis=0)
        ck = cache_k[0, 16 * r:16 * r + 16]
        cvv = cache_v[0, 16 * r:16 * r + 16]
        ckt = ck.transpose(3, 0, 2, 1)
        cvt = cvv.transpose(1, 0, 2, 3)
        sp = state_pool[0, 16 * r:16 * r + 16]
        m = dict(common)
        m.update({
            "xT": _bf(xT),
            "cT": _bf(cc.T.reshape(32, 128, 17).transpose(1, 0, 2)),
            "cmat": _consts(hf), "rope": _rope_tab(hf),
            "cKT": _bf(np.concatenate([ckt, ckt], axis=0)),
            "cV": _bf(np.concatenate([cvt, cvt], axis=3)),
            "ckn": _bf(ck.reshape(16, 128, 256)), "cvn": _bf(cvv.reshape(16, 128, 256)),
            "sptm": _bf(sp.reshape(2, 120, 2048).transpose(1, 0, 2)), "spn": _bf(sp),
        })
        in_maps.append(m)
    return in_maps


def _assemble(res, cores=tuple(range(8))):
    y_p = np.zeros((4, 2048, D), np.float32)
    y_s = np.zeros((128, 8, D), np.float32)
    nkp = np.zeros((1, 4, 128, 4, 64), np.float32)
    nvp = np.zeros_like(nkp)
    npp = np.zeros((1, 4, 15, 2048), np.float32)
    nks = np.zeros((1, 128, 128, 4, 64), np.float32)
    nvs = np.zeros_like(nks)
    nps = np.zeros((1, 128, 15, 2048), np.float32)
    for r in cores:
        b, hf = r // 2, r % 2
        o = res[cores.index(r)]
        yT = o["yT"]
        y_p[b, hf * 1024:(hf + 1) * 1024] = yT[:, 0:1024].T
        y_s[16 * r:16 * r + 16] = yT[:, 1024:1152].T.reshape(16, 8, D)
        if hf == 1:
            nkp[0, b] = o["nkp"].reshape(128, 4, 64)
            nvp[0, b] = o["nvp"].reshape(128, 4, 64)
            npp[0, b] = o["npp"]
        nks[0, 16 * r:16 * r + 16] = o["nks"].reshape(16, 128, 4, 64)
        nvs[0, 16 * r:16 * r + 16] = o["nvs"].reshape(16, 128, 4, 64)
        nps[0, 16 * r:16 * r + 16] = o["nps"]
    return (y_p, y_s, nkp, nvp, npp, nks, nvs, nps)


def kernel(**inputs):
    in_maps = _prep(**inputs)
    if 'nc' not in _NC_CACHE:
        _NC_CACHE['nc'] = build_nc()
    res = run_bass_kernel_spmd(_NC_CACHE['nc'], in_maps, core_ids=list(range(8))).results
    return _assemble(res)
```

```python
import numpy as np
from contextlib import ExitStack
import concourse.bass as bass
import concourse.mybir as mybir
from concourse.bass_utils import run_bass_kernel_spmd

F32 = mybir.dt.float32
BF16 = mybir.dt.bfloat16
ALU = mybir.AluOpType
AF = mybir.ActivationFunctionType
AX = mybir.AxisListType
ENG = ('pe', 'act', 'dve', 'pool', 'sp')


class Prog:
    def __init__(self, nc, es):
        self.nc = nc
        self.es = es
        self.ops = {e: [] for e in ENG}
        self.n = {e: 0 for e in ENG}
        self.seen = {e: {} for e in ENG}
        self.res = {}
        self.dcount = {}
        self.targets = {e: set() for e in ENG}
        self.out_slots = set()

    def _deps(self, reads, writes):
        d = []
        for k in reads:
            r = self.res.get(k)
            if r and r[0] is not None:
                d.append(r[0])
        for k in writes:
            r = self.res.get(k)
            if r:
                if r[0] is not None:
                    d.append(r[0])
                for sk, v in r[1].items():
                    d.append((sk[0], sk[1], v))
        return d

    def _waits(self, eng, deps, skip_same=False):
        for kind, key, val in deps:
            if kind == 'e' and key == eng and skip_same:
                continue
            if kind == 'd':
                val = self.dcount[key]
            sk = (kind, key)
            if self.seen[eng].get(sk, -1) >= val:
                continue
            self.seen[eng][sk] = val
            self.ops[eng].append(('w', kind, key, val))
            if kind == 'e':
                self.targets[key].add(val)

    def _update(self, me, reads, writes):
        for k in writes:
            self.res[k] = [me, {}]
        for k in reads:
            r = self.res.setdefault(k, [None, {}])
            sk = (me[0], me[1])
            if r[1].get(sk, -1) < me[2]:
                r[1][sk] = me[2]

    def op(self, eng, fn, reads=(), writes=(), skip_same=False):
        self._waits(eng, self._deps(reads, writes), skip_same)
        idx = self.n[eng]
        self.n[eng] += 1
        self.ops[eng].append(('i', fn, idx))
        self._update(('e', eng, idx), reads, writes)

    def dma(self, eng, out, in_, reads=(), writes=(), slot=None, is_out=False):
        self._waits(eng, self._deps(reads, writes))
        c = self.dcount.get(slot, 0) + 16
        self.dcount[slot] = c
        self.ops[eng].append(('d', out, in_, slot))
        self._update(('d', slot, c), reads, writes)
        if is_out:
            self.out_slots.add(slot)

    def barrier(self):
        for e in ENG:
            deps = []
            for f in ENG:
                if f != e and self.n[f] > 0:
                    deps.append(('e', f, self.n[f] - 1))
            for s, c in self.dcount.items():
                deps.append(('d', s, c))
            self._waits(e, deps)

    def mm(self, out, lhsT, rhs, start, stop, reads=(), writes=()):
        self.op('pe', lambda h: h.matmul(out, lhsT, rhs, start=start, stop=stop), reads, writes, skip_same=True)

    def tr(self, out, in_, ident, reads=(), writes=()):
        self.op('pe', lambda h: h.transpose(out, in_, ident), reads, writes, skip_same=True)

    def actf(self, out, in_, func, bias=None, scale=None, reads=(), writes=(), eng='act'):
        kw = {}
        if bias is not None:
            kw['bias'] = bias
        if scale is not None:
            kw['scale'] = scale
        self.op(eng, lambda h: h.activation(out, in_, func, **kw), reads, writes)

    def tt(self, eng, out, in0, in1, op, reads=(), writes=()):
        self.op(eng, lambda h: h.tensor_tensor(out, in0, in1, op), reads, writes)

    def ts(self, eng, out, in0, s1, s2, op0, op1=None, reads=(), writes=()):
        if op1 is None:
            self.op(eng, lambda h: h.tensor_scalar(out, in0, s1, None, op0), reads, writes)
        else:
            self.op(eng, lambda h: h.tensor_scalar(out, in0, s1, s2, op0, op1), reads, writes)

    def stt(self, eng, out, in0, scalar, in1, op0, op1, reads=(), writes=()):
        self.op(eng, lambda h: h.scalar_tensor_tensor(out, in0, scalar, in1, op0, op1), reads, writes)

    def copy(self, eng, out, in_, reads=(), writes=()):
        if eng == 'act':
            self.op(eng, lambda h: h.copy(out, in_), reads, writes)
        else:
            self.op(eng, lambda h: h.tensor_copy(out, in_), reads, writes)

    def emit(self):
        nc = self.nc
        es = self.es
        sem = {e: es.enter_context(nc.semaphore("sem_" + e)) for e in ENG}
        dsem = {}
        for i, s in enumerate(self.dcount):
            dsem[s] = es.enter_context(nc.semaphore("dsem%d" % i))
        rank = {e: {idx: i + 1 for i, idx in enumerate(sorted(self.targets[e]))} for e in ENG}
        for s in sorted(self.out_slots, key=str):
            self.ops['sp'].append(('w', 'd', s, self.dcount[s]))
        block = es.enter_context(nc.Block())

        def run(e, h):
            for rec in self.ops[e]:
                if rec[0] == 'w':
                    _, kind, key, val = rec
                    if kind == 'e':
                        h.wait_ge(sem[key], rank[key][val])
                    else:
                        h.wait_ge(dsem[key], val)
                elif rec[0] == 'i':
                    ins = rec[1](h)
                    if rec[2] in rank[e]:
                        ins.then_inc(sem[e], 1)
                else:
                    _, out, in_, slot = rec
                    h.dma_start(out=out, in_=in_).then_inc(dsem[slot], 16)

        block.tensor(lambda h: run('pe', h))
        block.scalar(lambda h: run('act', h))
        block.vector(lambda h: run('dve', h))
        block.gpsimd(lambda h: run('pool', h))
        block.sync(lambda h: run('sp', h))
        print("PROG ops:", {e: len(self.ops[e]) for e in ENG}, "dsems", len(dsem))


CUT = 99
D = 4096
ALPHA = 2.0 ** 0.25
LN_EPS = 1e-5
MP, MC, MFP, MSN, IDM, BP, BC, BPF, BCF, BSA, BSB, BSM, MSC = 0, 1, 2, 3, 4, 5, 9, 13, 17, 21, 25, 29, 33
NMAT = 34
STAGES = 99
VAL_ENG = 'act'


def build_nc(stages=STAGES, groups=(0, 1, 2), skipA=False):
    nc = bass.Bass("TRN2", target_bir_lowering=False)
    dt_in = lambda n, s: nc.dram_tensor(n, s, F32, kind="ExternalInput").ap()
    dt_out = lambda n, s: nc.dram_tensor(n, s, F32, kind="ExternalOutput").ap()
    xT_d = dt_in("xT", [D, 1280])
    cT_d = dt_in("cT", [128, 32, 17])
    cmat_d = dt_in("cmat", [128, NMAT, 128])
    rope_d = dt_in("rope", [128, 10, 2, 8])
    vecs_d = dt_in("vecs", [128, 384])
    cKT_d = dt_in("cKT", [128, 16, 4, 128])
    cV_d = dt_in("cV", [128, 16, 4, 128])
    ckn_d = dt_in("ckn", [16, 128, 256])
    cvn_d = dt_in("cvn", [16, 128, 256])
    sptm_d = dt_in("sptm", [120, 2, 2048])
    spn_d = dt_in("spn", [16, 15, 2048])
    wada_d = dt_in("w_ada", [D, 24576] if not skipA else [128, 128])
    win_d = dt_in("w_in", [D, 5120])
    wout_d = dt_in("w_out", [D, D] if stages >= 4 else [128, 128])
    wq_d = dt_in("wq", [D, 2048] if stages >= 5 else [128, 128])
    subk_d = dt_in("subk", [128, 16, 128])
    UT_d = dt_in("UT", [D, 16384] if stages >= 5 else [128, 128])
    V_d = dt_in("V", [16384, D] if stages >= 5 else [128, 128])
    poolw_d = dt_in("poolw", [128, 4, 4, 512])
    yT_d = dt_out("yT", [D, 1152])
    nkp_d = dt_out("nkp", [128, 256])
    nvp_d = dt_out("nvp", [128, 256])
    npp_d = dt_out("npp", [15, 2048])
    nks_d = dt_out("nks", [16, 128, 256])
    nvs_d = dt_out("nvs", [16, 128, 256])
    nps_d = dt_out("nps", [16, 15, 2048])

    es = ExitStack()
    with es:
        P = Prog(nc, es)
        sb = lambda name, shape, dt: es.enter_context(nc.sbuf_tensor("s_" + name, shape, dt))
        ps = [es.enter_context(nc.psum_tensor("ps%d" % i, [128, 512], F32)) for i in range(8)]
        psk = lambda i: ('ps', i)

        cmat = sb("cmat", [128, NMAT, 128], BF16)
        rope = sb("rope", [128, 10, 2, 8], F32)
        vecs = sb("vecs", [128, 384], F32)
        modP = sb("modP", [128, 192], F32)
        modS = sb("modS", [128, 192, 16], F32)
        cT = sb("cTs", [128, 32, 17], F32)
        siluT = sb("siluT", [128, 32, 17], BF16)
        sinkE = sb("sinkE", [128, 32], F32)
        ones_b = sb("ones_b", [128, 128], BF16)
        ones_f = sb("ones_f", [128, 128], F32)
        W = [sb("W%d" % i, [128, 8192], BF16) for i in range(3)]
        R2 = sb("R2", [128, 12288], BF16)
        big = sb("big", [128, 12288], F32)
        S2 = sb("S2", [128, 14848], F32)

        b_adaT = vecs[:, 0:192]
        pscT = vecs[:, 192:208]
        ln1g, ln1b, ln2g, ln2b = (vecs[:, 208 + 32 * i:240 + 32 * i] for i in range(4))
        ident = cmat[:, IDM, :]

        class Carver:
            def __init__(self, t):
                self.t = t
                self.off = 0

            def get(self, shape, dt):
                n = int(np.prod(shape[1:]))
                nb = n * (2 if dt == BF16 else 4)
                nw = (nb + 3) // 4
                a = self.t[:, self.off:self.off + nw]
                self.off += nw
                assert self.off <= 14848, self.off
                if dt == BF16:
                    a = a.bitcast(BF16)[:, 0:n]
                if len(shape) == 3:
                    a = a.rearrange("p (a b) -> p a b", a=shape[1])
                elif len(shape) == 4:
                    a = a.rearrange("p (a b c) -> p a b c", a=shape[1], b=shape[2])
                return a

        wslot = [0]

        def wload(src_ap, view_shape, key_extra=None):
            s = wslot[0] % 3
            wslot[0] += 1
            n = int(np.prod(view_shape[1:]))
            a = W[s][0:view_shape[0], 0:n]
            if len(view_shape) == 3:
                a = a.rearrange("p (a b) -> p a b", a=view_shape[1])
            P.dma('pool', a, src_ap, writes=[('W', s)], slot=('W', s))
            return a, ('W', s)

        P.dma('pool', cmat[:], cmat_d, writes=['cmat'], slot='c_cmat')
        P.dma('sp', rope[:], rope_d, writes=['rope'], slot='c_rope')
        P.dma('sp', vecs[:], vecs_d, writes=['vecs'], slot='c_vecs')
        P.dma('sp', cT[:], cT_d, writes=['cT'], slot='c_cT')
        P.op('dve', lambda h: h.memset(ones_b[:], 1.0), writes=['ones_b'])
        P.op('dve', lambda h: h.memset(ones_f[:], 1.0), writes=['ones_f'])
        P.actf(siluT[:], cT[:], AF.Silu, reads=['cT'], writes=['siluT'])
        P.actf(sinkE[:], vecs[:, 336:368], AF.Exp, reads=['vecs'], writes=['sinkE'])
        P.dma('sp', nks_d[:, 0:120, :], ckn_d[:, 8:128, :], slot='o_misc', is_out=True)
        P.dma('sp', nvs_d[:, 0:120, :], cvn_d[:, 8:128, :], slot='o_misc', is_out=True)
        P.dma('sp', nps_d[:, 0:7, :], spn_d[:, 8:15, :], slot='o_misc', is_out=True)

        wada_v = wada_d.rearrange("(kc p) c -> p kc c", p=128)
        if skipA:
            P.op('dve', lambda h: h.memset(modP[:], 0.25), writes=['mod'])
            P.op('dve', lambda h: h.memset(modS[:], 0.25), writes=['mod'])
        WA = [big[:, 0:8192].bitcast(BF16).rearrange("p (a b) -> p a b", a=32),
              S2[:, 0:8192].bitcast(BF16).rearrange("p (a b) -> p a b", a=32)]
        for cg in range(0 if skipA else 48):
            s = cg % 2
            P.dma('pool', WA[s], wada_v[:, :, cg * 512:(cg + 1) * 512], writes=[('WA', s)], slot=('WA', s))
            bank = cg % 2
            pv = ps[bank][:, 0:68].rearrange("p (j n) -> p j n", j=4)
            for j in range(4):
                for kc in range(32):
                    P.mm(pv[:, j, :], WA[s][:, kc, j * 128:(j + 1) * 128], siluT[:, kc, :], kc == 0, kc == 31,
                         reads=[('WA', s), 'siluT'], writes=[psk(bank)])
            m0 = cg * 4
            P.tt('dve', modP[:, m0:m0 + 4].unsqueeze(2), pv[:, :, 0:1], b_adaT[:, m0:m0 + 4].unsqueeze(2), ALU.add,
                 reads=[psk(bank), 'vecs'], writes=['mod'])
            P.tt('dve', modS[:, m0:m0 + 4, :], pv[:, :, 1:17],
                 b_adaT[:, m0:m0 + 4].unsqueeze(2).to_broadcast([128, 4, 16]), ALU.add,
                 reads=[psk(bank), 'vecs'], writes=['mod'])
        for m in (1, 4):
            P.ts('dve', modP[:, m * 32:(m + 1) * 32], modP[:, m * 32:(m + 1) * 32], 1.0, None, ALU.add,
                 reads=['mod'], writes=['mod'])
            P.ts('dve', modS[:, m * 32:(m + 1) * 32, :], modS[:, m * 32:(m + 1) * 32, :], 1.0, None, ALU.add,
                 reads=['mod'], writes=['mod'])
        P.barrier()

        xv = xT_d.rearrange("(kc p) t -> p kc t", p=128)
        yv = yT_d.rearrange("(kc p) t -> p kc t", p=128)
        win_v = win_d.rearrange("(kc p) c -> p kc c", p=128)
        wout_v = wout_d.rearrange("(kc p) c -> p kc c", p=128)
        wq_v = wq_d.rearrange("(kc p) c -> p kc c", p=128)
        UT_v = UT_d.rearrange("(kc p) c -> p kc c", p=128)
        V_v = V_d.rearrange("(c p) f -> p c f", p=128)

        def bc_s(ap_b16, n=8):
            return ap_b16.unsqueeze(2).to_broadcast([ap_b16.shape[0], 16, n])

        pbank = [0]

        def nextbank(lo, hi):
            b = lo + pbank[0] % (hi - lo)
            pbank[0] += 1
            return b

        def ln_block(z, gcol, bcol, g, mod_post, stash):
            cv = Carver(S2)
            cv.off = 11000
            mean = cv.get([128, 384], F32)
            rstd = cv.get([128, 384], F32)
            tmpn = cv.get([128, 384], F32)
            P.op('act', lambda h: h.mul(mean, ps[6][:, 0:384], 1.0 / D), reads=[psk(6)], writes=['mean'])
            P.op('act', lambda h: h.mul(rstd, ps[7][:, 0:384], 1.0 / D), reads=[psk(7)], writes=['rstd'])
            P.tt('dve', tmpn, mean, mean, ALU.mult, reads=['mean'], writes=['tmpn'])
            P.tt('dve', rstd, rstd, tmpn, ALU.subtract, reads=['rstd', 'tmpn'], writes=['rstd'])
            P.ts('dve', rstd, rstd, LN_EPS, None, ALU.add, reads=['rstd'], writes=['rstd'])
            P.actf(rstd, rstd, AF.Sqrt, reads=['rstd'], writes=['rstd'])
            P.op('dve', lambda h: h.reciprocal(rstd, rstd), reads=['rstd'], writes=['rstd'])
            for fc in range(32):
                zk = ('z', fc)
                P.tt('dve', z[:, fc, :], z[:, fc, :], mean, ALU.subtract, reads=[zk, 'mean'], writes=[zk])
                P.tt('dve', z[:, fc, :], z[:, fc, :], rstd, ALU.mult, reads=[zk, 'rstd'], writes=[zk])
                P.ts('dve', z[:, fc, :], z[:, fc, :], gcol[:, fc:fc + 1], bcol[:, fc:fc + 1], ALU.mult, ALU.add,
                     reads=[zk, 'vecs'], writes=[zk])
                if mod_post is not None:
                    mod_post(fc)
                if stash is not None and fc % 4 == 3:
                    stash(fc - 3)

        for g in (groups if stages >= 2 else ()):
            c0 = 3 * g * 128
            smp = (g == 2)
            ncp = 384 if smp else 512
            own = [1, 2] if smp else [1, 2, 3]
            tiles_all = [0, 1, 2, 3]
            hT = big[:, 0:8192].bitcast(BF16).rearrange("p (a b) -> p a b", a=32)
            xs = big[:, 8192:12288].rearrange("p (s j t) -> p s j t", s=2, j=4)
            mixT = R2[:, :].rearrange("p (a b) -> p a b", a=32)
            cv = Carver(S2)
            KT = cv.get([128, 4, 512], BF16)
            Vtok = cv.get([128, 4, 512], BF16)
            stf = [cv.get([128, 512], F32) for _ in range(2)]
            stb = [cv.get([128, 512], BF16) for _ in range(2)]
            rt = cv.get([128, 4, 64], F32)
            QTz = cv.get([128, 2, 4, 384], BF16)
            Eb = cv.get([128, 2048], BF16)
            Ef = cv.get([128, 2048], F32)
            Rr = cv.get([128, 2, 512], F32)
            cK = cv.get([128, 16, 128], BF16)
            cVt = cv.get([128, 16, 128], BF16)
            Ec = cv.get([128, 1024], BF16)
            tmpS = cv.get([128, 128], F32)
            off_pool = cv.off
            P.op('dve', lambda h: h.memset(QTz, 0.0), writes=['QT'])
            for kq in range(8):
                sl = kq % 2
                P.dma('sp', xs[:, sl], xv[:, kq * 4:(kq + 1) * 4, c0:c0 + 512], writes=[('xs', sl)], slot=('xs', sl))
                for j in range(4):
                    kc = kq * 4 + j
                    if kc % 2 == 0:
                        P.ts('dve', hT[:, kc, 0:ncp], xs[:, sl, j, 0:ncp], modP[:, 32 + kc:33 + kc], modP[:, kc:kc + 1],
                             ALU.mult, ALU.add, reads=[('xs', sl), 'mod'], writes=[('hT', kc)])
                    else:
                        P.actf(hT[:, kc, 0:ncp], xs[:, sl, j, 0:ncp], AF.Identity, bias=modP[:, kc:kc + 1],
                               scale=modP[:, 32 + kc:33 + kc], reads=[('xs', sl), 'mod'], writes=[('hT', kc)])
                    if smp:
                        tv = tmpS.rearrange("p (b t) -> p b t", b=16)
                        P.tt('dve', tv, xs[:, sl, j, 384:512].rearrange("p (b t) -> p b t", b=16),
                             bc_s(modS[:, 32 + kc, :]), ALU.mult, reads=[('xs', sl), 'mod'], writes=['tmpS'])
                        P.tt('dve', hT[:, kc, 384:512].rearrange("p (b t) -> p b t", b=16), tv,
                             bc_s(modS[:, kc, :]), ALU.add, reads=['tmpS', 'mod'], writes=[('hT', kc)])
            if CUT == 1:
                P.barrier()
                continue

            def rope_inplace(f, gt):
                f3 = f.rearrange("p (h d) -> p h d", h=8)
                x1 = f3[:, :, 0:8]
                x2 = f3[:, :, 8:16]
                cs = rope[:, gt, 0, :].unsqueeze(1).to_broadcast([128, 8, 8])
                sn = rope[:, gt, 1, :].unsqueeze(1).to_broadcast([128, 8, 8])
                r = [rt[:, i, :].rearrange("p (h d) -> p h d", h=8) for i in range(4)]
                P.tt('dve', r[0], x1, cs, ALU.mult, reads=['stf', 'rope'], writes=['rt'])
                P.tt('dve', r[1], x2, sn, ALU.mult, reads=['stf', 'rope'], writes=['rt'])
                P.tt('dve', r[2], x2, cs, ALU.mult, reads=['stf', 'rope'], writes=['rt'])
                P.tt('dve', r[3], x1, sn, ALU.mult, reads=['stf', 'rope'], writes=['rt'])
                P.tt('dve', x1, r[0], r[1], ALU.subtract, reads=['rt'], writes=['stf'])
                P.tt('dve', x2, r[2], r[3], ALU.add, reads=['rt'], writes=['stf'])

            def inproj(cg, lts, evac):
                tl = []
                for half in range(2):
                    a, k = wload(win_v[:, :, cg * 512 + half * 256: cg * 512 + half * 256 + 256], [128, 32, 256])
                    tl.append((a, k))
                prev = None
                for lt in lts:
                    bank = nextbank(0, 4)
                    for half in range(2):
                        a, k = tl[half]
                        for kc in range(32):
                            P.mm(ps[bank][:, half * 256:(half + 1) * 256], hT[:, kc, lt * 128:(lt + 1) * 128], a[:, kc, :],
                                 kc == 0, kc == 31, reads=[('hT', kc), k], writes=[psk(bank)])
                    if prev is not None:
                        evac(*prev)
                    prev = (lt, bank)
                if prev is not None:
                    evac(*prev)

            def out_rows(dst_p, dst_s, src, lt, view=None):
                if not smp:
                    return
                if lt == 2 and dst_p is not None:
                    P.dma('sp', dst_p, src if view is None else view(src), reads=['stf'], slot='o_misc', is_out=True)
                if lt == 3:
                    for b in range(16):
                        s_ = src[b * 8:(b + 1) * 8]
                        P.dma('sp', dst_s(b), s_ if view is None else view(s_), reads=['stf'], slot='o_misc', is_out=True)

            kview = lambda a: a.rearrange("p (k c d) -> p k c d", k=4, c=2)[:, :, 0, :]

            def evac_k(lt, bank):
                gt = 3 * g + lt
                f = stf[0]
                P.copy('act', f, ps[bank][:, :], reads=[psk(bank)], writes=['stf'])
                if CUT == 3:
                    return
                rope_inplace(f, gt)
                P.copy('act', stb[0], f, reads=['stf'], writes=['stb'])
                if CUT == 4:
                    return
                pb = ps[4 + lt % 2][:].bitcast(BF16)
                for kv in range(4):
                    P.tr(pb[:, kv * 128:(kv + 1) * 128], stb[0][:, kv * 128:(kv + 1) * 128], ident,
                         reads=['stb', 'cmat'], writes=[psk(4 + lt % 2)])
                P.copy('dve', KT[:, :, lt * 128:(lt + 1) * 128], pb[:, 0:512].rearrange("p (k t) -> p k t", k=4),
                       reads=[psk(4 + lt % 2)], writes=['KT'])
                out_rows(nkp_d.rearrange("p (k d) -> p k d", k=4),
                         lambda b: nks_d[b, 120:128, :].rearrange("p (k d) -> p k d", k=4), f, lt, kview)
            inproj(0, tiles_all, evac_k)
            if CUT <= 5:
                P.barrier()
                continue

            def evac_v(lt, bank):
                P.copy('dve', Vtok[:, lt, :], ps[bank][:, :], reads=[psk(bank)], writes=[('Vtok', lt)])
                if smp and lt >= 2:
                    f = stf[0]
                    P.copy('dve', f, ps[bank][:, :], reads=[psk(bank)], writes=['stf'])
                    out_rows(nvp_d.rearrange("p (k d) -> p k d", k=4),
                             lambda b: nvs_d[b, 120:128, :].rearrange("p (k d) -> p k d", k=4), f, lt, kview)
            inproj(1, tiles_all, evac_v)
            if CUT == 6:
                P.barrier()
                continue

            for kvh in range(4 if stages >= 3 else 0):
                def evac_q(lt, bank):
                    gt = 3 * g + lt
                    f = stf[1]
                    P.copy('act', f, ps[bank][:, :], reads=[psk(bank)], writes=['stf'])
                    rope_inplace(f, gt)
                    P.copy('act', stb[1], f, reads=['stf'], writes=['stb'])
                    pb = ps[4 + lt % 2][:].bitcast(BF16)
                    for pc in range(4):
                        P.tr(pb[:, pc * 128:(pc + 1) * 128], stb[1][:, pc * 128:(pc + 1) * 128], ident,
                             reads=['stb', 'cmat'], writes=[psk(4 + lt % 2)])
                    for c in range(2):
                        hs = slice(c * 64, (c + 1) * 64)
                        P.copy('dve', QTz[hs, c, :, (lt - 1) * 128:lt * 128], pb[hs, 0:512].rearrange("p (k t) -> p k t", k=4),
                               reads=[psk(4 + lt % 2)], writes=['QT'])
                inproj(2 + kvh, [1, 2, 3], evac_q)
                if smp:
                    P.dma('pool', cK, cKT_d[:, :, kvh, :], writes=['cK'], slot='cK')
                    P.dma('pool', cVt, cV_d[:, :, kvh, :], writes=['cV'], slot='cV')
                for lt in [1, 2, 3]:
                    if CUT == 7:
                        break
                    qc = (lt - 1) * 128
                    is_s = smp and lt == 3
                    vsl = slice(kvh * 128, (kvh + 1) * 128)
                    if not is_s:
                        for kt, ktile in enumerate([lt - 1, lt]):
                            for c in range(2):
                                bank = kt * 2 + c
                                hs = slice(c * 64, (c + 1) * 64)
                                P.mm(ps[bank][:, :].rearrange("p (a b) -> p a b", a=4),
                                     KT[:, kvh, ktile * 128:(ktile + 1) * 128], QTz[:, c, :, qc:qc + 128], True, True,
                                     reads=['KT', 'QT'], writes=[psk(bank)])
                                if CUT == 8:
                                    continue
                                P.actf(Ef[:, bank * 512:(bank + 1) * 512], ps[bank][:, :], AF.Exp, scale=0.125,
                                       reads=[psk(bank)], writes=[('Ef', bank)])
                            if CUT in (8, 9):
                                continue
                            midx = MC if kt == 1 else (MFP if (g == 0 and lt == 1) else MP)
                            ev = Eb[:, kt * 1024:(kt + 1) * 1024].rearrange("p (a b) -> p a b", a=8)
                            efv = Ef[:, kt * 1024:(kt + 1) * 1024].rearrange("p (a b) -> p a b", a=8)
                            P.tt('dve', ev, efv, cmat[:, midx, :].unsqueeze(1).to_broadcast([128, 8, 128]), ALU.mult,
                                 reads=[('Ef', kt * 2), ('Ef', kt * 2 + 1), 'cmat'], writes=[('Eb', kt * 2), ('Eb', kt * 2 + 1)])
                        for c in range(2):
                            if CUT in (8, 9, 10):
                                continue
                            P.mm(ps[6 + c][:, :], ones_b[:], Eb[:, c * 512:(c + 1) * 512], True, False,
                                 reads=[('Eb', c), 'ones_b'], writes=[psk(6 + c)])
                            P.mm(ps[6 + c][:, :], ones_b[:], Eb[:, (2 + c) * 512:(3 + c) * 512], False, True,
                                 reads=[('Eb', 2 + c)], writes=[psk(6 + c)])
                            P.mm(ps[4 + c][:, :], Vtok[:, lt - 1, vsl], Eb[:, c * 512:(c + 1) * 512], True, False,
                                 reads=[('Eb', c), ('Vtok', lt - 1 if not is_s else 3)], writes=[psk(4 + c)])
                            P.mm(ps[4 + c][:, :], Vtok[:, lt, vsl], Eb[:, (2 + c) * 512:(3 + c) * 512], False, True,
                                 reads=[('Eb', 2 + c), ('Vtok', lt)], writes=[psk(4 + c)])
                    else:
                        for c in range(2):
                            hs = slice(c * 64, (c + 1) * 64)
                            P.mm(ps[c][:, :].rearrange("p (a b) -> p a b", a=4), KT[:, kvh, 384:512], QTz[:, c, :, qc:qc + 128],
                                 True, True, reads=['KT', 'QT'], writes=[psk(c)])
                            P.actf(Ef[:, c * 512:(c + 1) * 512], ps[c][:, :], AF.Exp, scale=0.125,
                                   reads=[psk(c)], writes=[('Ef', c)])
                        ev = Eb[:, 0:1024].rearrange("p (a b) -> p a b", a=8)
                        efv = Ef[:, 0:1024].rearrange("p (a b) -> p a b", a=8)
                        P.tt('dve', ev, efv, cmat[:, MSN, :].unsqueeze(1).to_broadcast([128, 8, 128]), ALU.mult,
                             reads=[('Ef', 0), ('Ef', 1), 'cmat'], writes=[('Eb', 0), ('Eb', 1)])
                        for b in range(16):
                            for c in range(2):
                                hs = slice(c * 64, (c + 1) * 64)
                                off = ((b % 8) * 2 + c) * 32
                                P.mm(ps[2 + b // 8][:, off:off + 32].rearrange("p (a t) -> p a t", a=4), cK[:, b, :],
                                     QTz[:, c, :, qc + b * 8:qc + b * 8 + 8], True, True,
                                     reads=['cK', 'QT'], writes=[psk(2 + b // 8)])
                        for hb in range(2):
                            P.actf(Ef[:, 1024 + hb * 512:1024 + (hb + 1) * 512], ps[2 + hb][:, :], AF.Exp, scale=0.125,
                                   reads=[psk(2 + hb)], writes=[('Ef', 2 + hb)])
                        ecv = Ec.rearrange("p (a t) -> p a t", t=8)
                        efv = Ef[:, 1024:2048].rearrange("p (a t) -> p a t", t=8)
                        P.tt('dve', ecv, efv, cmat[:, MSC, 0:8].unsqueeze(1).to_broadcast([128, 128, 8]), ALU.mult,
                             reads=[('Ef', 2), ('Ef', 3), 'cmat'], writes=['Ec'])
                        Ec4 = Ec.rearrange("p (b c a t) -> p b c a t", b=16, c=2, a=4)
                        for c in range(2):
                            pperm = lambda bk: ps[bk][:, :].rearrange("p (a b t) -> p b a t", a=4, b=16)
                            P.mm(ps[6 + c][:, :], ones_b[:], Eb[:, c * 512:(c + 1) * 512], True, False,
                                 reads=[('Eb', c), 'ones_b'], writes=[psk(6 + c)])
                            P.mm(pperm(6 + c), ones_b[:], Ec4[:, :, c, :, :], False, True,
                                 reads=['Ec'], writes=[psk(6 + c)])
                            P.mm(ps[4 + c][:, :], Vtok[:, 3, vsl], Eb[:, c * 512:(c + 1) * 512], True, False,
                                 reads=[('Eb', c), ('Vtok', lt - 1 if not is_s else 3)], writes=[psk(4 + c)])
                            for b in range(16):
                                P.mm(pperm(4 + c)[:, b, :, :], cVt[:, b, :], Ec4[:, b, c, :, :], False, b == 15,
                                     reads=['Ec', 'cV'], writes=[psk(4 + c)])
                    for c in range(2):
                        if CUT in (8, 9, 10, 11):
                            continue
                        hs = slice(c * 64, (c + 1) * 64)
                        rv = Rr[hs, c, :].rearrange("p (a b) -> p a b", a=4)
                        sk = sinkE[hs, c * 16 + kvh * 4: c * 16 + kvh * 4 + 4].unsqueeze(2).to_broadcast([64, 4, 128])
                        P.tt('dve', rv, ps[6 + c][hs, :].rearrange("p (a b) -> p a b", a=4), sk, ALU.add,
                             reads=[psk(6 + c), 'sinkE'], writes=['Rr'])
                        P.op('dve', lambda h, rv=rv: h.reciprocal(rv, rv), reads=['Rr'], writes=['Rr'])
                        P.tt('dve', mixT[hs, kvh * 4:(kvh + 1) * 4, qc:qc + 128],
                             ps[4 + c][hs, :].rearrange("p (a b) -> p a b", a=4), rv, ALU.mult,
                             reads=[psk(4 + c), 'Rr'], writes=['mixT'])

            if CUT in (7, 8, 9, 10, 11, 12):
                P.barrier()
                continue
            cv.off = off_pool
            Utok = cv.get([128, 4, 512], BF16)
            dT = cv.get([128, 4, 384], BF16)
            SPt = cv.get([128, 2, 512], BF16)
            for g4 in range(4 if stages >= 3 else (4 if stages >= 2 else 0)):
                def evac_u(lt, bank):
                    P.copy('dve', Utok[:, lt, :], ps[bank][:, :], reads=[psk(bank)], writes=[('Utok', lt)])
                    if smp and lt >= 2:
                        f = stf[0]
                        P.copy('dve', f, ps[bank][:, :], reads=[psk(bank)], writes=['stf'])
                        if lt == 2:
                            P.dma('sp', npp_d[:, g4 * 512:(g4 + 1) * 512], f[113:128, :], reads=['stf'], slot='o_misc', is_out=True)
                        else:
                            for b in range(16):
                                P.dma('sp', nps_d[b, 7:15, g4 * 512:(g4 + 1) * 512], f[b * 8:(b + 1) * 8, :], reads=['stf'],
                                      slot='o_misc', is_out=True)
                inproj(6 + g4, tiles_all, evac_u)
                if stages < 3:
                    continue
                Wp, wpk = wload(poolw_d[:, g4], [128, 4, 512])
                if smp:
                    P.dma('pool', SPt[0:120], sptm_d[:, :, g4 * 512:(g4 + 1) * 512], writes=['SPt'], slot='SPt')
                for lt in [1, 2, 3]:
                    bank = nextbank(0, 2)
                    first = (g == 0 and lt == 1)
                    for cc in range(4):
                        o = ps[bank][:, cc * 128:(cc + 1) * 128]
                        csl = slice(cc * 128, (cc + 1) * 128)
                        if smp and lt == 3:
                            P.mm(o, SPt[0:120, 0, csl], cmat[0:120, BSA + g4, :], True, False, reads=['SPt', 'cmat'], writes=[psk(bank)])
                            P.mm(o, SPt[0:120, 1, csl], cmat[0:120, BSB + g4, :], False, False, reads=['SPt'], writes=[psk(bank)])
                            P.mm(o, Utok[:, 3, csl], cmat[:, BSM + g4, :], False, True, reads=[('Utok', 3)], writes=[psk(bank)])
                        else:
                            P.mm(o, Utok[:, lt - 1, csl], cmat[:, (BPF if first else BP) + g4, :], True, False,
                                 reads=[('Utok', lt - 1), 'cmat'], writes=[psk(bank)])
                            P.mm(o, Utok[:, lt, csl], cmat[:, (BCF if first else BC) + g4, :], False, True,
                                 reads=[('Utok', lt)], writes=[psk(bank)])
                    P.copy('dve', dT[:, :, (lt - 1) * 128:lt * 128], ps[bank][:, :].rearrange("p (a b) -> p a b", a=4),
                           reads=[psk(bank)], writes=['dT'])
                for dc in range(4):
                    bank = nextbank(2, 4)
                    for cc in range(4):
                        P.mm(ps[bank][:, 0:384], Wp[:, cc, dc * 128:(dc + 1) * 128], dT[:, cc, :], cc == 0, cc == 3,
                             reads=['dT', wpk], writes=[psk(bank)])
                    fcm = 16 + g4 * 4 + dc
                    P.ts('dve', mixT[:, fcm, :], ps[bank][:, 0:384], pscT[:, g4 * 4 + dc:g4 * 4 + dc + 1], None, ALU.mult,
                         reads=[psk(bank), 'vecs'], writes=['mixT'])
            if stages < 4:
                P.barrier()
                continue

            P.barrier()
            z = big[:, :].rearrange("p (a b) -> p a b", a=32)
            h2T = R2[:, :].rearrange("p (a b) -> p a b", a=32)
            cv = Carver(S2)
            sqt = [cv.get([128, 384], F32) for _ in range(2)]
            tmpz = cv.get([128, 128], F32)
            oc0 = 3 * g * 128
            for kq in range(8):
                P.dma('sp', z[:, kq * 4:(kq + 1) * 4, :], xv[:, kq * 4:(kq + 1) * 4, c0 + 128:c0 + 512],
                      writes=[('z', kq * 4 + j) for j in range(4)], slot=('zl', kq))
                for j in range(4):
                    fc = kq * 4 + j
                    P.op('act', lambda h, fc=fc: h.mul(z[:, fc, :], z[:, fc, :], ALPHA), reads=[('z', fc)], writes=[('z', fc)])

            def z_accum(fc, src, gbase, srckey):
                zk = ('z', fc)
                P.stt('dve', z[:, fc, 0:ncp - 128], src[:, 0:ncp - 128], modP[:, gbase + fc:gbase + fc + 1], z[:, fc, 0:ncp - 128],
                      ALU.mult, ALU.add, reads=[srckey, zk, 'mod'], writes=[zk])
                if smp:
                    tv = tmpz.rearrange("p (b t) -> p b t", b=16)
                    P.tt('dve', tv, src[:, 256:384].rearrange("p (b t) -> p b t", b=16), bc_s(modS[:, gbase + fc, :]), ALU.mult,
                         reads=[srckey, 'mod'], writes=['tmpz'])
                    P.tt('dve', z[:, fc, 256:384], z[:, fc, 256:384], tmpz, ALU.add, reads=['tmpz', zk], writes=[zk])
                sq = sqt[fc % 2]
                P.actf(sq, z[:, fc, :], AF.Square, reads=[zk], writes=[('sqt', fc % 2)])
                zpe.append(fc)

            zpe = []

            def z_pe():
                while zpe:
                    fc = zpe.pop(0)
                    P.mm(ps[6][:, 0:384], ones_f[:], z[:, fc, :], fc == 0, fc == 31, reads=[('z', fc), 'ones_f'], writes=[psk(6)])
                    P.mm(ps[7][:, 0:384], ones_f[:], sqt[fc % 2], fc == 0, fc == 31, reads=[('sqt', fc % 2)], writes=[psk(7)])

            for wt in range(16):
                a, k = wload(wout_v[:, :, wt * 256:(wt + 1) * 256], [128, 32, 256])
                for j in range(2):
                    fc = wt * 2 + j
                    bank = nextbank(0, 4)
                    for kc in range(32):
                        P.mm(ps[bank][:, 0:384], a[:, kc, j * 128:(j + 1) * 128], mixT[:, kc, :], kc == 0, kc == 31,
                             reads=['mixT', k], writes=[psk(bank)])
                    z_pe()
                    z_accum(fc, ps[bank], 64, psk(bank))
            z_pe()

            def post1(fc):
                zk = ('z', fc)
                P.actf(h2T[:, fc, 0:ncp - 128], z[:, fc, 0:ncp - 128], AF.Identity, bias=modP[:, 96 + fc:97 + fc],
                       scale=modP[:, 128 + fc:129 + fc], reads=[zk, 'mod'], writes=[('h2T', fc)])
                if smp:
                    tv = tmpz.rearrange("p (b t) -> p b t", b=16)
                    P.tt('dve', tv, z[:, fc, 256:384].rearrange("p (b t) -> p b t", b=16), bc_s(modS[:, 128 + fc, :]), ALU.mult,
                         reads=[zk, 'mod'], writes=['tmpz'])
                    P.tt('dve', h2T[:, fc, 256:384].rearrange("p (b t) -> p b t", b=16), tv, bc_s(modS[:, 96 + fc, :]), ALU.add,
                         reads=['tmpz', 'mod'], writes=[('h2T', fc)])
                P.op('act', lambda h: h.mul(z[:, fc, :], z[:, fc, :], ALPHA), reads=[zk], writes=[zk])

            def stash1(fc0):
                P.dma('sp', yv[:, fc0:fc0 + 4, oc0:oc0 + 384], z[:, fc0:fc0 + 4, :], reads=[('z', fc0 + j) for j in range(4)],
                      writes=[('yst', fc0)], slot=('yst', fc0))
            ln_block(z, ln1g, ln1b, g, post1, stash1)
            P.barrier()
            if stages < 5:
                continue

            y2T = big[:, :].rearrange("p (a b) -> p a b", a=32)
            cv = Carver(S2)
            s_sb = cv.get([128, 3, 16, 128], F32)
            tau = cv.get([128, 3, 8], F32)
            off_tmp = cv.off
            qT = cv.get([128, 16, 384], BF16)
            t16 = cv.get([128, 16, 16], F32)
            wk = cv.get([128, 256], F32)
            cand = cv.get([128, 8, 256], F32)
            b16 = cv.get([128, 8, 16], F32)
            smal = cv.get([128, 8, 8], F32)
            for wt in range(8):
                a, k = wload(wq_v[:, :, wt * 256:(wt + 1) * 256], [128, 32, 256])
                for j in range(2):
                    pc = wt * 2 + j
                    bank = nextbank(0, 4)
                    for kc in range(32):
                        P.mm(ps[bank][:, 0:384], a[:, kc, j * 128:(j + 1) * 128], h2T[:, kc, :], kc == 0, kc == 31,
                             reads=[('h2T', kc), k], writes=[psk(bank)])
                    P.copy('dve', qT[:, pc, :], ps[bank][:, 0:384], reads=[psk(bank)], writes=['qT'])
            subk, subk_key = wload(subk_d, [128, 16, 128])
            for t in range(3):
                for pc in range(16):
                    bk = 4 + pc // 4
                    P.mm(ps[bk][:, (pc % 4) * 128:(pc % 4 + 1) * 128], qT[:, pc, t * 128:(t + 1) * 128], subk[:, pc, :], True, True,
                         reads=['qT', subk_key], writes=[psk(bk)])
                for q4 in range(4):
                    P.copy('act' if q4 % 2 else 'dve', s_sb[:, t, q4 * 4:(q4 + 1) * 4, :],
                           ps[4 + q4][:, :].rearrange("p (a b) -> p a b", a=4), reads=[psk(4 + q4)], writes=['s_sb'])
                for pc in range(16):
                    P.op('dve', lambda h, pc=pc, t=t: h.max(t16[:, pc, 0:8], s_sb[:, t, pc, :]), reads=['s_sb'], writes=['t16'])
                    P.op('dve', lambda h, pc=pc, t=t: h.match_replace(wk[:, 0:128], t16[:, pc, 0:8], s_sb[:, t, pc, :], -1e30),
                         reads=['s_sb', 't16'], writes=['wk'])
                    P.op('dve', lambda h, pc=pc: h.max(t16[:, pc, 8:16], wk[:, 0:128]), reads=['wk'], writes=['t16'])
                t16v = t16.rearrange("p (h q) k -> p h q k", q=2)
                s_v = s_sb[:, t].rearrange("p (h q) k -> p h q k", q=2)

                def cand_top(tag):
                    P.tt('dve', cand.rearrange("p h (i j) -> p h i j", i=16),
                         t16v[:, :, 0, :].unsqueeze(3).to_broadcast([128, 8, 16, 16]),
                         t16v[:, :, 1, :].unsqueeze(2).to_broadcast([128, 8, 16, 16]), ALU.add, reads=['t16'], writes=['cand'])
                    for hh in range(8):
                        P.op('dve', lambda h, hh=hh: h.max(b16[:, hh, 0:8], cand[:, hh, :]), reads=['cand'], writes=['b16'])
                        P.op('dve', lambda h, hh=hh: h.match_replace(wk[:, :], b16[:, hh, 0:8], cand[:, hh, :], -1e30),
                             reads=['cand', 'b16'], writes=['wk'])
                        P.op('dve', lambda h, hh=hh: h.max(b16[:, hh, 8:16], wk[:, :]), reads=['wk'], writes=['b16'])
                cand_top(0)
                mcol = smal[:, 0, :]
                zcol = smal[:, 1, :]
                P.copy('dve', mcol, b16[:, :, 0], reads=['b16'], writes=['smal'])
                P.tt('dve', b16[:, :, :], b16[:, :, :], mcol.unsqueeze(2).to_broadcast([128, 8, 16]), ALU.subtract,
                     reads=['b16', 'smal'], writes=['b16'])
                P.actf(b16[:, :, :], b16[:, :, :], AF.Exp, reads=['b16'], writes=['b16'])
                P.op('dve', lambda h: h.reduce_sum(zcol, b16[:, :, :], AX.X), reads=['b16'], writes=['smal'])
                P.actf(zcol, zcol, AF.Ln, reads=['smal'], writes=['smal'])
                P.tt('dve', zcol, zcol, mcol, ALU.add, reads=['smal'], writes=['smal'])
                P.tt('dve', s_v[:, :, 1, :], s_v[:, :, 1, :], zcol.unsqueeze(2).to_broadcast([128, 8, 128]), ALU.subtract,
                     reads=['s_sb', 'smal'], writes=['s_sb'])
                P.tt('dve', t16v[:, :, 1, :], t16v[:, :, 1, :], zcol.unsqueeze(2).to_broadcast([128, 8, 16]), ALU.subtract,
                     reads=['t16', 'smal'], writes=['t16'])
                cand_top(1)
                P.copy('dve', tau[:, t, :], b16[:, :, 15], reads=['b16'], writes=['tau'])
            cv.off = off_tmp
            P.barrier()
            actT = [cv.get([128, 8, 384], BF16) for _ in range(2)]
            gel = [cv.get([128, 384], F32) for _ in range(2)]
            valb = [cv.get([128, 8, 128], F32) for _ in range(2)]
            Ebf = [cv.get([128, 8, 128], BF16) for _ in range(2)]
            maskb = [cv.get([128, 8, 128], BF16) for _ in range(2)]
            Gb = Ebf
            Uslot = [W[0][:, :].rearrange("p (a b) -> p a b", a=32), W[1][:, :].rearrange("p (a b) -> p a b", a=32)]
            Vslot = [W[2][:, 0:4096].rearrange("p (a b) -> p a b", a=8), W[2][:, 4096:8192].rearrange("p (a b) -> p a b", a=8)]
            NCH = 128
            ucount = [0]
            pend = [None]
            finp = [None]

            def stage1(i, t, sl):
                s_v = s_sb[:, t].rearrange("p (h q) k -> p h q k", q=2)
                if t == 2:
                    for hh in range(8):
                        P.actf(valb[sl][:, hh, :], s_v[:, hh, 1, :], AF.Identity, bias=s_v[:, hh, 0, i:i + 1],
                               reads=['s_sb'], writes=[('valb', sl)])
                else:
                    P.tt('dve', valb[sl], s_v[:, :, 1, :], s_v[:, :, 0, i:i + 1].to_broadcast([128, 8, 128]), ALU.add,
                         reads=['s_sb'], writes=[('valb', sl)])
                P.actf(Ebf[sl], valb[sl], AF.Exp, reads=[('valb', sl)], writes=[('Ebf', sl)])

            def stage2(i, t, sl):
                gb = 2 + i % 2
                P.tt('dve', maskb[sl], valb[sl], tau[:, t, :].unsqueeze(2).to_broadcast([128, 8, 128]), ALU.is_ge,
                     reads=[('valb', sl), 'tau'], writes=[('maskb', sl)])
                P.tt('dve', Gb[sl], maskb[sl], Ebf[sl], ALU.mult, reads=[('maskb', sl), ('Ebf', sl)], writes=[('Ebf', sl)])
                for hh in range(8):
                    P.mm(ps[gb][:, t * 128:(t + 1) * 128], Gb[sl][:, hh, :], ident, hh == 0, hh == 7,
                         reads=[('Ebf', sl), 'cmat'], writes=[psk(gb)])

            def finalize(i):
                EB, ci = i // 8, i % 8
                P.tt('dve', actT[EB % 2][:, ci, :], ps[2 + i % 2][:, 0:384], gel[i % 2], ALU.mult,
                     reads=[psk(2 + i % 2), ('gel', i % 2)], writes=[('actT', EB % 2, ci)])

            def step_unit(i, t):
                sl = ucount[0] % 2
                ucount[0] += 1
                stage1(i, t, sl)
                if finp[0] is not None and t == 2:
                    finalize(finp[0])
                    finp[0] = None
                if pend[0] is not None:
                    pi, pt, psl = pend[0]
                    stage2(pi, pt, psl)
                    if pt == 2:
                        finp[0] = pi
                pend[0] = (i, t, sl)
                emit_one_add()

            def flush_units():
                if finp[0] is not None:
                    finalize(finp[0])
                    finp[0] = None
                if pend[0] is not None:
                    pi, pt, psl = pend[0]
                    stage2(pi, pt, psl)
                    if pt == 2:
                        finalize(pi)
                    pend[0] = None

            vcount = [0]
            ypend = []

            def emit_one_add():
                if not ypend:
                    return
                fc, yb, first = ypend.pop(0)
                if first:
                    P.copy('dve', y2T[:, fc, :], ps[yb][:, 0:384], reads=[psk(yb)], writes=[('z', fc)])
                else:
                    P.tt('dve', y2T[:, fc, :], y2T[:, fc, :], ps[yb][:, 0:384], ALU.add, reads=[psk(yb), ('z', fc)],
                         writes=[('z', fc)])

            def vpart(v):
                EB, fq = v // 8, v % 8
                while ypend:
                    emit_one_add()
                vs = vcount[0] % 2
                vcount[0] += 1
                va = Vslot[vs]
                P.dma('pool', va, V_v[:, EB * 8:(EB + 1) * 8, fq * 512:(fq + 1) * 512], writes=[('Wv', vs)], slot=('Wv', vs))
                for fj in range(4):
                    fc = fq * 4 + fj
                    yb = 4 + fj
                    for ci in range(8):
                        P.mm(ps[yb][:, 0:384], va[:, ci, fj * 128:(fj + 1) * 128], actT[EB % 2][:, ci, :], ci == 0, ci == 7,
                             reads=[('actT', EB % 2, ci), ('Wv', vs)], writes=[psk(yb)])
                    ypend.append((fc, yb, EB == 0))

            VLAG = 9
            abank = [0, 1]
            for i in range(NCH):
                if i % 2 == 0:
                    us = (i // 2) % 2
                    P.dma('pool', Uslot[us], UT_v[:, :, i * 128:(i + 2) * 128], writes=[('W', us)], slot=('W', us))
                us = (i // 2) % 2
                j = i % 2
                ab = nextbank(0, 2)
                for kc in range(32):
                    P.mm(ps[ab][:, 0:384], Uslot[us][:, kc, j * 128:(j + 1) * 128], h2T[:, kc, :], kc == 0, kc == 31,
                         reads=[('h2T', kc), ('W', us)], writes=[psk(ab)])
                abank[i % 2] = ab
                if i % 2 == 1:
                    for ii in (i - 1, i):
                        P.actf(gel[ii % 2], ps[abank[ii % 2]][:, 0:384], AF.Gelu, reads=[psk(abank[ii % 2])], writes=[('gel', ii % 2)])
                if i >= VLAG:
                    vpart(i - VLAG)
                for t in range(3):
                    step_unit(i, t)
            flush_units()
            for v in range(NCH - VLAG, NCH):
                vpart(v)
            while ypend:
                emit_one_add()
            P.barrier()
            cv = Carver(S2)
            sqt = [cv.get([128, 384], F32) for _ in range(2)]
            tmpz = cv.get([128, 128], F32)
            xst = [cv.get([128, 4, 384], F32) for _ in range(2)]
            for kq in range(8):
                sl = kq % 2
                P.dma('sp', xst[sl], yv[:, kq * 4:(kq + 1) * 4, oc0:oc0 + 384], reads=[('yst', kq * 4)], writes=[('xst', sl)],
                      slot=('xst', sl))
                for j in range(4):
                    fc = kq * 4 + j
                    zk = ('z', fc)
                    P.stt('dve', y2T[:, fc, 0:ncp - 128], y2T[:, fc, 0:ncp - 128], modP[:, 160 + fc:161 + fc], xst[sl][:, j, 0:ncp - 128],
                          ALU.mult, ALU.add, reads=[zk, ('xst', sl), 'mod'], writes=[zk])
                    if smp:
                        tv = tmpz.rearrange("p (b t) -> p b t", b=16)
                        P.tt('dve', tv, y2T[:, fc, 256:384].rearrange("p (b t) -> p b t", b=16), bc_s(modS[:, 160 + fc, :]), ALU.mult,
                             reads=[zk, 'mod'], writes=['tmpz'])
                        P.tt('dve', y2T[:, fc, 256:384], tmpz, xst[sl][:, j, 256:384], ALU.add, reads=['tmpz', ('xst', sl)], writes=[zk])
                    sq = sqt[fc % 2]
                    P.actf(sq, y2T[:, fc, :], AF.Square, reads=[zk], writes=[('sqt', fc % 2)])
                    P.mm(ps[6][:, 0:384], ones_f[:], y2T[:, fc, :], fc == 0, fc == 31, reads=[zk, 'ones_f'], writes=[psk(6)])
                    P.mm(ps[7][:, 0:384], ones_f[:], sq, fc == 0, fc == 31, reads=[('sqt', fc % 2)], writes=[psk(7)])

            def stash2(fc0):
                P.dma('sp', yv[:, fc0:fc0 + 4, oc0:oc0 + 384], y2T[:, fc0:fc0 + 4, :], reads=[('z', fc0 + j) for j in range(4)],
                      writes=[('yst', fc0)], slot='o_y', is_out=True)
            ln_block(y2T, ln2g, ln2b, g, None, stash2)
            P.barrier()
        P.emit()
    return nc


def _bf(x):
    return np.ascontiguousarray(x, dtype=np.float32)


def _consts(hf):
    cm = np.zeros((NMAT, 128, 128), np.float32)
    k = np.arange(128)[:, None]
    q = np.arange(128)[None, :]
    cm[MP] = (k >= q)
    cm[MC] = (k <= q)
    cm[MFP] = cm[MP] if hf == 1 else 0.0
    cm[MSN] = ((k // 8) == (q // 8)) & ((k % 8) <= (q % 8))
    cm[IDM] = np.eye(128)
    cm[MSC, :, 0:8] = (np.arange(128)[:, None] >= np.arange(8)[None, :])
    for g4, w in enumerate((2, 4, 8, 16)):
        tp = np.arange(128)[:, None]
        t = np.arange(128)[None, :]
        cur = ((tp <= t) & (tp > t - w)).astype(np.float32)
        prv = (tp - 128 > t - w).astype(np.float32)
        cm[BP + g4] = prv / w
        cm[BC + g4] = cur / w - np.eye(128)
        if hf == 1:
            cm[BPF + g4] = cm[BP + g4]
            cm[BCF + g4] = cm[BC + g4]
        else:
            cnt = np.minimum(w, t + 1).astype(np.float32)
            cm[BPF + g4] = 0.0
            cm[BCF + g4] = cur / cnt - np.eye(128)
        bq, tq = np.arange(128)[None, :] // 8, np.arange(128)[None, :] % 8
        r_b, r_r = np.arange(120)[:, None] // 15, np.arange(120)[:, None] % 15
        for half, idx in ((0, BSA), (1, BSB)):
            m = ((r_b + 8 * half) == bq) & (r_r > 15 + tq - w)
            cm[idx + g4, 0:120] = m / w
        bk, tk = np.arange(128)[:, None] // 8, np.arange(128)[:, None] % 8
        m = (bk == bq) & (tk <= tq) & (tk > tq - w)
        cm[BSM + g4] = m / w - np.eye(128)
    return np.ascontiguousarray(cm.transpose(1, 0, 2))


def _rope_tab(hf):
    half = 8
    inv = (np.float32(500000.0) ** (-np.arange(half, dtype=np.float32) / np.float32(half))).astype(np.float32)
    tab = np.zeros((128, 10, 2, 8), np.float32)
    for gt in range(10):
        if gt < 9:
            pos = hf * 1024 + (gt - 1) * 128 + np.arange(128)
        else:
            pos = 8192 + (np.arange(128) % 8)
        ang = pos.astype(np.float32)[:, None] * inv[None, :]
        tab[:, gt, 0, :] = np.cos(ang)
        tab[:, gt, 1, :] = np.sin(ang)
    return tab


_NC_CACHE = {}


def _prep(x_prompt, x_sample, cache_k, cache_v, state_pool, c_prompt, c_sample, w_ada, b_ada, w_in,
          sinks, pool_w, pool_scale, w_out, ln1_g, ln1_b, peer_wq, peer_subkeys, peer_u, peer_v,
          ln2_g, ln2_b):
    f = lambda a: np.asarray(a, dtype=np.float32)
    x_prompt, x_sample, cache_k, cache_v, state_pool = map(f, (x_prompt, x_sample, cache_k, cache_v, state_pool))
    c_prompt, c_sample = f(c_prompt), f(c_sample)
    wi = f(w_in)[0]
    kcol = wi[:, 2048:2304].reshape(D, 4, 1, 64)
    vcol = wi[:, 2304:2560].reshape(D, 4, 1, 64)
    win_r = np.concatenate([np.broadcast_to(kcol, (D, 4, 2, 64)).reshape(D, 512),
                            np.broadcast_to(vcol, (D, 4, 2, 64)).reshape(D, 512),
                            wi[:, 0:2048], wi[:, 2560:4608]], axis=1)
    vecs = np.zeros((128, 384), np.float32)
    vecs[:, 0:192] = f(b_ada)[0].reshape(192, 128).T
    vecs[:, 192:208] = f(pool_scale)[0].reshape(16, 128).T
    for i, a in enumerate((ln1_g, ln1_b, ln2_g, ln2_b)):
        vecs[:, 208 + 32 * i:240 + 32 * i] = f(a)[0].reshape(32, 128).T
    vecs[:, 336:368] = np.broadcast_to(f(sinks)[0].reshape(16, 2).T.reshape(1, 32), (128, 32))
    common = {
        "w_ada": _bf(f(w_ada)[0]), "w_in": _bf(win_r), "w_out": _bf(f(w_out)[0]), "wq": _bf(f(peer_wq)[0]),
        "subk": _bf(f(peer_subkeys)[0].transpose(3, 0, 1, 2).reshape(128, 16, 128)),
        "UT": _bf(f(peer_u)[0].T), "V": _bf(f(peer_v)[0]),
        "poolw": _bf(f(pool_w)[0].reshape(4, 4, 128, 512).transpose(2, 0, 1, 3)),
        "vecs": vecs,
    }
    in_maps = []
    for r in range(8):
        b, hf = r // 2, r % 2
        xs = x_sample[16 * r:16 * r + 16].reshape(128, D)
        t0 = hf * 1024
        halo = x_prompt[b, t0 - 128:t0] if hf == 1 else np.zeros((128, D), np.float32)
        xT = np.concatenate([halo, x_prompt[b, t0:t0 + 1024], xs], axis=0).T
        cc = np.concatenate([c_prompt[b:b + 1], c_sample[16 * r:16 * r + 16]], axis=0)
        ck = cache_k[0, 16 * r:16 * r + 16]
        cvv = cache_v[0, 16 * r:16 * r + 16]
        ckt = ck.transpose(3, 0, 2, 1)
        cvt = cvv.transpose(1, 0, 2, 3)
        sp = state_pool[0, 16 * r:16 * r + 16]
        m = dict(common)
        m.update({
            "xT": _bf(xT),
            "cT": _bf(cc.T.reshape(32, 128, 17).transpose(1, 0, 2)),
            "cmat": _consts(hf), "rope": _rope_tab(hf),
            "cKT": _bf(np.concatenate([ckt, ckt], axis=0)),
            "cV": _bf(np.concatenate([cvt, cvt], axis=3)),
            "ckn": _bf(ck.reshape(16, 128, 256)), "cvn": _bf(cvv.reshape(16, 128, 256)),
            "sptm": _bf(sp.reshape(2, 120, 2048).transpose(1, 0, 2)), "spn": _bf(sp),
        })
        in_maps.append(m)
    return in_maps


def _assemble(res, cores=tuple(range(8))):
    y_p = np.zeros((4, 2048, D), np.float32)
    y_s = np.zeros((128, 8, D), np.float32)
    nkp = np.zeros((1, 4, 128, 4, 64), np.float32)
    nvp = np.zeros_like(nkp)
    npp = np.zeros((1, 4, 15, 2048), np.float32)
    nks = np.zeros((1, 128, 128, 4, 64), np.float32)
    nvs = np.zeros_like(nks)
    nps = np.zeros((1, 128, 15, 2048), np.float32)
    for r in cores:
        b, hf = r // 2, r % 2
        o = res[cores.index(r)]
        yT = o["yT"]
        y_p[b, hf * 1024:(hf + 1) * 1024] = yT[:, 0:1024].T
        y_s[16 * r:16 * r + 16] = yT[:, 1024:1152].T.reshape(16, 8, D)
        if hf == 1:
            nkp[0, b] = o["nkp"].reshape(128, 4, 64)
            nvp[0, b] = o["nvp"].reshape(128, 4, 64)
            npp[0, b] = o["npp"]
        nks[0, 16 * r:16 * r + 16] = o["nks"].reshape(16, 128, 4, 64)
        nvs[0, 16 * r:16 * r + 16] = o["nvs"].reshape(16, 128, 4, 64)
        nps[0, 16 * r:16 * r + 16] = o["nps"]
    return (y_p, y_s, nkp, nvp, npp, nks, nvs, nps)


def kernel(**inputs):
    in_maps = _prep(**inputs)
    if 'nc' not in _NC_CACHE:
        _NC_CACHE['nc'] = build_nc()
    res = run_bass_kernel_spmd(_NC_CACHE['nc'], in_maps, core_ids=list(range(8))).results
    return _assemble(res)
```
